# Optimizing a Trainium2 kernel written in Bass

```python
import jax, jax.numpy as jnp
from jax import lax
import numpy as np

D_MODEL = 1024
BATCH = 8
SEQ = 8192
DEPTH = 2

CHUNK = 64
EPS = 1e-6
HG_HEADS = 4
HG_DK = 128
HG_DV = 128
HG_WIDTH = HG_HEADS * HG_DK
HG_VWIDTH = HG_HEADS * HG_DV
DN_HEADS = 4
DN_DK = 128
DN_DV = 128
DN_KW = DN_HEADS * DN_DK
DN_VW = DN_HEADS * DN_DV
CONV_W = 4
D_FF = -(-8 * D_MODEL // (3 * 256)) * 256
IN_SIZES = (HG_WIDTH, HG_WIDTH, HG_VWIDTH, HG_VWIDTH,
            DN_KW, DN_KW, DN_VW,
            DN_VW, DN_HEADS, DN_HEADS,
            D_MODEL, D_MODEL)
IN_WIDTH = sum(IN_SIZES)
IN_SPLITS = tuple(int(s) for s in np.cumsum(IN_SIZES)[:-1])

kernel_name = "hgrn2_gdn_parallel_hybrid"


def rms_norm(x, w):
    xf = x.astype(jnp.float32)
    y = xf * lax.rsqrt(jnp.mean(xf * xf, axis=-1, keepdims=True) + EPS)
    return (y * w.astype(jnp.float32)).astype(x.dtype)


def head_rms_norm(o, w, n_heads, d):
    b, t, _ = o.shape
    oh = o.reshape(b, t, n_heads, d)
    oh = oh * lax.rsqrt(jnp.mean(oh * oh, axis=-1, keepdims=True) + EPS) * w.astype(jnp.float32)
    return oh.reshape(b, t, n_heads * d)


def _chunk(t, n_heads, d):
    b, s, _ = t.shape
    return t.reshape(b, s // CHUNK, CHUNK, n_heads, d).transpose(1, 0, 3, 2, 4)


def _chunk_heads(t):
    b, s, h = t.shape
    return t.reshape(b, s // CHUNK, CHUNK, h).transpose(1, 0, 3, 2)


def _unchunk(o):
    n, b, h, c, d = o.shape
    return o.transpose(1, 0, 3, 2, 4).reshape(b, n * c, h * d)


def causal_short_conv(x, w):
    cc = x.shape[-1]
    return lax.conv_general_dilated(x, w[:, None, :].astype(x.dtype), window_strides=(1,),
                                    padding=[(CONV_W - 1, 0)],
                                    dimension_numbers=('NWC', 'WIO', 'NWC'),
                                    feature_group_count=cc)


def hgrn2_mix(q, f_logit, i, lb):
    q = q.astype(jnp.float32)
    f_logit = f_logit.astype(jnp.float32)
    i = i.astype(jnp.float32)
    log_f = jnp.logaddexp(jnp.log(lb), jnp.log1p(-lb) + jax.nn.log_sigmoid(f_logit))
    k = (1.0 - lb) * jax.nn.sigmoid(-f_logit)
    qc = _chunk(q, HG_HEADS, HG_DK)
    kc = _chunk(k, HG_HEADS, HG_DK)
    vc = _chunk(i, HG_HEADS, HG_DV)
    bc = jnp.cumsum(_chunk(log_f, HG_HEADS, HG_DK), axis=3)
    causal = jnp.tril(jnp.ones((CHUNK, CHUNK), dtype=bool))

    def step(S, xs):
        q_c, k_c, v_c, b_c = xs
        b_last = b_c[:, :, -1:, :]
        diff = b_c[:, :, :, None, :] - b_c[:, :, None, :, :]
        decay = jnp.exp(jnp.where(causal[:, :, None], diff, -jnp.inf))
        attn = jnp.einsum('bhid,bhjd,bhijd->bhij', q_c, k_c, decay)
        o = (jnp.einsum('bhij,bhjv->bhiv', attn, v_c)
             + jnp.einsum('bhid,bhdv->bhiv', q_c * jnp.exp(b_c), S))
        S = (S * jnp.exp(b_last[:, :, 0, :])[..., None]
             + jnp.einsum('bhjd,bhjv->bhdv', k_c * jnp.exp(b_last - b_c), v_c))
        return S, o

    S0 = jnp.zeros((q.shape[0], HG_HEADS, HG_DK, HG_DV), jnp.float32)
    _, o = lax.scan(step, S0, (qc, kc, vc, bc))
    return _unchunk(o)


def gated_deltanet_mix(q, k, v, a, b_logit, a_log, dt_bias):
    q = q.astype(jnp.float32)
    k = k.astype(jnp.float32)
    v = v.astype(jnp.float32)
    b, s, _ = q.shape
    qh = q.reshape(b, s, DN_HEADS, DN_DK)
    kh = k.reshape(b, s, DN_HEADS, DN_DK)
    qh = qh * lax.rsqrt(jnp.sum(qh * qh, -1, keepdims=True) + EPS) * (DN_DK ** -0.5)
    kh = kh * lax.rsqrt(jnp.sum(kh * kh, -1, keepdims=True) + EPS)
    beta = jax.nn.sigmoid(b_logit.astype(jnp.float32))
    g = -jnp.exp(a_log.astype(jnp.float32)) * jax.nn.softplus(
        a.astype(jnp.float32) + dt_bias.astype(jnp.float32))

    qc = _chunk(qh.reshape(b, s, DN_KW), DN_HEADS, DN_DK)
    kc = _chunk(kh.reshape(b, s, DN_KW), DN_HEADS, DN_DK)
    vc = _chunk(v, DN_HEADS, DN_DV)
    betac = _chunk_heads(beta)[..., None]
    gc = jnp.cumsum(_chunk_heads(g), axis=-1)

    causal = jnp.tril(jnp.ones((CHUNK, CHUNK), dtype=bool))
    strict = jnp.tril(jnp.ones((CHUNK, CHUNK), dtype=bool), -1)
    L = jnp.exp(jnp.where(causal, gc[..., :, None] - gc[..., None, :], -jnp.inf))
    k_beta = kc * betac
    v_beta = vc * betac
    A = jnp.where(strict, jnp.einsum('nbhid,nbhjd->nbhij', k_beta, kc) * L, 0.0)
    eye = jnp.eye(CHUNK, dtype=jnp.float32)
    M = eye + A
    T = lax.linalg.triangular_solve(M, jnp.broadcast_to(eye, M.shape), left_side=True,
                                    lower=True, unit_diagonal=True)
    u = jnp.einsum('nbhij,nbhjv->nbhiv', T, v_beta)
    w = jnp.einsum('nbhij,nbhjd->nbhid', T, k_beta * jnp.exp(gc)[..., None])
    attn_qk = jnp.einsum('nbhid,nbhjd->nbhij', qc, kc) * L
    qg = qc * jnp.exp(gc)[..., None]
    kd = kc * jnp.exp(gc[..., -1:] - gc)[..., None]
    dl = jnp.exp(gc[..., -1])

    def step(S, xs):
        u_c, w_c, a_c, qg_c, kd_c, dl_c = xs
        v_new = u_c - jnp.einsum('bhcd,bhdv->bhcv', w_c, S)
        o = jnp.einsum('bhcd,bhdv->bhcv', qg_c, S) + jnp.einsum('bhij,bhjv->bhiv', a_c, v_new)
        S = S * dl_c[..., None, None] + jnp.einsum('bhcd,bhcv->bhdv', kd_c, v_new)
        return S, o

    S0 = jnp.zeros((b, DN_HEADS, DN_DK, DN_DV), jnp.float32)
    _, o = lax.scan(step, S0, (u, w, attn_qk, qg, kd, dl))
    return _unchunk(o)


def hybrid_layer(x, lb, pre_mix_w, w_in, hg_norm_w, conv_w, dn_a_log, dn_dt_bias, dn_norm_w,
                 w_o_hg, w_o_dn, w_out, post_mix_w, pre_ffn_w, w_gate_up, w_down, post_ffn_w):
    u = rms_norm(x, pre_mix_w)
    proj = jnp.einsum('btd,de->bte', u, w_in)
    (hg_q, hg_f, hg_i, hg_g, dn_q, dn_k, dn_v, dn_g, dn_a, dn_b,
     m_a, m_b) = jnp.split(proj, IN_SPLITS, axis=-1)

    o_a = hgrn2_mix(hg_q, hg_f, hg_i, lb)
    o_a = head_rms_norm(o_a, hg_norm_w, HG_HEADS, HG_DV) * jax.nn.silu(hg_g.astype(jnp.float32))

    qkv = jax.nn.silu(causal_short_conv(jnp.concatenate([dn_q, dn_k, dn_v], axis=-1), conv_w))
    cq, ck, cv = jnp.split(qkv, (DN_KW, 2 * DN_KW), axis=-1)
    o_b = gated_deltanet_mix(cq, ck, cv, dn_a, dn_b, dn_a_log, dn_dt_bias)
    o_b = head_rms_norm(o_b, dn_norm_w, DN_HEADS, DN_DV) * jax.nn.silu(dn_g.astype(jnp.float32))

    y_a = jnp.einsum('bte,ed->btd', o_a.astype(x.dtype), w_o_hg)
    y_b = jnp.einsum('bte,ed->btd', o_b.astype(x.dtype), w_o_dn)
    merged = jax.nn.sigmoid(m_a) * y_a + jax.nn.sigmoid(m_b) * y_b
    mix_out = jnp.einsum('btd,de->bte', merged, w_out)
    h = x + rms_norm(mix_out, post_mix_w)

    v = rms_norm(h, pre_ffn_w)
    gate, up = jnp.split(jnp.einsum('btd,df->btf', v, w_gate_up), 2, axis=-1)
    f = jnp.einsum('btf,fd->btd', jax.nn.silu(gate) * up, w_down)
    return h + rms_norm(f, post_ffn_w)


def setup_inputs(seed: int = 0) -> dict:
    key = jax.random.key(seed)
    ks = jax.random.split(key, 20)
    f32 = jnp.float32

    def nrm(k, shape, scale):
        return jax.random.normal(k, shape, f32) * scale

    def gain(k, n):
        return 1.0 + 0.02 * jax.random.normal(k, (DEPTH, n), f32)

    dt = jnp.exp(jax.random.uniform(ks[6], (DEPTH, DN_HEADS), f32, np.log(1e-3), np.log(1e-1)))
    return {
        "x": jax.random.normal(ks[0], (BATCH, SEQ, D_MODEL), f32),
        "hg_lower_bounds": nrm(ks[1], (DEPTH, HG_WIDTH), 0.1),
        "pre_mix_w": gain(ks[2], D_MODEL),
        "w_in": nrm(ks[3], (DEPTH, D_MODEL, IN_WIDTH), D_MODEL ** -0.5),
        "hg_norm_w": gain(ks[4], HG_DV),
        "conv_w": nrm(ks[5], (DEPTH, CONV_W, 2 * DN_KW + DN_VW), CONV_W ** -0.5),
        "dn_a_log": jnp.log(jax.random.uniform(ks[7], (DEPTH, DN_HEADS), f32, 1.0, 16.0)),
        "dn_dt_bias": dt + jnp.log(-jnp.expm1(-dt)),
        "dn_norm_w": gain(ks[8], DN_DV),
        "w_o_hg": nrm(ks[9], (DEPTH, HG_VWIDTH, D_MODEL), HG_VWIDTH ** -0.5),
        "w_o_dn": nrm(ks[10], (DEPTH, DN_VW, D_MODEL), DN_VW ** -0.5),
        "w_out": nrm(ks[11], (DEPTH, D_MODEL, D_MODEL), D_MODEL ** -0.5),
        "post_mix_w": gain(ks[12], D_MODEL),
        "pre_ffn_w": gain(ks[13], D_MODEL),
        "w_gate_up": nrm(ks[14], (DEPTH, D_MODEL, 2 * D_FF), D_MODEL ** -0.5),
        "w_down": nrm(ks[15], (DEPTH, D_FF, D_MODEL), D_FF ** -0.5),
        "post_ffn_w": gain(ks[16], D_MODEL),
    }


def reference(x, hg_lower_bounds, pre_mix_w, w_in, hg_norm_w, conv_w, dn_a_log, dn_dt_bias,
              dn_norm_w, w_o_hg, w_o_dn, w_out, post_mix_w, pre_ffn_w, w_gate_up, w_down,
              post_ffn_w):
    lbs = jnp.cumsum(jax.nn.softmax(hg_lower_bounds.astype(jnp.float32), axis=0), axis=0)
    lbs = lbs - lbs[0]
    h = x
    for l in range(DEPTH):
        h = hybrid_layer(h, lbs[l], pre_mix_w[l], w_in[l], hg_norm_w[l], conv_w[l], dn_a_log[l],
                         dn_dt_bias[l], dn_norm_w[l], w_o_hg[l], w_o_dn[l], w_out[l],
                         post_mix_w[l], pre_ffn_w[l], w_gate_up[l], w_down[l], post_ffn_w[l])
    return h
```

```python
from contextlib import ExitStack

import numpy as np
import concourse.bass as bass
import concourse.mybir as mybir
from concourse.bass_utils import run_bass_kernel_spmd

F32 = mybir.dt.float32
BF16 = mybir.dt.bfloat16
AF = mybir.ActivationFunctionType
ALU = mybir.AluOpType

D = 1024
DFF = 2816
INW = 6152
EPS = 1e-6
ENGS = ("pe", "act", "dve", "pool", "sp")
CUT = None


class Tok:
    __slots__ = ("last_w", "readers")

    def __init__(self):
        self.last_w = None
        self.readers = []


class Op:
    __slots__ = ("eng", "fn", "idx", "deps", "signal", "semval", "clock", "dma_chan", "waits", "barrier")

    def __init__(self, eng, fn):
        self.eng = eng
        self.fn = fn
        self.idx = -1
        self.deps = []
        self.signal = False
        self.semval = 0
        self.clock = None
        self.dma_chan = None
        self.waits = []
        self.barrier = False


class Prog:
    def __init__(self):
        self.streams = {e: [] for e in ENGS}
        self.chan_count = {}
        self.chan_last = {}
        self.all_ops = []

    def op(self, eng, fn, reads=(), writes=(), dma_chan=None):
        o = Op(eng, fn)
        o.dma_chan = dma_chan
        deps = []
        for r in reads:
            if r.last_w is not None:
                deps.append((r.last_w, "raw"))
        for w in writes:
            if w.last_w is not None:
                deps.append((w.last_w, "waw"))
            for rd in w.readers:
                deps.append((rd, "war"))
        o.deps = deps
        for r in reads:
            r.readers.append(o)
        for w in writes:
            w.last_w = o
            w.readers = []
        o.idx = len(self.streams[eng])
        self.streams[eng].append(o)
        self.all_ops.append(o)
        if dma_chan is not None:
            c = self.chan_count.get(dma_chan, 0) + 1
            self.chan_count[dma_chan] = c
            o.semval = 16 * c
            self.chan_last[dma_chan] = o
        return o

    def barrier(self):
        lasts = []
        for e in ENGS:
            for o in reversed(self.streams[e]):
                if o.dma_chan is None and not o.barrier:
                    lasts.append(o)
                    break
        dl = list(self.chan_last.values())
        for e in ENGS:
            o = Op(e, None)
            o.barrier = True
            o.deps = [(d, "raw") for d in lasts if d.eng != e] + [(d, "raw") for d in dl]
            o.idx = len(self.streams[e])
            self.streams[e].append(o)
            self.all_ops.append(o)

    def resolve(self):
        eng_clock = {e: ({}, {}) for e in ENGS}
        for o in self.all_ops:
            cc, dc = eng_clock[o.eng]
            waits = []
            for d, kind in o.deps:
                if d is o:
                    continue
                if d.dma_chan is not None:
                    if dc.get(d.dma_chan, 0) >= d.semval:
                        continue
                    waits.append(d)
                    dc[d.dma_chan] = d.semval
                else:
                    if d.eng == o.eng and kind != "raw" and o.eng == "pe":
                        continue
                    if cc.get(d.eng, -1) >= d.idx:
                        continue
                    waits.append(d)
                    d.signal = True
                    cc[d.eng] = d.idx
                dcc, ddc = d.clock
                for k, v in dcc.items():
                    if cc.get(k, -1) < v:
                        cc[k] = v
                for k, v in ddc.items():
                    if dc.get(k, 0) < v:
                        dc[k] = v
            best = {}
            for d in waits:
                if d.dma_chan is not None:
                    key = ("d", d.dma_chan)
                    val = d.semval
                else:
                    key = ("c", d.eng)
                    val = d.idx
                cur = best.get(key)
                if cur is None or val > cur[0]:
                    best[key] = (val, d)
            o.waits = [v[1] for v in best.values()]
            o.clock = (dict(cc), dict(dc))
            if o.dma_chan is None:
                o.clock[0][o.eng] = max(o.clock[0].get(o.eng, -1), o.idx - 1)
        for e in ENGS:
            n = 0
            for o in self.streams[e]:
                if o.dma_chan is None and o.signal:
                    n += 1
                    o.semval = n

    def emit(self, nc, final_waits=()):
        self.resolve()
        with ExitStack() as es:
            sems = {}
            for e in ENGS:
                sems[("c", e)] = es.enter_context(nc.semaphore("s_" + e))
            for ch in self.chan_count:
                sems[("d", ch)] = es.enter_context(nc.semaphore("d_" + str(ch)))
            block = es.enter_context(nc.Block())

            def run_stream(ename):
                def body(eng):
                    for o in self.streams[ename]:
                        for d in o.waits:
                            if d.dma_chan is not None:
                                eng.wait_ge(sems[("d", d.dma_chan)], d.semval)
                            else:
                                eng.wait_ge(sems[("c", d.eng)], d.semval)
                        if o.fn is None:
                            continue
                        ins = o.fn(eng)
                        if o.dma_chan is not None:
                            ins.then_inc(sems[("d", o.dma_chan)], 16)
                        elif o.signal:
                            ins.then_inc(sems[("c", ename)], 1)
                    if ename == "sp":
                        done = {}
                        for d in final_waits:
                            done[d.dma_chan] = max(done.get(d.dma_chan, 0), d.semval)
                        for ch, v in done.items():
                            eng.wait_ge(sems[("d", ch)], v)

                return body

            block.tensor(run_stream("pe"))
            block.scalar(run_stream("act"))
            block.vector(run_stream("dve"))
            block.gpsimd(run_stream("pool"))
            block.sync(run_stream("sp"))


class V:
    __slots__ = ("ap", "toks")

    def __init__(self, ap, toks):
        self.ap = ap
        self.toks = tuple(toks)


class Buf:
    def __init__(self, t, nparts=1):
        self.t = t
        self.toks = [Tok() for _ in range(nparts)]

    def __getitem__(self, idx):
        return V(self.t[idx], self.toks)

    def p(self, i, idx):
        if isinstance(i, int):
            return V(self.t[idx], (self.toks[i],))
        return V(self.t[idx], [self.toks[j] for j in i])


class K:
    def __init__(self, nc):
        self.nc = nc
        self.P = Prog()

    def sb(self, es, name, shape, dtype, nparts=1):
        self._uid = getattr(self, "_uid", 0) + 1
        return Buf(es.enter_context(self.nc.sbuf_tensor("%s_%d" % (name, self._uid), list(shape), dtype)), nparts)

    def ps(self, es, name, shape, dtype):
        return Buf(es.enter_context(self.nc.psum_tensor(name, list(shape), dtype)), 1)

    @staticmethod
    def _rw(*vs):
        t = []
        for v in vs:
            if isinstance(v, V):
                t.extend(v.toks)
        return t

    @staticmethod
    def _a(v):
        return v.ap if isinstance(v, V) else v

    def mm(self, out, lhsT, rhs, start=True, stop=True):
        self.P.op("pe", lambda e: e.matmul(out.ap, lhsT=lhsT.ap, rhs=rhs.ap, start=start, stop=stop),
                  reads=self._rw(lhsT, rhs), writes=out.toks)

    def tr(self, out, in_, ident):
        self.P.op("pe", lambda e: e.transpose(out=out.ap, in_=in_.ap, identity=ident.ap),
                  reads=self._rw(in_, ident), writes=out.toks)

    def act(self, out, in_, func, bias=None, scale=None, accum=None, eng="act"):
        kw = {}
        if bias is not None:
            kw["bias"] = self._a(bias)
        if scale is not None:
            kw["scale"] = self._a(scale)
        if accum is not None:
            kw["accum_out"] = accum.ap
        self.P.op(eng, lambda e: e.activation(out=out.ap, in_=in_.ap, func=func, **kw),
                  reads=self._rw(in_, bias, scale), writes=self._rw(out, accum))

    def tt(self, out, in0, in1, op, eng="dve"):
        self.P.op(eng, lambda e: e.tensor_tensor(out=out.ap, in0=in0.ap, in1=in1.ap, op=op),
                  reads=self._rw(in0, in1), writes=out.toks)

    def ts(self, out, in0, s1, s2, op0, op1=None, eng="dve", accum=None):
        kw = {}
        if op1 is not None:
            kw["op1"] = op1
        if accum is not None:
            kw["accum_out"] = accum.ap
        self.P.op(eng, lambda e: e.tensor_scalar(out=out.ap, in0=in0.ap, scalar1=self._a(s1), scalar2=self._a(s2),
                                                 op0=op0, **kw),
                  reads=self._rw(in0, s1, s2), writes=self._rw(out, accum))

    def stt(self, out, in0, scalar, in1, op0, op1, eng="dve"):
        self.P.op(eng, lambda e: e.scalar_tensor_tensor(out=out.ap, in0=in0.ap, scalar=self._a(scalar), in1=in1.ap,
                                                        op0=op0, op1=op1),
                  reads=self._rw(in0, scalar, in1), writes=out.toks)

    def cp(self, out, in_, eng="dve"):
        if eng == "act":
            self.P.op("act", lambda e: e.copy(out=out.ap, in_=in_.ap), reads=in_.toks, writes=out.toks)
        else:
            self.P.op(eng, lambda e: e.tensor_copy(out=out.ap, in_=in_.ap), reads=in_.toks, writes=out.toks)

    def recip(self, out, in_):
        self.P.op("dve", lambda e: e.reciprocal(out=out.ap, in_=in_.ap), reads=in_.toks, writes=out.toks)

    def memset(self, out, val, eng="pool"):
        self.P.op(eng, lambda e: e.memset(out.ap, val), writes=out.toks)

    def asel(self, out, in_, pattern, cmp, fill, base, cm):
        self.P.op("pool", lambda e: e.affine_select(out=out.ap, in_=in_.ap, pattern=pattern, compare_op=cmp,
                                                    fill=fill, base=base, channel_multiplier=cm),
                  reads=in_.toks, writes=out.toks)

    def dma(self, q, out, in_, chan, noncontig=False):
        kw = {"allow_slow_non_contiguous": True} if noncontig else {}
        return self.P.op(q, lambda e: e.dma_start(out=self._a(out), in_=self._a(in_), **kw),
                         reads=self._rw(in_), writes=self._rw(out), dma_chan=chan)


class DramT:
    def __init__(self, ap, ntiles):
        self.ap = ap
        self.toks = [Tok() for _ in range(max(1, ntiles))]

    def rows(self, t, n=128):
        return V(self.ap[t * 128:t * 128 + n], (self.toks[t],))


def build(T=8192, L=2, phases=("A", "B", "C"), dbg=()):
    NT = T // 128
    nc = bass.Bass("TRN2", target_bir_lowering=False)
    k = K(nc)
    P = k.P

    def din(name, shape):
        return nc.dram_tensor(name, list(shape), F32, kind="ExternalInput").ap()

    x_d = DramT(din("x", [T, D]), NT)
    hglb_d = din("hg_lower_bounds", [L, 512])
    pre_mix_d = din("pre_mix_w", [L, D])
    w_in_d = din("w_in", [L, D, INW])
    hg_norm_d = din("hg_norm_w", [L, 128])
    conv_d = din("conv_w", [L, 4, 1536])
    alog_d = din("dn_a_log", [L, 4])
    dtb_d = din("dn_dt_bias", [L, 4])
    dn_norm_d = din("dn_norm_w", [L, 128])
    w_o_hg_d = din("w_o_hg", [L, 512, D])
    w_o_dn_d = din("w_o_dn", [L, 512, D])
    w_out_d = din("w_out", [L, D, D])
    post_mix_d = din("post_mix_w", [L, D])
    pre_ffn_d = din("pre_ffn_w", [L, D])
    w_gu_d = din("w_gate_up", [L, D, 2 * DFF])
    w_dn_d = din("w_down", [L, DFF, D])
    post_ffn_d = din("post_ffn_w", [L, D])
    out_d = DramT(nc.dram_tensor("out", [T, D], F32, kind="ExternalOutput").ap(), NT)
    skind = "ExternalOutput" if dbg else "Internal"
    hbuf_d = DramT(nc.dram_tensor("hbuf", [T, D], F32, kind=skind).ap(), NT)
    x1_d = DramT(nc.dram_tensor("x1buf", [T, D], F32, kind=skind).ap(), NT)
    oab_d = DramT(nc.dram_tensor("oab", [T, D], BF16, kind=skind).ap(), NT)

    final_ops = []
    with ExitStack() as pes:
        PF = [k.ps(pes, "pf%d" % i, [128, 512], F32) for i in range(6)]
        PB = [k.ps(pes, "pb%d" % i, [128, 1024], BF16) for i in range(2)]
        ident_f = k.sb(pes, "ident_f", [128, 128], F32)
        ident = k.sb(pes, "ident", [128, 128], BF16)
        k.memset(ident_f[:], 1.0)
        k.asel(ident_f[:], ident_f[:], [[-1, 128]], ALU.is_equal, 0.0, 0, 1)
        k.cp(ident[:], ident_f[:])

        if "A" in phases:
            mk = make_masks(k, pes)
        for l in range(L):
            xin = x_d if l == 0 else x1_d
            xout = out_d if l == L - 1 else x1_d
            if "A" in phases:
                with ExitStack() as es:
                    phase_a(k, es, l, NT, PF, PB, ident, ident_f, xin, oab_d, hglb_d, pre_mix_d, w_in_d, hg_norm_d,
                            conv_d, alog_d, dtb_d, dn_norm_d, mk)
                P.barrier()
            if "B" in phases:
                with ExitStack() as es:
                    phase_b(k, es, l, NT, PF, PB, ident, ident_f, xin, oab_d, hbuf_d, pre_mix_d, w_in_d, w_o_hg_d, w_o_dn_d,
                            w_out_d, post_mix_d)
                P.barrier()
            if "C" in phases:
                with ExitStack() as es:
                    phase_c(k, es, l, NT, PF, PB, ident, ident_f, hbuf_d if ("A" in phases or "B" in phases) else xin,
                            xout, pre_ffn_d, w_gu_d, w_dn_d, post_ffn_d, final_ops, l == L - 1)
                P.barrier()
        P.emit(nc, final_waits=final_ops)
    return nc


def rms_stats(k, src_list, ss, tmp_junk, rstd, lnv):
    n = len(src_list)
    for i, s in enumerate(src_list):
        k.act(tmp_junk[i], s, AF.Square, scale=float(D ** -0.5), accum=ss[:, i:i + 1])
    if n == 2:
        k.tt(ss[:, 0:1], ss[:, 0:1], ss[:, 1:2], ALU.add)
    k.act(lnv[:, 0:1], ss[:, 0:1], AF.Ln, bias=EPS)
    k.act(rstd[:, 0:1], lnv[:, 0:1], AF.Exp, scale=-0.5)


def load_weight_cast(k, dst, src_ap, chan_prefix, counter):
    ch = "%s%d" % (chan_prefix, counter[0])
    counter[0] += 1
    return k.dma("pool", dst, src_ap, ch)


def phase_c(k, es, l, NT, PF, PB, ident, ident_f, hin_d, xout_d, pre_ffn_d, w_gu_d, w_dn_d, post_ffn_d, final_ops, is_last):
    nc = k.nc
    NB = 6
    bw = [512] * 5 + [256]
    boff = [512 * i for i in range(6)]
    wg = [k.sb(es, "wg%d" % b, [128, 8, bw[b]], BF16) for b in range(NB)]
    wu = [k.sb(es, "wu%d" % b, [128, 8, bw[b]], BF16) for b in range(NB)]
    wd = [k.sb(es, "wd%d" % h, [128, 11, D], BF16) for h in range(2)]
    gcol = load_gcol(k, es, "gcol", pre_ffn_d[l], PF[0], ident_f, "cst0")
    gbc = k.sb(es, "gbc", [128, D], F32)
    cnt = [0]
    k.dma("sp", gbc[:], post_ffn_d[l:l + 1, :].partition_broadcast(128), "cst1")
    wsrc = w_gu_d[l].rearrange("(k p) n -> p k n", p=128)
    for b in range(NB):
        for (wt, off) in ((wg[b], boff[b]), (wu[b], DFF + boff[b])):
            load_weight_cast(k, wt[:], wsrc[:, :, off:off + bw[b]], "w", cnt)
    dsrc = w_dn_d[l].rearrange("(k p) n -> p k n", p=128)
    for h in range(2):
        load_weight_cast(k, wd[h][:], dsrc[:, 11 * h:11 * h + 11, :], "w", cnt)
    for b in range(NB):
        for wt in (wg[b], wu[b]):
            k.tt(wt[:], wt[:], V(gcol.t[:, :].unsqueeze(2).broadcast_to([128, 8, bw[b]]), gcol.toks), ALU.mult)

    ht = [k.sb(es, "ht%d" % i, [128, D], F32) for i in range(2)]
    ot = [k.sb(es, "ot%d" % i, [128, D], F32) for i in range(2)]
    junk = k.sb(es, "junk", [128, D], BF16, nparts=2)
    ss = k.sb(es, "ss", [128, 2], F32)
    lnv = k.sb(es, "lnv", [128, 1], F32)
    rstd = k.sb(es, "rstd", [128, 1], F32)
    ss2 = k.sb(es, "ss2", [128, 2], F32)
    lnv2 = k.sb(es, "lnv2", [128, 1], F32)
    rstd2 = k.sb(es, "rstd2", [128, 1], F32)
    vb = k.sb(es, "vb", [128, D], BF16)
    vT = k.sb(es, "vT", [128, D], BF16)
    sg = [k.sb(es, "sg%d" % i, [128, 512], F32) for i in range(2)]
    actb = k.sb(es, "actb", [128, DFF], BF16, nparts=NB)
    actT = k.sb(es, "actT", [128, DFF], BF16, nparts=3)

    for t in range(NT):
        h = ht[t % 2]
        o = ot[t % 2]
        k.dma("sp", h[:], hin_d.rows(t), "hin%d" % (t % 2))
        rms_stats(k, [h[:]], ss, [junk.p(0, (slice(None), slice(None)))], rstd, lnv)
        k.act(vb[:], h[:], AF.Copy, scale=rstd[:, 0:1])
        for c in range(8):
            k.tr(PB[0][:, c * 128:(c + 1) * 128], vb[:, c * 128:(c + 1) * 128], ident[:])
        k.cp(vT[:], PB[0][:])
        for b in range(NB):
            pg = PF[(2 * b) % 4]
            pu = PF[(2 * b + 1) % 4]
            n = bw[b]
            for c in range(8):
                k.mm(pg[:, 0:n], vT[:, c * 128:(c + 1) * 128], wg[b][:, c, :], start=(c == 0), stop=(c == 7))
            for c in range(8):
                k.mm(pu[:, 0:n], vT[:, c * 128:(c + 1) * 128], wu[b][:, c, :], start=(c == 0), stop=(c == 7))
            s = sg[b % 2]
            k.act(s[:, 0:n], pg[:, 0:n], AF.Silu)
            k.tt(actb.p(b, (slice(None), slice(boff[b], boff[b] + n))), s[:, 0:n], pu[:, 0:n], ALU.mult)
        for g in range(3):
            c0 = g * 8
            c1 = min(22, c0 + 8)
            pb = PB[(g + 1) % 2]
            for c in range(c0, c1):
                blk = (c * 128) // 512
                k.tr(pb[:, (c - c0) * 128:(c - c0 + 1) * 128],
                     actb.p(blk, (slice(None), slice(c * 128, (c + 1) * 128))), ident[:])
            k.cp(actT.p(g, (slice(None), slice(c0 * 128, c1 * 128))), pb[:, 0:(c1 - c0) * 128],
                 eng=("act" if g == 1 else "dve"))
        for hh in range(2):
            pf = PF[4 + hh]
            for c in range(22):
                k.mm(pf[:], actT.p(c // 8, (slice(None), slice(c * 128, (c + 1) * 128))),
                     wd[c // 11][:, c % 11, hh * 512:(hh + 1) * 512], start=(c == 0), stop=(c == 21))
        rms_stats(k, [PF[4][:], PF[5][:]], ss2,
                  [junk.p(0, (slice(None), slice(0, 512))), junk.p(1, (slice(None), slice(512, 1024)))], rstd2, lnv2)
        for hh in range(2):
            sl = slice(hh * 512, (hh + 1) * 512)
            k.stt(o[:, sl], PF[4 + hh][:], rstd2[:, 0:1], gbc[:, sl], ALU.mult, ALU.mult)
        k.tt(o[:], o[:], h[:], ALU.add, eng="pool")
        d = k.dma("sp", xout_d.rows(t), o[:], "oout%d" % (t % 2))
        if is_last:
            final_ops.append(d)


def bc(buf, idx, shape):
    ap = buf.t[idx]
    return V(ap.unsqueeze(len(ap.shape)).broadcast_to(list(shape)), buf.toks)


def bc_mid(buf, idx, shape):
    ap = buf.t[idx]
    return V(ap.unsqueeze(1).broadcast_to(list(shape)), buf.toks)


def load_wcols(k, es, name, src_l, c0, c1, cnt, kch=8):
    wt = k.sb(es, name, [128, kch, c1 - c0], BF16)
    load_weight_cast(k, wt[:], src_l.rearrange("(k p) n -> p k n", p=128)[:, :, c0:c1], "w", cnt)
    return wt


def load_gcol(k, es, name, src_row_ap, PFb, ident_f, chan):
    rows = k.sb(es, name + "_r", [8, 128], F32)
    gcol = k.sb(es, name, [128, 8], F32)
    k.dma("sp", rows[:], src_row_ap.rearrange("(k p) -> k p", p=128), chan)
    k.tr(PFb[:, 0:8], rows[:], ident_f[0:8, 0:8])
    k.cp(gcol[:], PFb[:, 0:8])
    return gcol


def fold_gain(k, wt, gcol, kch, n):
    k.tt(wt[:], wt[:], V(gcol.t[:, :].unsqueeze(2).broadcast_to([128, kch, n]), gcol.toks), ALU.mult)


def x_to_uT(k, xin_d, t, xt, junk, ss, lnv, rstd, ub, uT, PBk, ident, chan):
    k.dma("sp", xt[:], xin_d.rows(t), chan)
    rms_stats(k, [xt[:]], ss, [junk[:]], rstd, lnv)
    k.act(ub[:], xt[:], AF.Copy, scale=rstd[:, 0:1])
    for c in range(8):
        k.tr(PBk[:, c * 128:(c + 1) * 128], ub[:, c * 128:(c + 1) * 128], ident[:])
    k.cp(uT[:], PBk[:])


def proj_tm(k, pf, uT, wt, n=512, c0=0):
    for c in range(8):
        k.mm(pf[:, 0:n], uT[:, c * 128:(c + 1) * 128], wt[:, c, c0:c0 + n], start=(c == 0), stop=(c == 7))


def make_masks(k, es):
    m = {}
    M1 = k.sb(es, "M1", [128, 128], F32)
    M2 = k.sb(es, "M2", [128, 128], F32)
    NS = k.sb(es, "NEGS", [128, 128], F32)
    NCT = k.sb(es, "NEGCT", [128, 128], F32)
    cind = k.sb(es, "cind", [128, 2], F32)
    ones = k.sb(es, "ones_f", [128, 128], F32)
    k.memset(M1[:], 1.0)
    k.asel(M1[:], M1[:], [[1, 128]], ALU.is_ge, 0.0, 0, -1)
    k.memset(M1[0:64, 64:128], 0.0)
    k.memset(M2[:], 1.0)
    k.asel(M2[:], M2[:], [[-1, 128]], ALU.is_gt, 0.0, 0, 1)
    k.memset(M2[64:128, 0:64], 0.0)
    k.ts(NS[:], M2[:], -1.0, 30000.0, ALU.add, ALU.mult)
    k.ts(NCT[:], M1[:], -1.0, 30000.0, ALU.add, ALU.mult)
    k.memset(cind[:], 0.0)
    k.memset(cind[0:64, 0:1], 1.0)
    k.memset(cind[64:128, 1:2], 1.0)
    k.memset(ones[:], 1.0)
    return dict(M1=M1, M2=M2, NS=NS, NCT=NCT, cind=cind, ones=ones)


def head_norm_gate(k, po, ss4, ln4, r4, osb, gsw, dst):
    for h in range(4):
        k.act(osb[:, h * 128:(h + 1) * 128], po[:, h * 128:(h + 1) * 128], AF.Square, scale=float(128 ** -0.5),
              accum=ss4[:, h:h + 1])
    k.act(ln4[:], ss4[:], AF.Ln, bias=EPS)
    k.act(r4[:], ln4[:], AF.Exp, scale=-0.5)
    k.tt(V(osb.t[:, :].rearrange("p (h d) -> p h d", h=4), osb.toks),
         V(po.t[:, :].rearrange("p (h d) -> p h d", h=4), po.toks), bc(r4, (slice(None), slice(None)), [128, 4, 128]),
         ALU.mult)
    k.tt(dst, osb[:], gsw[:], ALU.mult)


def phase_a(k, es, l, NT, PF, PB, ident, ident_f, xin_d, oab_d, hglb_d, pre_mix_d, w_in_d, hg_norm_d, conv_d,
            alog_d, dtb_d, dn_norm_d, mk):
    nc = k.nc
    M1, M2, NS, NCT, cind, ones = mk["M1"], mk["M2"], mk["NS"], mk["NCT"], mk["cind"], mk["ones"]
    cnt = [0]
    win = w_in_d[l]
    Wq = load_wcols(k, es, "Wq", win, 0, 512, cnt)
    Wf = load_wcols(k, es, "Wf", win, 512, 1024, cnt)
    Wi = load_wcols(k, es, "Wi", win, 1024, 1536, cnt)
    Wg = load_wcols(k, es, "Wg", win, 1536, 2048, cnt)
    Wc = [load_wcols(k, es, "Wc%d" % i, win, 2048 + 512 * i, 2560 + 512 * i, cnt) for i in range(3)]
    Wdg = load_wcols(k, es, "Wdg", win, 3584, 4096, cnt)
    Wab = load_wcols(k, es, "Wab", win, 4096, 4104, cnt)
    gcol = load_gcol(k, es, "gcolA", pre_mix_d[l], PF[0], ident_f, "cst0")
    for wt in (Wq, Wf, Wi, Wg, Wc[0], Wc[1], Wc[2], Wdg):
        fold_gain(k, wt, gcol, 8, 512)
    fold_gain(k, Wab, gcol, 8, 8)
    cw = k.sb(es, "cw", [128, 12, 4], F32)
    cwr = k.sb(es, "cwr", [4, 1536], F32)
    k.dma("sp", cwr[:], conv_d[l], "cst1")
    for c in range(12):
        k.tr(PF[1][:, 4 * c:4 * c + 4], cwr[:, c * 128:(c + 1) * 128], ident_f[0:4, 0:4])
    k.cp(V(cw.t[:, :, :].rearrange("p c j -> p (c j)"), cw.toks), PF[1][:, 0:48])
    dtb = k.sb(es, "dtb", [128, 4], F32)
    k.dma("sp", dtb[:], dtb_d[l:l + 1, :].partition_broadcast(128), "cst2")
    negA = k.sb(es, "negA", [128, 4], F32)
    k.dma("sp", negA[:], alog_d[l:l + 1, :].partition_broadcast(128), "cst3")
    k.act(negA[:], negA[:], AF.Exp)
    k.ts(negA[:], negA[:], -1.0, None, ALU.mult)
    hgn = k.sb(es, "hgn", [128, 128], F32)
    k.dma("sp", hgn[:], hg_norm_d[l:l + 1, :].partition_broadcast(128), "cst4")
    dnn = k.sb(es, "dnn", [128, 128], F32)
    k.dma("sp", dnn[:], dn_norm_d[l:l + 1, :].partition_broadcast(128), "cst5")
    omlb = k.sb(es, "omlb", [128, 512], F32)
    if l == 0:
        k.memset(omlb[:], 1.0)
    else:
        lb0 = k.sb(es, "lb0", [128, 512], F32)
        k.dma("sp", omlb[:], hglb_d[1:2, :].partition_broadcast(128), "cst6")
        k.dma("sp", lb0[:], hglb_d[0:1, :].partition_broadcast(128), "cst7")
        k.tt(omlb[:], omlb[:], lb0[:], ALU.subtract)
        k.act(omlb[:], omlb[:], AF.Exp)
        k.ts(omlb[:], omlb[:], 1.0, None, ALU.add)
        k.recip(omlb[:], omlb[:])
    Sh = k.sb(es, "Sh", [128, 4, 128], F32, nparts=4)
    Shb = k.sb(es, "Shb", [128, 4, 128], BF16, nparts=4)
    Sd = k.sb(es, "Sd", [128, 4, 128], F32, nparts=4)
    Sdb = k.sb(es, "Sdb", [128, 4, 128], BF16, nparts=4)
    for S_ in (Sh, Shb, Sd, Sdb):
        k.memset(S_[:], 0.0)
    cvin = k.sb(es, "cvin", [128, 12, 131], F32)
    k.memset(cvin[:], 0.0)
    xt = [k.sb(es, "xtA%d" % i, [128, D], F32) for i in range(2)]
    junk = k.sb(es, "junkA", [128, D], BF16)
    ss = k.sb(es, "ssA", [128, 2], F32)
    lnv = k.sb(es, "lnvA", [128, 1], F32)
    rstd = k.sb(es, "rstdA", [128, 1], F32)
    ub = k.sb(es, "ubA", [128, D], BF16)
    uT = k.sb(es, "uTA", [128, D], BF16)
    kf = k.sb(es, "kf", [128, 512], F32)
    lf = k.sb(es, "lf", [128, 512], F32)
    eb = k.sb(es, "eb", [128, 512], F32)
    enb = k.sb(es, "enb", [128, 512], F32)
    esf = k.sb(es, "esf", [128, 512], F32)
    qe = k.sb(es, "qe", [128, 512], BF16)
    ke = k.sb(es, "ke", [128, 512], BF16)
    kdh = k.sb(es, "kdh", [128, 512], BF16)
    vh = k.sb(es, "vh", [128, 512], BF16)
    gsw = k.sb(es, "gsw", [128, 512], F32)
    qkT = k.sb(es, "qkT", [128, 1024], BF16)
    attnT = k.sb(es, "attnT", [128, 4, 128], BF16)
    dlh = k.sb(es, "dlh", [128, 8], F32)
    osb = k.sb(es, "osb", [128, 512], F32)
    ss4 = k.sb(es, "ss4", [128, 4], F32)
    ln4 = k.sb(es, "ln4", [128, 4], F32)
    r4 = k.sb(es, "r4", [128, 4], F32)
    oab = [k.sb(es, "oabt%d" % i, [128, D], BF16, nparts=2) for i in range(2)]
    cva = k.sb(es, "cva", [128, 12, 128], F32)
    tmpA = k.sb(es, "tmpA", [128, 12, 128], F32)
    qkvF = k.sb(es, "qkvF", [128, 12, 128], BF16)
    qkvT = k.sb(es, "qkvT", [128, 1536], BF16)
    ss8 = k.sb(es, "ss8", [128, 8], F32)
    ln8 = k.sb(es, "ln8", [128, 8], F32)
    r8 = k.sb(es, "r8", [128, 8], F32)
    sq4 = k.sb(es, "sq4", [128, 4], F32)
    beta = k.sb(es, "beta", [128, 4], F32)
    ab8 = k.sb(es, "ab8", [128, 8], F32)
    gt = k.sb(es, "gt", [128, 4], F32)
    egc = k.sb(es, "egc", [128, 4], F32)
    egs = k.sb(es, "egs", [128, 4], F32)
    bg = k.sb(es, "bg", [128, 4], F32)
    gmask = k.sb(es, "gmask", [128, 4, 2], F32)
    dld = k.sb(es, "dld", [128, 8], F32)
    kn = k.sb(es, "kn", [128, 512], BF16)
    kbg = k.sb(es, "kbg", [128, 512], BF16)
    kdd = k.sb(es, "kdd", [128, 512], BF16)
    vbd = k.sb(es, "vbd", [128, 512], BF16)
    qn = k.sb(es, "qn", [128, 512], BF16)
    qg = k.sb(es, "qg", [128, 512], BF16)
    kqT = k.sb(es, "kqT", [128, 8, 128], BF16)
    qgT = k.sb(es, "qgT", [128, 4, 128], BF16)
    Mg = k.sb(es, "Mg", [128, 4, 128], F32, nparts=4)
    Ls = k.sb(es, "Ls", [128, 4, 128], F32)
    LT = k.sb(es, "LT", [128, 4, 128], F32)
    Am = [k.sb(es, "Am%d" % i, [128, 4, 128], BF16) for i in range(2)]
    AmT = [k.sb(es, "AmT%d" % i, [128, 4, 128], BF16) for i in range(2)]
    TT = [k.sb(es, "TT%d" % i, [128, 4, 128], BF16) for i in range(2)]
    nwT = k.sb(es, "nwT", [128, 4, 128], BF16)
    aqk = k.sb(es, "aqk", [128, 4, 128], BF16)
    vn = k.sb(es, "vn", [128, 4, 128], BF16, nparts=4)
    gsd = k.sb(es, "gsd", [128, 512], F32)
    osd = k.sb(es, "osd", [128, 512], F32)
    A = slice(None)

    def v4(b):
        return V(b.t[:, :].rearrange("p (h d) -> p h d", h=4), b.toks)

    for t in range(NT):
        x_ = xt[t % 2]
        ob = oab[t % 2]
        x_to_uT(k, xin_d, t, x_, junk, ss, lnv, rstd, ub, uT, PB[0], ident, "xin%d" % (t % 2))
        proj_tm(k, PF[0], uT, Wf)
        k.act(kf[:], PF[0][:], AF.Sigmoid, scale=-1.0)
        k.tt(kf[:], kf[:], omlb[:], ALU.mult)
        k.act(lf[:], kf[:], AF.Ln, scale=-1.0, bias=1.0)
        k.mm(PF[0][:], M1[:], lf[:])
        k.mm(PF[1][:], M2[:], lf[:])
        for h in range(4):
            k.mm(PF[2][:, 2 * h:2 * h + 2], lf[:, h * 128:(h + 1) * 128], cind[:])
        k.act(eb[:], PF[0][:], AF.Exp)
        k.act(enb[:], PF[0][:], AF.Exp, scale=-1.0)
        k.act(esf[:], PF[1][:], AF.Exp)
        k.act(dlh[:], PF[2][:, 0:8], AF.Exp)
        proj_tm(k, PF[3], uT, Wq)
        k.tt(qe[:], PF[3][:], eb[:], ALU.mult)
        k.tt(ke[:], kf[:], enb[:], ALU.mult)
        k.tt(kdh[:], kf[:], esf[:], ALU.mult)
        proj_tm(k, PF[4], uT, Wi)
        k.cp(vh[:], PF[4][:], eng="act")
        proj_tm(k, PF[5], uT, Wg)
        k.act(gsw[:], PF[5][:], AF.Silu)
        k.tt(v4(gsw), v4(gsw), bc_mid(hgn, (A, A), [128, 4, 128]), ALU.mult)
        for h in range(4):
            k.tr(PB[1][:, h * 128:(h + 1) * 128], qe[:, h * 128:(h + 1) * 128], ident[:])
            k.tr(PB[1][:, 512 + h * 128:512 + (h + 1) * 128], ke[:, h * 128:(h + 1) * 128], ident[:])
        k.cp(qkT[:], PB[1][:])
        for h in range(4):
            k.mm(PF[0][:, h * 128:(h + 1) * 128], qkT[:, 512 + h * 128:512 + (h + 1) * 128],
                 qkT[:, h * 128:(h + 1) * 128])
        k.tt(attnT[:], v4(PF[0]), bc_mid(M1, (A, A), [128, 4, 128]), ALU.mult)
        po = PF[1]
        for h in range(4):
            hs = slice(h * 128, (h + 1) * 128)
            k.mm(po[:, hs], attnT[:, h, :], vh[:, hs], start=True, stop=False)
            for c in range(2):
                cs = slice(64 * c, 64 * c + 64)
                k.mm(po[cs, hs], qkT[:, h * 128 + 64 * c:h * 128 + 64 * c + 64], Shb.p(h, (A, h, A)),
                     start=False, stop=True)
                k.mm(PF[2 + (h % 2)][:, 0:128], kdh[cs, hs], vh[cs, hs])
                k.stt(Sh.p(h, (A, h, A)), Sh.p(h, (A, h, A)), dlh[:, 2 * h + c:2 * h + c + 1],
                      PF[2 + (h % 2)][:, 0:128], ALU.mult, ALU.add)
                k.cp(Shb.p(h, (A, h, A)), Sh.p(h, (A, h, A)), eng="act")
        head_norm_gate(k, po, ss4, ln4, r4, osb, gsw, ob.p(0, (A, slice(0, 512))))

        if CUT == "hg":
            k.dma("sp", oab_d.rows(t), ob[:], "oabw%d" % (t % 2))
            continue
        k.cp(cvin[:, :, 0:3], cvin[:, :, 128:131])
        for i in range(3):
            pf = PF[2 + i]
            for cc in range(4):
                for c in range(8):
                    k.mm(pf[:, cc * 128:(cc + 1) * 128], Wc[i][:, c, cc * 128:(cc + 1) * 128],
                         uT[:, c * 128:(c + 1) * 128], start=(c == 0), stop=(c == 7))
            k.cp(cvin[:, 4 * i:4 * i + 4, 3:131], v4(pf), eng="act")
        for c in range(12):
            k.act(cva[:, c, :], cvin[:, c, 0:128], AF.Copy, scale=cw[:, c, 0:1])
            k.act(tmpA[:, c, :], cvin[:, c, 2:130], AF.Copy, scale=cw[:, c, 2:3])
        for c in range(12):
            k.stt(cva[:, c, :], cvin[:, c, 1:129], cw[:, c, 1:2], cva[:, c, :], ALU.mult, ALU.add)
            k.stt(tmpA[:, c, :], cvin[:, c, 3:131], cw[:, c, 3:4], tmpA[:, c, :], ALU.mult, ALU.add)
        k.tt(cva[:], cva[:], tmpA[:], ALU.add)
        k.act(qkvF[:], cva[:], AF.Silu)
        for c in range(8):
            k.tr(PB[0][:, c * 128:(c + 1) * 128], qkvF[:, c, :], ident[:])
        k.cp(qkvT[:, 0:1024], PB[0][:])
        for c in range(8, 12):
            k.tr(PB[1][:, (c - 8) * 128:(c - 7) * 128], qkvF[:, c, :], ident[:])
        k.cp(qkvT[:, 1024:1536], PB[1][:, 0:512], eng="act")
        if CUT == "conv":
            k.cp(ob.p(1, (A, slice(512, 1024))), qkvT[:, 0:512])
            k.dma("sp", oab_d.rows(t), ob[:], "oabw%d" % (t % 2))
            continue
        for j in range(8):
            k.act(osd[:, (j % 4) * 128:(j % 4 + 1) * 128], qkvT[:, j * 128:(j + 1) * 128], AF.Square,
                  accum=ss8[:, j:j + 1])
        k.act(ln8[:], ss8[:], AF.Ln, bias=EPS)
        k.act(r8[:], ln8[:], AF.Exp, scale=-0.5)
        if CUT == "n1":
            k.dma("sp", oab_d.rows(t), ob[:], "oabw%d" % (t % 2))
            continue
        pab = PF[5]
        for c in range(8):
            k.mm(pab[:, 0:8], uT[:, c * 128:(c + 1) * 128], Wab[:, c, :], start=(c == 0), stop=(c == 7))
        k.cp(ab8[:], pab[:, 0:8], eng="act")
        k.act(beta[:], ab8[:, 4:8], AF.Sigmoid)
        k.tt(gt[:], ab8[:, 0:4], dtb[:], ALU.add)
        k.act(gt[:], gt[:], AF.Exp)
        k.act(gt[:], gt[:], AF.Ln, bias=1.0)
        k.tt(gt[:], gt[:], negA[:], ALU.mult)
        if CUT == "n2":
            k.dma("sp", oab_d.rows(t), ob[:], "oabw%d" % (t % 2))
            continue
        k.mm(pab[:, 16:20], M1[:], gt[:])
        k.mm(pab[:, 24:28], M2[:], gt[:])
        k.tt(gmask[:], bc(gt, (A, A), [128, 4, 2]), bc_mid(cind, (A, A), [128, 4, 2]), ALU.mult)
        k.mm(pab[:, 32:40], ones[:], V(gmask.t[:, :, :].rearrange("p h c -> p (h c)"), gmask.toks))
        k.act(egc[:], pab[:, 16:20], AF.Exp)
        k.act(egs[:], pab[:, 24:28], AF.Exp)
        k.act(dld[:], pab[:, 32:40], AF.Exp)
        if CUT == "n3":
            k.dma("sp", oab_d.rows(t), ob[:], "oabw%d" % (t % 2))
            continue
        k.ts(sq4[:], r8[:, 0:4], float(128 ** -0.5), None, ALU.mult)
        k.tt(bg[:], beta[:], egc[:], ALU.mult)

        def hv(b, lo):
            return V(b.t[:, lo:lo + 512].rearrange("p (h d) -> p h d", h=4), b.toks)

        sh3 = [128, 4, 128]
        for h in range(4):
            hs = slice(h * 128, (h + 1) * 128)
            k.act(kn[:, hs], qkvT[:, 512 + h * 128:512 + (h + 1) * 128], AF.Copy, scale=r8[:, 4 + h:5 + h])
            k.act(qn[:, hs], qkvT[:, h * 128:(h + 1) * 128], AF.Copy, scale=sq4[:, h:h + 1])
            k.act(vbd[:, hs], qkvT[:, 1024 + h * 128:1024 + (h + 1) * 128], AF.Copy, scale=beta[:, h:h + 1])
        for h in range(4):
            hs = slice(h * 128, (h + 1) * 128)
            k.ts(kbg[:, hs], kn[:, hs], bg[:, h:h + 1], None, ALU.mult)
            k.ts(kdd[:, hs], kn[:, hs], egs[:, h:h + 1], None, ALU.mult)
            k.ts(qg[:, hs], qn[:, hs], egc[:, h:h + 1], None, ALU.mult)
        if CUT == "n4":
            k.dma("sp", oab_d.rows(t), ob[:], "oabw%d" % (t % 2))
            continue
        for h in range(4):
            hs = slice(h * 128, (h + 1) * 128)
            k.tr(PB[0][:, hs], kn[:, hs], ident[:])
            k.tr(PB[0][:, 512 + h * 128:512 + (h + 1) * 128], qn[:, hs], ident[:])
            k.tr(PB[1][:, hs], qg[:, hs], ident[:])
        k.cp(V(kqT.t[:, :, :].rearrange("p h d -> p (h d)"), kqT.toks), PB[0][:])
        k.cp(V(qgT.t[:, :, :].rearrange("p h d -> p (h d)"), qgT.toks), PB[1][:, 0:512], eng="act")
        if CUT == "prep":
            k.cp(ob.p(1, (A, slice(512, 1024))), kbg[:])
            k.dma("sp", oab_d.rows(t), ob[:], "oabw%d" % (t % 2))
            continue
        for h in range(4):
            hs = slice(h * 128, (h + 1) * 128)
            k.ts(Mg.p(h, (A, h, A)), M1[:], gt[:, h:h + 1], None, ALU.mult)
            k.mm(PF[2][:, hs], Mg.p(h, (A, h, A)), M2[:], start=True, stop=False)
            k.mm(PF[2][:, hs], ident_f[:], NS[:], start=False, stop=True)
            k.mm(PF[3][:, hs], M2[:], Mg.p(h, (A, h, A)), start=True, stop=False)
            k.mm(PF[3][:, hs], ident_f[:], NCT[:], start=False, stop=True)
            k.mm(PF[0][:, hs], kqT[:, h, :], kqT[:, h, :])
            k.mm(PF[4][:, hs], kqT[:, h, :], kqT[:, 4 + h, :])
        k.act(Ls[:], v4(PF[2]), AF.Exp)
        k.act(LT[:], v4(PF[3]), AF.Exp)
        for h in range(4):
            hs = slice(h * 128, (h + 1) * 128)
            k.stt(Am[0][:, h, :], PF[0][:, hs], beta[:, h:h + 1], Ls[:, h, :], ALU.mult, ALU.mult)
        k.tt(aqk[:], v4(PF[4]), LT[:], ALU.mult)
        for h in range(4):
            k.tr(PB[0][:, h * 128:(h + 1) * 128], Am[0][:, h, :], ident[:])
        k.cp(V(AmT[0].t[:, :, :].rearrange("p h d -> p (h d)"), AmT[0].toks), PB[0][:, 0:512], eng="act")
        k.tt(TT[0][:], bc_mid(ident_f, (A, A), sh3), AmT[0][:], ALU.subtract)
        cur = 0
        for r in range(1, 6):
            nxt = 1 - cur
            pP, pPT, pT = PF[2], PF[3], PF[4]
            for h in range(4):
                hs = slice(h * 128, (h + 1) * 128)
                k.mm(pP[:, hs], AmT[cur][:, h, :], Am[cur][:, h, :])
                if r < 5:
                    k.mm(pPT[:, hs], Am[cur][:, h, :], AmT[cur][:, h, :])
            k.cp(Am[nxt][:], v4(pP))
            if r < 5:
                k.cp(AmT[nxt][:], v4(pPT), eng="act")
            for h in range(4):
                hs = slice(h * 128, (h + 1) * 128)
                k.mm(pT[:, hs], Am[nxt][:, h, :], TT[cur][:, h, :], start=True, stop=False)
                k.mm(pT[:, hs], ident[:], TT[cur][:, h, :], start=False, stop=True)
            k.cp(TT[nxt][:], v4(pT))
            cur = nxt
        TTf = TT[cur]
        if CUT == "inv":
            k.cp(ob.p(1, (A, slice(512, 1024))), V(TTf.t[:, :, :].rearrange("p h d -> p (h d)"), TTf.toks))
            k.dma("sp", oab_d.rows(t), ob[:], "oabw%d" % (t % 2))
            continue
        for h in range(4):
            hs = slice(h * 128, (h + 1) * 128)
            k.mm(PF[2][:, hs], kbg[:, hs], TTf[:, h, :])
        k.act(nwT[:], v4(PF[2]), AF.Copy, scale=-1.0)
        pod = PF[4]
        for h in range(4):
            hs = slice(h * 128, (h + 1) * 128)
            pvn = PF[3] if h % 2 == 0 else PF[2]
            k.mm(pvn[:, hs], TTf[:, h, :], vbd[:, hs], start=True, stop=False)
            for c in range(2):
                cs = slice(64 * c, 64 * c + 64)
                k.mm(pvn[cs, hs], nwT[:, h, cs], Sdb.p(h, (A, h, A)), start=False, stop=True)
                k.mm(pod[cs, hs], qgT[:, h, cs], Sdb.p(h, (A, h, A)), start=True, stop=False)
                k.cp(vn.p(h, (cs, h, A)), pvn[cs, hs], eng="act")
                k.mm(PF[(h % 2)][:, 0:128], kdd[cs, hs], vn.p(h, (cs, h, A)))
                k.stt(Sd.p(h, (A, h, A)), Sd.p(h, (A, h, A)), dld[:, 2 * h + c:2 * h + c + 1],
                      PF[(h % 2)][:, 0:128], ALU.mult, ALU.add)
                k.cp(Sdb.p(h, (A, h, A)), Sd.p(h, (A, h, A)), eng="act")
            k.mm(pod[:, hs], aqk[:, h, :], vn.p(h, (A, h, A)), start=False, stop=True)
        proj_tm(k, PF[5], uT, Wdg)
        k.act(gsd[:], PF[5][:], AF.Silu)
        k.tt(v4(gsd), v4(gsd), bc_mid(dnn, (A, A), [128, 4, 128]), ALU.mult)
        head_norm_gate(k, pod, ss4, ln4, r4, osd, gsd, ob.p(1, (A, slice(512, 1024))))
        k.dma("sp", oab_d.rows(t), ob[:], "oabw%d" % (t % 2))


def phase_b(k, es, l, NT, PF, PB, ident, ident_f, xin_d, oab_d, hout_d, pre_mix_d, w_in_d, w_o_hg_d, w_o_dn_d, w_out_d,
            post_mix_d):
    cnt = [0]
    win = w_in_d[l]
    Wm = [load_wcols(k, es, "Wm%d" % i, win, 4104 + 512 * i, 4616 + 512 * i, cnt) for i in range(4)]
    Whg = load_wcols(k, es, "Whg", w_o_hg_d[l], 0, D, cnt, kch=4)
    Wdn = load_wcols(k, es, "Wdn", w_o_dn_d[l], 0, D, cnt, kch=4)
    Wout = load_wcols(k, es, "Wout", w_out_d[l], 0, D, cnt)
    gcol = load_gcol(k, es, "gcolB", pre_mix_d[l], PF[0], ident_f, "cst0")
    gbc = k.sb(es, "gbcB", [128, D], F32)
    k.dma("sp", gbc[:], post_mix_d[l:l + 1, :].partition_broadcast(128), "cst1")
    for wt in Wm:
        fold_gain(k, wt, gcol, 8, 512)
    xt = [k.sb(es, "xtB%d" % i, [128, D], F32) for i in range(2)]
    ot = [k.sb(es, "otB%d" % i, [128, D], BF16) for i in range(2)]
    ht = [k.sb(es, "htB%d" % i, [128, D], F32) for i in range(2)]
    junk = k.sb(es, "junkB", [128, D], BF16, nparts=2)
    ss = k.sb(es, "ssB", [128, 2], F32)
    lnv = k.sb(es, "lnvB", [128, 1], F32)
    rstd = k.sb(es, "rstdB", [128, 1], F32)
    ss2 = k.sb(es, "ss2B", [128, 2], F32)
    lnv2 = k.sb(es, "lnv2B", [128, 1], F32)
    rstd2 = k.sb(es, "rstd2B", [128, 1], F32)
    ub = k.sb(es, "ubB", [128, D], BF16)
    uT = k.sb(es, "uTB", [128, D], BF16)
    oT = k.sb(es, "oTB", [128, D], BF16)
    sgm = k.sb(es, "sgm", [128, 4, 512], F32, nparts=4)
    t1 = k.sb(es, "t1B", [128, 512], F32)
    mg = k.sb(es, "mgB", [128, D], BF16, nparts=2)
    mgT = k.sb(es, "mgTB", [128, D], BF16)
    A = slice(None)
    for t in range(NT):
        x_ = xt[t % 2]
        o_ = ot[t % 2]
        h_ = ht[t % 2]
        k.dma("sp", o_[:], oab_d.rows(t), "oabr%d" % (t % 2))
        x_to_uT(k, xin_d, t, x_, junk, ss, lnv, rstd, ub, uT, PB[0], ident, "xin%d" % (t % 2))
        for i in range(4):
            proj_tm(k, PF[i], uT, Wm[i])
            k.act(sgm.p(i, (A, i, A)), PF[i][:], AF.Sigmoid)
        for c in range(8):
            k.tr(PB[1][:, c * 128:(c + 1) * 128], o_[:, c * 128:(c + 1) * 128], ident[:])
        k.cp(oT[:], PB[1][:])
        for hh in range(2):
            cs = slice(hh * 512, (hh + 1) * 512)
            pa, pb_ = PF[2 * hh], PF[2 * hh + 1]
            for c in range(4):
                k.mm(pa[:], oT[:, c * 128:(c + 1) * 128], Whg[:, c, cs], start=(c == 0), stop=(c == 3))
            for c in range(4):
                k.mm(pb_[:], oT[:, 512 + c * 128:512 + (c + 1) * 128], Wdn[:, c, cs], start=(c == 0), stop=(c == 3))
            k.tt(t1[:], pa[:], sgm.p(hh, (A, hh, A)), ALU.mult)
            k.tt(sgm.p(2 + hh, (A, 2 + hh, A)), pb_[:], sgm.p(2 + hh, (A, 2 + hh, A)), ALU.mult)
            k.tt(mg.p(hh, (A, cs)), t1[:], sgm.p(2 + hh, (A, 2 + hh, A)), ALU.add)
        for c in range(8):
            k.tr(PB[0][:, c * 128:(c + 1) * 128], mg.p(c // 4, (A, slice(c * 128, (c + 1) * 128))), ident[:])
        k.cp(mgT[:], PB[0][:], eng="act")
        for hh in range(2):
            for c in range(8):
                k.mm(PF[4 + hh][:], mgT[:, c * 128:(c + 1) * 128], Wout[:, c, hh * 512:(hh + 1) * 512],
                     start=(c == 0), stop=(c == 7))
        rms_stats(k, [PF[4][:], PF[5][:]], ss2,
                  [junk.p(0, (A, slice(0, 512))), junk.p(1, (A, slice(512, 1024)))], rstd2, lnv2)
        for hh in range(2):
            sl = slice(hh * 512, (hh + 1) * 512)
            k.stt(h_[:, sl], PF[4 + hh][:], rstd2[:, 0:1], gbc[:, sl], ALU.mult, ALU.mult)
        k.tt(h_[:], h_[:], x_[:], ALU.add, eng="pool")
        k.dma("sp", hout_d.rows(t), h_[:], "hout%d" % (t % 2))


_CACHE = {}

WNAMES = ["hg_lower_bounds", "pre_mix_w", "w_in", "hg_norm_w", "conv_w", "dn_a_log", "dn_dt_bias", "dn_norm_w",
          "w_o_hg", "w_o_dn", "w_out", "post_mix_w", "pre_ffn_w", "w_gate_up", "w_down", "post_ffn_w"]


def kernel(**inputs):
    x = np.ascontiguousarray(inputs["x"], dtype=np.float32)
    B, T, _ = x.shape
    if "nc" not in _CACHE:
        _CACHE["nc"] = build(T=T, L=2)
    nc = _CACHE["nc"]
    shared = {n: np.ascontiguousarray(inputs[n], dtype=np.float32) for n in WNAMES}
    in_maps = []
    for b in range(B):
        m = dict(shared)
        m["x"] = x[b]
        in_maps.append(m)
    res = run_bass_kernel_spmd(nc, in_maps, core_ids=list(range(B)))
    return np.stack([r["out"] for r in res.results], axis=0)
```

```python
from contextlib import ExitStack

import numpy as np
import concourse.bass as bass
import concourse.mybir as mybir
from concourse.bass_utils import run_bass_kernel_spmd

F32 = mybir.dt.float32
BF16 = mybir.dt.bfloat16
AF = mybir.ActivationFunctionType
ALU = mybir.AluOpType

D = 1024
DFF = 2816
INW = 6152
EPS = 1e-6
ENGS = ("pe", "act", "dve", "pool", "sp")
CUT = None


class Tok:
    __slots__ = ("last_w", "readers")

    def __init__(self):
        self.last_w = None
        self.readers = []


class Op:
    __slots__ = ("eng", "fn", "idx", "deps", "signal", "semval", "clock", "dma_chan", "waits", "barrier",
                 "cost", "start", "finish", "users", "nun", "ready", "done", "chan_snap")

    def __init__(self, eng, fn):
        self.eng = eng
        self.fn = fn
        self.idx = -1
        self.deps = []
        self.signal = False
        self.semval = 0
        self.clock = None
        self.dma_chan = None
        self.waits = []
        self.barrier = False
        self.cost = 100.0
        self.start = 0.0
        self.finish = 0.0
        self.users = None
        self.nun = 0
        self.ready = 0.0
        self.done = False
        self.chan_snap = None


class Prog:
    def __init__(self):
        self.streams = {e: [] for e in ENGS}
        self.chan_count = {}
        self.chan_last = {}
        self.all_ops = []

    def op(self, eng, fn, reads=(), writes=(), dma_chan=None, cost=100.0):
        o = Op(eng, fn)
        o.dma_chan = dma_chan
        o.cost = cost
        deps = []
        for r in reads:
            if r.last_w is not None:
                deps.append((r.last_w, "raw"))
        for w in writes:
            if w.last_w is not None:
                deps.append((w.last_w, "waw"))
            for rd in w.readers:
                deps.append((rd, "war"))
        o.deps = deps
        for r in reads:
            r.readers.append(o)
        for w in writes:
            w.last_w = o
            w.readers = []
        o.idx = len(self.streams[eng])
        self.streams[eng].append(o)
        self.all_ops.append(o)
        if dma_chan is not None:
            c = self.chan_count.get(dma_chan, 0) + 1
            self.chan_count[dma_chan] = c
            o.semval = 16 * c
            self.chan_last[dma_chan] = o
        return o

    def barrier(self):
        m = Op("sp", None)
        m.barrier = True
        m.chan_snap = list(self.chan_last.values())
        self.all_ops.append(m)

    def schedule(self, window=64, lat=120.0):
        segs = [[]]
        marks = []
        for o in self.all_ops:
            if o.barrier:
                marks.append(o)
                segs.append([])
            else:
                segs[-1].append(o)
        new_streams = {e: [] for e in ENGS}
        new_all = []
        free = {e: 0.0 for e in ENGS}
        reorder = ("pe", "act", "dve")
        for si, seg in enumerate(segs):
            st = {e: [] for e in ENGS}
            inseg = set()
            for o in seg:
                st[o.eng].append(o)
                o.users = []
                o.nun = 0
                o.ready = 0.0
                o.done = False
                inseg.add(id(o))
            for o in seg:
                seen = set()
                for d, _ in o.deps:
                    if d is o or id(d) in seen:
                        continue
                    seen.add(id(d))
                    if id(d) in inseg:
                        d.users.append(o)
                        o.nun += 1
            head = {e: 0 for e in ENGS}
            nleft = len(seg)
            order = []
            while nleft:
                best = None
                for e in ENGS:
                    lst = st[e]
                    h = head[e]
                    while h < len(lst) and lst[h].done:
                        h += 1
                    head[e] = h
                    if h >= len(lst):
                        continue
                    if e in reorder:
                        cand = None
                        cstart = None
                        fe = free[e]
                        for j in range(h, min(len(lst), h + window)):
                            o = lst[j]
                            if o.done or o.nun:
                                continue
                            s0 = o.ready if o.ready > fe else fe
                            if cand is None or s0 < cstart - 1e-9:
                                cand, cstart = o, s0
                                if s0 <= fe:
                                    break
                    else:
                        o = lst[h]
                        if o.nun:
                            continue
                        cand = o
                        cstart = o.ready if o.ready > free[e] else free[e]
                    if cand is not None and (best is None or cstart < best[0]):
                        best = (cstart, cand)
                assert best is not None, "scheduler deadlock"
                t0, o = best
                o.start = t0
                o.done = True
                nleft -= 1
                if o.dma_chan is not None:
                    free[o.eng] = t0 + 60.0
                    o.finish = t0 + o.cost
                else:
                    o.finish = t0 + o.cost
                    free[o.eng] = o.finish
                for u in o.users:
                    u.nun -= 1
                    r = o.finish + (lat if u.eng != o.eng or o.dma_chan is not None else 40.0)
                    if r > u.ready:
                        u.ready = r
                order.append(o)
            order.sort(key=lambda q: q.start)
            for o in order:
                o.idx = len(new_streams[o.eng])
                new_streams[o.eng].append(o)
                new_all.append(o)
            if si < len(marks):
                tmax = max(free.values())
                tmax = max([tmax] + [o.finish for o in order]) if order else tmax
                for e in ENGS:
                    free[e] = tmax
                lasts = []
                for e in ENGS:
                    for o in reversed(new_streams[e]):
                        if o.dma_chan is None and not o.barrier:
                            lasts.append(o)
                            break
                for e in ENGS:
                    b = Op(e, None)
                    b.barrier = True
                    b.deps = [(d, "raw") for d in lasts if d.eng != e] + [(d, "raw") for d in marks[si].chan_snap]
                    b.idx = len(new_streams[e])
                    new_streams[e].append(b)
                    new_all.append(b)
        self.streams = new_streams
        self.all_ops = new_all
        self.est_ns = max(free.values())

    def resolve(self):
        eng_clock = {e: ({}, {}) for e in ENGS}
        for o in self.all_ops:
            cc, dc = eng_clock[o.eng]
            waits = []
            for d, kind in o.deps:
                if d is o:
                    continue
                if d.dma_chan is not None:
                    if dc.get(d.dma_chan, 0) >= d.semval:
                        continue
                    waits.append(d)
                    dc[d.dma_chan] = d.semval
                else:
                    if d.eng == o.eng and kind != "raw" and o.eng == "pe":
                        continue
                    if cc.get(d.eng, -1) >= d.idx:
                        continue
                    waits.append(d)
                    d.signal = True
                    cc[d.eng] = d.idx
                dcc, ddc = d.clock
                for k, v in dcc.items():
                    if cc.get(k, -1) < v:
                        cc[k] = v
                for k, v in ddc.items():
                    if dc.get(k, 0) < v:
                        dc[k] = v
            best = {}
            for d in waits:
                if d.dma_chan is not None:
                    key = ("d", d.dma_chan)
                    val = d.semval
                else:
                    key = ("c", d.eng)
                    val = d.idx
                cur = best.get(key)
                if cur is None or val > cur[0]:
                    best[key] = (val, d)
            o.waits = [v[1] for v in best.values()]
            o.clock = (dict(cc), dict(dc))
            if o.dma_chan is None:
                o.clock[0][o.eng] = max(o.clock[0].get(o.eng, -1), o.idx - 1)
        for e in ENGS:
            n = 0
            for o in self.streams[e]:
                if o.dma_chan is None and o.signal:
                    n += 1
                    o.semval = n

    def emit(self, nc, final_waits=()):
        self.schedule()
        self.resolve()
        with ExitStack() as es:
            sems = {}
            for e in ENGS:
                sems[("c", e)] = es.enter_context(nc.semaphore("s_" + e))
            for ch in self.chan_count:
                sems[("d", ch)] = es.enter_context(nc.semaphore("d_" + str(ch)))
            block = es.enter_context(nc.Block())

            def run_stream(ename):
                def body(eng):
                    for o in self.streams[ename]:
                        for d in o.waits:
                            if d.dma_chan is not None:
                                eng.wait_ge(sems[("d", d.dma_chan)], d.semval)
                            else:
                                eng.wait_ge(sems[("c", d.eng)], d.semval)
                        if o.fn is None:
                            continue
                        ins = o.fn(eng)
                        if o.dma_chan is not None:
                            ins.then_inc(sems[("d", o.dma_chan)], 16)
                        elif o.signal:
                            ins.then_inc(sems[("c", ename)], 1)
                    if ename == "sp":
                        done = {}
                        for d in final_waits:
                            done[d.dma_chan] = max(done.get(d.dma_chan, 0), d.semval)
                        for ch, v in done.items():
                            eng.wait_ge(sems[("d", ch)], v)

                return body

            block.tensor(run_stream("pe"))
            block.scalar(run_stream("act"))
            block.vector(run_stream("dve"))
            block.gpsimd(run_stream("pool"))
            block.sync(run_stream("sp"))


class V:
    __slots__ = ("ap", "toks")

    def __init__(self, ap, toks):
        self.ap = ap
        self.toks = tuple(toks)


class Buf:
    def __init__(self, t, nparts=1):
        self.t = t
        self.toks = [Tok() for _ in range(nparts)]

    def __getitem__(self, idx):
        return V(self.t[idx], self.toks)

    def p(self, i, idx):
        if isinstance(i, int):
            return V(self.t[idx], (self.toks[i],))
        return V(self.t[idx], [self.toks[j] for j in i])


def _fsz(v):
    ap = v.ap if isinstance(v, V) else v
    n = 1
    for d in ap.shape[1:]:
        n *= int(d)
    return n


def _is_f32(v):
    return v.ap.dtype == F32


class K:
    def __init__(self, nc):
        self.nc = nc
        self.P = Prog()

    def sb(self, es, name, shape, dtype, nparts=1):
        self._uid = getattr(self, "_uid", 0) + 1
        return Buf(es.enter_context(self.nc.sbuf_tensor("%s_%d" % (name, self._uid), list(shape), dtype)), nparts)

    def ps(self, es, name, shape, dtype):
        return Buf(es.enter_context(self.nc.psum_tensor(name, list(shape), dtype)), 1)

    @staticmethod
    def _rw(*vs):
        t = []
        for v in vs:
            if isinstance(v, V):
                t.extend(v.toks)
        return t

    @staticmethod
    def _a(v):
        return v.ap if isinstance(v, V) else v

    def mm(self, out, lhsT, rhs, start=True, stop=True):
        n = _fsz(rhs)
        c = 30.0 + n * (1.7 if _is_f32(rhs) else 0.45)
        self.P.op("pe", lambda e: e.matmul(out.ap, lhsT=lhsT.ap, rhs=rhs.ap, start=start, stop=stop),
                  reads=self._rw(lhsT, rhs), writes=out.toks, cost=c)

    def tr(self, out, in_, ident):
        self.P.op("pe", lambda e: e.transpose(out=out.ap, in_=in_.ap, identity=ident.ap),
                  reads=self._rw(in_, ident), writes=out.toks, cost=100.0)

    def act(self, out, in_, func, bias=None, scale=None, accum=None, eng="act"):
        kw = {}
        if bias is not None:
            kw["bias"] = self._a(bias)
        if scale is not None:
            kw["scale"] = self._a(scale)
        if accum is not None:
            kw["accum_out"] = accum.ap
        self.P.op(eng, lambda e: e.activation(out=out.ap, in_=in_.ap, func=func, **kw),
                  reads=self._rw(in_, bias, scale), writes=self._rw(out, accum), cost=220.0 + _fsz(in_) * 1.05)

    def tt(self, out, in0, in1, op, eng="dve"):
        self.P.op(eng, lambda e: e.tensor_tensor(out=out.ap, in0=in0.ap, in1=in1.ap, op=op),
                  reads=self._rw(in0, in1), writes=out.toks,
                  cost=(100.0 + _fsz(out) * 1.05) * (2.0 if eng == "pool" else 1.0))

    def ts(self, out, in0, s1, s2, op0, op1=None, eng="dve", accum=None):
        kw = {}
        if op1 is not None:
            kw["op1"] = op1
        if accum is not None:
            kw["accum_out"] = accum.ap
        self.P.op(eng, lambda e: e.tensor_scalar(out=out.ap, in0=in0.ap, scalar1=self._a(s1), scalar2=self._a(s2),
                                                 op0=op0, **kw),
                  reads=self._rw(in0, s1, s2), writes=self._rw(out, accum), cost=100.0 + _fsz(out) * 0.8)

    def stt(self, out, in0, scalar, in1, op0, op1, eng="dve"):
        self.P.op(eng, lambda e: e.scalar_tensor_tensor(out=out.ap, in0=in0.ap, scalar=self._a(scalar), in1=in1.ap,
                                                        op0=op0, op1=op1),
                  reads=self._rw(in0, scalar, in1), writes=out.toks, cost=100.0 + _fsz(out) * 1.05)

    def cp(self, out, in_, eng="dve"):
        if eng == "act":
            self.P.op("act", lambda e: e.copy(out=out.ap, in_=in_.ap), reads=in_.toks, writes=out.toks,
                      cost=220.0 + _fsz(out) * 1.05)
        else:
            self.P.op(eng, lambda e: e.tensor_copy(out=out.ap, in_=in_.ap), reads=in_.toks, writes=out.toks,
                      cost=100.0 + _fsz(out) * 0.8)

    def recip(self, out, in_):
        self.P.op("dve", lambda e: e.reciprocal(out=out.ap, in_=in_.ap), reads=in_.toks, writes=out.toks)

    def memset(self, out, val, eng="pool"):
        self.P.op(eng, lambda e: e.memset(out.ap, val), writes=out.toks)

    def asel(self, out, in_, pattern, cmp, fill, base, cm):
        self.P.op("pool", lambda e: e.affine_select(out=out.ap, in_=in_.ap, pattern=pattern, compare_op=cmp,
                                                    fill=fill, base=base, channel_multiplier=cm),
                  reads=in_.toks, writes=out.toks)

    def dma(self, q, out, in_, chan, noncontig=False):
        kw = {"allow_slow_non_contiguous": True} if noncontig else {}
        ap = out.ap if isinstance(out, V) else out
        nbytes = 128.0 * 4
        try:
            nbytes = float(ap.nbytes())
        except Exception:
            pass
        return self.P.op(q, lambda e: e.dma_start(out=self._a(out), in_=self._a(in_), **kw),
                         reads=self._rw(in_), writes=self._rw(out), dma_chan=chan, cost=2500.0 + nbytes / 150.0)


class DramT:
    def __init__(self, ap, ntiles):
        self.ap = ap
        self.toks = [Tok() for _ in range(max(1, ntiles))]

    def rows(self, t, n=128):
        return V(self.ap[t * 128:t * 128 + n], (self.toks[t],))


def build(T=8192, L=2, phases=("A", "B", "C"), dbg=()):
    NT = T // 128
    nc = bass.Bass("TRN2", target_bir_lowering=False)
    k = K(nc)
    P = k.P

    def din(name, shape):
        return nc.dram_tensor(name, list(shape), F32, kind="ExternalInput").ap()

    x_d = DramT(din("x", [T, D]), NT)
    hglb_d = din("hg_lower_bounds", [L, 512])
    pre_mix_d = din("pre_mix_w", [L, D])
    w_in_d = din("w_in", [L, D, INW])
    hg_norm_d = din("hg_norm_w", [L, 128])
    conv_d = din("conv_w", [L, 4, 1536])
    alog_d = din("dn_a_log", [L, 4])
    dtb_d = din("dn_dt_bias", [L, 4])
    dn_norm_d = din("dn_norm_w", [L, 128])
    w_o_hg_d = din("w_o_hg", [L, 512, D])
    w_o_dn_d = din("w_o_dn", [L, 512, D])
    w_out_d = din("w_out", [L, D, D])
    post_mix_d = din("post_mix_w", [L, D])
    pre_ffn_d = din("pre_ffn_w", [L, D])
    w_gu_d = din("w_gate_up", [L, D, 2 * DFF])
    w_dn_d = din("w_down", [L, DFF, D])
    post_ffn_d = din("post_ffn_w", [L, D])
    out_d = DramT(nc.dram_tensor("out", [T, D], F32, kind="ExternalOutput").ap(), NT)
    skind = "ExternalOutput" if dbg else "Internal"
    hbuf_d = DramT(nc.dram_tensor("hbuf", [T, D], F32, kind=skind).ap(), NT)
    x1_d = DramT(nc.dram_tensor("x1buf", [T, D], F32, kind=skind).ap(), NT)
    oab_d = DramT(nc.dram_tensor("oab", [T, D], BF16, kind=skind).ap(), NT)

    final_ops = []
    with ExitStack() as pes:
        PF = [k.ps(pes, "pf%d" % i, [128, 512], F32) for i in range(6)]
        PB = [k.ps(pes, "pb%d" % i, [128, 1024], BF16) for i in range(2)]
        ident_f = k.sb(pes, "ident_f", [128, 128], F32)
        ident = k.sb(pes, "ident", [128, 128], BF16)
        k.memset(ident_f[:], 1.0)
        k.asel(ident_f[:], ident_f[:], [[-1, 128]], ALU.is_equal, 0.0, 0, 1)
        k.cp(ident[:], ident_f[:])

        if "A" in phases:
            mk = make_masks(k, pes)
        for l in range(L):
            xin = x_d if l == 0 else x1_d
            xout = out_d if l == L - 1 else x1_d
            if "A" in phases:
                with ExitStack() as es:
                    phase_a(k, es, l, NT, PF, PB, ident, ident_f, xin, oab_d, hglb_d, pre_mix_d, w_in_d, hg_norm_d,
                            conv_d, alog_d, dtb_d, dn_norm_d, mk)
                P.barrier()
            if "B" in phases:
                with ExitStack() as es:
                    phase_b(k, es, l, NT, PF, PB, ident, ident_f, xin, oab_d, hbuf_d, pre_mix_d, w_in_d, w_o_hg_d, w_o_dn_d,
                            w_out_d, post_mix_d)
                P.barrier()
            if "C" in phases:
                with ExitStack() as es:
                    phase_c(k, es, l, NT, PF, PB, ident, ident_f, hbuf_d if ("A" in phases or "B" in phases) else xin,
                            xout, pre_ffn_d, w_gu_d, w_dn_d, post_ffn_d, final_ops, l == L - 1)
                P.barrier()
        P.emit(nc, final_waits=final_ops)
    return nc


def rms_stats(k, src_list, ss, tmp_junk, rstd, lnv):
    n = len(src_list)
    for i, s in enumerate(src_list):
        k.act(tmp_junk[i], s, AF.Square, scale=float(D ** -0.5), accum=ss[:, i:i + 1])
    if n == 2:
        k.tt(ss[:, 0:1], ss[:, 0:1], ss[:, 1:2], ALU.add)
    k.act(lnv[:, 0:1], ss[:, 0:1], AF.Ln, bias=EPS)
    k.act(rstd[:, 0:1], lnv[:, 0:1], AF.Exp, scale=-0.5)


def load_weight_cast(k, dst, src_ap, chan_prefix, counter):
    ch = "%s%d" % (chan_prefix, counter[0])
    counter[0] += 1
    return k.dma("pool", dst, src_ap, ch)


def phase_c(k, es, l, NT, PF, PB, ident, ident_f, hin_d, xout_d, pre_ffn_d, w_gu_d, w_dn_d, post_ffn_d, final_ops, is_last):
    nc = k.nc
    NB = 6
    bw = [512] * 5 + [256]
    boff = [512 * i for i in range(6)]
    wg = [k.sb(es, "wg%d" % b, [128, 8, bw[b]], BF16) for b in range(NB)]
    wu = [k.sb(es, "wu%d" % b, [128, 8, bw[b]], BF16) for b in range(NB)]
    wd = [k.sb(es, "wd%d" % h, [128, 11, D], BF16) for h in range(2)]
    gcol = load_gcol(k, es, "gcol", pre_ffn_d[l], PF[0], ident_f, "cst0")
    gbc = k.sb(es, "gbc", [128, D], F32)
    cnt = [0]
    k.dma("sp", gbc[:], post_ffn_d[l:l + 1, :].partition_broadcast(128), "cst1")
    wsrc = w_gu_d[l].rearrange("(k p) n -> p k n", p=128)
    for b in range(NB):
        for (wt, off) in ((wg[b], boff[b]), (wu[b], DFF + boff[b])):
            load_weight_cast(k, wt[:], wsrc[:, :, off:off + bw[b]], "w", cnt)
    dsrc = w_dn_d[l].rearrange("(k p) n -> p k n", p=128)
    for h in range(2):
        load_weight_cast(k, wd[h][:], dsrc[:, 11 * h:11 * h + 11, :], "w", cnt)
    for b in range(NB):
        for wt in (wg[b], wu[b]):
            k.tt(wt[:], wt[:], V(gcol.t[:, :].unsqueeze(2).broadcast_to([128, 8, bw[b]]), gcol.toks), ALU.mult)

    ht = [k.sb(es, "ht%d" % i, [128, D], F32) for i in range(2)]
    ot = [k.sb(es, "ot%d" % i, [128, D], F32) for i in range(2)]
    junk = k.sb(es, "junk", [128, D], BF16, nparts=2)
    ss = k.sb(es, "ss", [128, 2], F32)
    lnv = k.sb(es, "lnv", [128, 1], F32)
    rstd = k.sb(es, "rstd", [128, 1], F32)
    ss2 = k.sb(es, "ss2", [128, 2], F32)
    lnv2 = k.sb(es, "lnv2", [128, 1], F32)
    rstd2 = k.sb(es, "rstd2", [128, 1], F32)
    vb = k.sb(es, "vb", [128, D], BF16)
    vT = k.sb(es, "vT", [128, D], BF16)
    sg = [k.sb(es, "sg%d" % i, [128, 512], F32) for i in range(2)]
    actb = k.sb(es, "actb", [128, DFF], BF16, nparts=NB)
    actT = k.sb(es, "actT", [128, DFF], BF16, nparts=3)

    for t in range(NT):
        h = ht[t % 2]
        o = ot[t % 2]
        k.dma("sp", h[:], hin_d.rows(t), "hin%d" % (t % 2))
        rms_stats(k, [h[:]], ss, [junk[:]], rstd, lnv)
        k.act(vb[:], h[:], AF.Copy, scale=rstd[:, 0:1])
        for c in range(8):
            k.tr(PB[0][:, c * 128:(c + 1) * 128], vb[:, c * 128:(c + 1) * 128], ident[:])
        k.cp(vT[:], PB[0][:])
        for b in range(NB):
            pg = PF[(2 * b) % 4]
            pu = PF[(2 * b + 1) % 4]
            n = bw[b]
            for c in range(8):
                k.mm(pg[:, 0:n], vT[:, c * 128:(c + 1) * 128], wg[b][:, c, :], start=(c == 0), stop=(c == 7))
            for c in range(8):
                k.mm(pu[:, 0:n], vT[:, c * 128:(c + 1) * 128], wu[b][:, c, :], start=(c == 0), stop=(c == 7))
            s = sg[b % 2]
            k.act(s[:, 0:n], pg[:, 0:n], AF.Silu)
            k.tt(actb.p(b, (slice(None), slice(boff[b], boff[b] + n))), s[:, 0:n], pu[:, 0:n], ALU.mult)
        for g in range(3):
            c0 = g * 8
            c1 = min(22, c0 + 8)
            pb = PB[(g + 1) % 2]
            for c in range(c0, c1):
                blk = (c * 128) // 512
                k.tr(pb[:, (c - c0) * 128:(c - c0 + 1) * 128],
                     actb.p(blk, (slice(None), slice(c * 128, (c + 1) * 128))), ident[:])
            k.cp(actT.p(g, (slice(None), slice(c0 * 128, c1 * 128))), pb[:, 0:(c1 - c0) * 128],
                 eng=("act" if g == 1 else "dve"))
        for hh in range(2):
            pf = PF[4 + hh]
            for c in range(22):
                k.mm(pf[:], actT.p(c // 8, (slice(None), slice(c * 128, (c + 1) * 128))),
                     wd[c // 11][:, c % 11, hh * 512:(hh + 1) * 512], start=(c == 0), stop=(c == 21))
        rms_stats(k, [PF[4][:], PF[5][:]], ss2,
                  [junk.p(0, (slice(None), slice(0, 512))), junk.p(1, (slice(None), slice(512, 1024)))], rstd2, lnv2)
        for hh in range(2):
            sl = slice(hh * 512, (hh + 1) * 512)
            k.stt(o[:, sl], PF[4 + hh][:], rstd2[:, 0:1], gbc[:, sl], ALU.mult, ALU.mult)
        k.tt(o[:], o[:], h[:], ALU.add, eng="pool")
        d = k.dma("pool", xout_d.rows(t), o[:], "oout%d" % (t % 2))
        if is_last:
            final_ops.append(d)


def bc(buf, idx, shape):
    ap = buf.t[idx]
    return V(ap.unsqueeze(len(ap.shape)).broadcast_to(list(shape)), buf.toks)


def bc_mid(buf, idx, shape):
    ap = buf.t[idx]
    return V(ap.unsqueeze(1).broadcast_to(list(shape)), buf.toks)


def load_wcols(k, es, name, src_l, c0, c1, cnt, kch=8):
    wt = k.sb(es, name, [128, kch, c1 - c0], BF16)
    load_weight_cast(k, wt[:], src_l.rearrange("(k p) n -> p k n", p=128)[:, :, c0:c1], "w", cnt)
    return wt


def load_gcol(k, es, name, src_row_ap, PFb, ident_f, chan):
    rows = k.sb(es, name + "_r", [8, 128], F32)
    gcol = k.sb(es, name, [128, 8], F32)
    k.dma("sp", rows[:], src_row_ap.rearrange("(k p) -> k p", p=128), chan)
    k.tr(PFb[:, 0:8], rows[:], ident_f[0:8, 0:8])
    k.cp(gcol[:], PFb[:, 0:8])
    return gcol


def fold_gain(k, wt, gcol, kch, n):
    k.tt(wt[:], wt[:], V(gcol.t[:, :].unsqueeze(2).broadcast_to([128, kch, n]), gcol.toks), ALU.mult)


def x_to_uT(k, xin_d, t, xt, junk, ss, lnv, rstd, ub, uT, PBk, ident, chan):
    k.dma("sp", xt[:], xin_d.rows(t), chan)
    rms_stats(k, [xt[:]], ss, [junk[:]], rstd, lnv)
    k.act(ub[:], xt[:], AF.Copy, scale=rstd[:, 0:1])
    for c in range(8):
        k.tr(PBk[:, c * 128:(c + 1) * 128], ub[:, c * 128:(c + 1) * 128], ident[:])
    k.cp(uT[:], PBk[:])


def proj_tm(k, pf, uT, wt, n=512, c0=0):
    for c in range(8):
        k.mm(pf[:, 0:n], uT[:, c * 128:(c + 1) * 128], wt[:, c, c0:c0 + n], start=(c == 0), stop=(c == 7))


def make_masks(k, es):
    m = {}
    M1 = k.sb(es, "M1", [128, 128], F32)
    M2 = k.sb(es, "M2", [128, 128], F32)
    NS = k.sb(es, "NEGS", [128, 128], F32)
    NCT = k.sb(es, "NEGCT", [128, 128], F32)
    cind = k.sb(es, "cind", [128, 2], F32)
    ones = k.sb(es, "ones_f", [128, 128], F32)
    k.memset(M1[:], 1.0)
    k.asel(M1[:], M1[:], [[1, 128]], ALU.is_ge, 0.0, 0, -1)
    k.memset(M1[0:64, 64:128], 0.0)
    k.memset(M2[:], 1.0)
    k.asel(M2[:], M2[:], [[-1, 128]], ALU.is_gt, 0.0, 0, 1)
    k.memset(M2[64:128, 0:64], 0.0)
    k.ts(NS[:], M2[:], -1.0, 30000.0, ALU.add, ALU.mult)
    k.ts(NCT[:], M1[:], -1.0, 30000.0, ALU.add, ALU.mult)
    k.memset(cind[:], 0.0)
    k.memset(cind[0:64, 0:1], 1.0)
    k.memset(cind[64:128, 1:2], 1.0)
    k.memset(ones[:], 1.0)
    return dict(M1=M1, M2=M2, NS=NS, NCT=NCT, cind=cind, ones=ones)


def head_norm_gate(k, po, ss4, ln4, r4, osb, gsw, dst):
    for h in range(4):
        k.act(osb[:, h * 128:(h + 1) * 128], po[:, h * 128:(h + 1) * 128], AF.Square, scale=float(128 ** -0.5),
              accum=ss4[:, h:h + 1])
    k.act(ln4[:], ss4[:], AF.Ln, bias=EPS)
    k.act(r4[:], ln4[:], AF.Exp, scale=-0.5)
    k.tt(V(osb.t[:, :].rearrange("p (h d) -> p h d", h=4), osb.toks),
         V(po.t[:, :].rearrange("p (h d) -> p h d", h=4), po.toks), bc(r4, (slice(None), slice(None)), [128, 4, 128]),
         ALU.mult)
    k.tt(dst, osb[:], gsw[:], ALU.mult)


def phase_a(k, es, l, NT, PF, PB, ident, ident_f, xin_d, oab_d, hglb_d, pre_mix_d, w_in_d, hg_norm_d, conv_d,
            alog_d, dtb_d, dn_norm_d, mk):
    nc = k.nc
    M1, M2, NS, NCT, cind, ones = mk["M1"], mk["M2"], mk["NS"], mk["NCT"], mk["cind"], mk["ones"]
    cnt = [0]
    win = w_in_d[l]
    Wq = load_wcols(k, es, "Wq", win, 0, 512, cnt)
    Wf = load_wcols(k, es, "Wf", win, 512, 1024, cnt)
    Wi = load_wcols(k, es, "Wi", win, 1024, 1536, cnt)
    Wg = load_wcols(k, es, "Wg", win, 1536, 2048, cnt)
    Wc = [load_wcols(k, es, "Wc%d" % i, win, 2048 + 512 * i, 2560 + 512 * i, cnt) for i in range(3)]
    Wdg = load_wcols(k, es, "Wdg", win, 3584, 4096, cnt)
    Wab = load_wcols(k, es, "Wab", win, 4096, 4104, cnt)
    gcol = load_gcol(k, es, "gcolA", pre_mix_d[l], PF[0], ident_f, "cst0")
    for wt in (Wq, Wf, Wi, Wg, Wc[0], Wc[1], Wc[2], Wdg):
        fold_gain(k, wt, gcol, 8, 512)
    fold_gain(k, Wab, gcol, 8, 8)
    cw = k.sb(es, "cw", [128, 12, 4], F32)
    cwr = k.sb(es, "cwr", [4, 1536], F32)
    k.dma("sp", cwr[:], conv_d[l], "cst1")
    for c in range(12):
        k.tr(PF[1][:, 4 * c:4 * c + 4], cwr[:, c * 128:(c + 1) * 128], ident_f[0:4, 0:4])
    k.cp(V(cw.t[:, :, :].rearrange("p c j -> p (c j)"), cw.toks), PF[1][:, 0:48])
    dtb = k.sb(es, "dtb", [128, 4], F32)
    k.dma("sp", dtb[:], dtb_d[l:l + 1, :].partition_broadcast(128), "cst2")
    negA = k.sb(es, "negA", [128, 4], F32)
    k.dma("sp", negA[:], alog_d[l:l + 1, :].partition_broadcast(128), "cst3")
    k.act(negA[:], negA[:], AF.Exp)
    k.ts(negA[:], negA[:], -1.0, None, ALU.mult)
    hgn = k.sb(es, "hgn", [128, 128], F32)
    k.dma("sp", hgn[:], hg_norm_d[l:l + 1, :].partition_broadcast(128), "cst4")
    dnn = k.sb(es, "dnn", [128, 128], F32)
    k.dma("sp", dnn[:], dn_norm_d[l:l + 1, :].partition_broadcast(128), "cst5")
    omlb = k.sb(es, "omlb", [128, 512], F32)
    if l == 0:
        k.memset(omlb[:], 1.0)
    else:
        lb0 = k.sb(es, "lb0", [128, 512], F32)
        k.dma("sp", omlb[:], hglb_d[1:2, :].partition_broadcast(128), "cst6")
        k.dma("sp", lb0[:], hglb_d[0:1, :].partition_broadcast(128), "cst7")
        k.tt(omlb[:], omlb[:], lb0[:], ALU.subtract)
        k.act(omlb[:], omlb[:], AF.Exp)
        k.ts(omlb[:], omlb[:], 1.0, None, ALU.add)
        k.recip(omlb[:], omlb[:])
    Sh = k.sb(es, "Sh", [128, 4, 128], F32, nparts=4)
    Shb = k.sb(es, "Shb", [128, 4, 128], BF16, nparts=4)
    Sd = k.sb(es, "Sd", [128, 4, 128], F32, nparts=4)
    Sdb = k.sb(es, "Sdb", [128, 4, 128], BF16, nparts=4)
    for S_ in (Sh, Shb, Sd, Sdb):
        k.memset(S_[:], 0.0)
    cvin = k.sb(es, "cvin", [128, 12, 131], F32)
    k.memset(cvin[:], 0.0)
    xt = [k.sb(es, "xtA%d" % i, [128, D], F32) for i in range(2)]
    junk = k.sb(es, "junkA", [128, D], BF16)
    ss = k.sb(es, "ssA", [128, 2], F32)
    lnv = k.sb(es, "lnvA", [128, 1], F32)
    rstd = k.sb(es, "rstdA", [128, 1], F32)
    ub = k.sb(es, "ubA", [128, D], BF16)
    uT = k.sb(es, "uTA", [128, D], BF16)
    kf = k.sb(es, "kf", [128, 512], F32)
    lf = k.sb(es, "lf", [128, 512], F32)
    eb = k.sb(es, "eb", [128, 512], F32)
    enb = k.sb(es, "enb", [128, 512], F32)
    esf = k.sb(es, "esf", [128, 512], F32)
    qe = k.sb(es, "qe", [128, 512], BF16)
    ke = k.sb(es, "ke", [128, 512], BF16)
    kdh = k.sb(es, "kdh", [128, 512], BF16)
    vh = k.sb(es, "vh", [128, 512], BF16)
    gsw = k.sb(es, "gsw", [128, 512], F32)
    qkT = k.sb(es, "qkT", [128, 1024], BF16)
    attnT = k.sb(es, "attnT", [128, 4, 128], BF16)
    dlh = k.sb(es, "dlh", [128, 8], F32)
    osb = k.sb(es, "osb", [128, 512], F32)
    ss4 = k.sb(es, "ss4", [128, 4], F32)
    ln4 = k.sb(es, "ln4", [128, 4], F32)
    r4 = k.sb(es, "r4", [128, 4], F32)
    oab = [k.sb(es, "oabt%d" % i, [128, D], BF16, nparts=2) for i in range(2)]
    cva = k.sb(es, "cva", [128, 12, 128], F32)
    tmpA = k.sb(es, "tmpA", [128, 12, 128], F32)
    qkvF = k.sb(es, "qkvF", [128, 12, 128], BF16)
    qkvT = k.sb(es, "qkvT", [128, 1536], BF16)
    ss8 = k.sb(es, "ss8", [128, 8], F32)
    ln8 = k.sb(es, "ln8", [128, 8], F32)
    r8 = k.sb(es, "r8", [128, 8], F32)
    sq4 = k.sb(es, "sq4", [128, 4], F32)
    beta = k.sb(es, "beta", [128, 4], F32)
    ab8 = k.sb(es, "ab8", [128, 8], F32)
    gt = k.sb(es, "gt", [128, 4], F32)
    egc = k.sb(es, "egc", [128, 4], F32)
    egs = k.sb(es, "egs", [128, 4], F32)
    bg = k.sb(es, "bg", [128, 4], F32)
    gmask = k.sb(es, "gmask", [128, 4, 2], F32)
    dld = k.sb(es, "dld", [128, 8], F32)
    kn = k.sb(es, "kn", [128, 512], BF16)
    kbg = k.sb(es, "kbg", [128, 512], BF16)
    kdd = k.sb(es, "kdd", [128, 512], BF16)
    vbd = k.sb(es, "vbd", [128, 512], BF16)
    qn = k.sb(es, "qn", [128, 512], BF16)
    qg = k.sb(es, "qg", [128, 512], BF16)
    kqT = k.sb(es, "kqT", [128, 8, 128], BF16)
    qgT = k.sb(es, "qgT", [128, 4, 128], BF16)
    Mg = k.sb(es, "Mg", [128, 4, 128], F32, nparts=4)
    Ls = k.sb(es, "Ls", [128, 4, 128], F32)
    LT = k.sb(es, "LT", [128, 4, 128], F32)
    Am = [k.sb(es, "Am%d" % i, [128, 4, 128], BF16) for i in range(2)]
    AmT = [k.sb(es, "AmT%d" % i, [128, 4, 128], BF16) for i in range(2)]
    TT = [k.sb(es, "TT%d" % i, [128, 4, 128], BF16) for i in range(2)]
    nwT = k.sb(es, "nwT", [128, 4, 128], BF16)
    aqk = k.sb(es, "aqk", [128, 4, 128], BF16)
    vn = k.sb(es, "vn", [128, 4, 128], BF16, nparts=4)
    gsd = k.sb(es, "gsd", [128, 512], F32)
    osd = k.sb(es, "osd", [128, 512], F32)
    A = slice(None)

    def v4(b):
        return V(b.t[:, :].rearrange("p (h d) -> p h d", h=4), b.toks)

    for t in range(NT):
        x_ = xt[t % 2]
        ob = oab[t % 2]
        x_to_uT(k, xin_d, t, x_, junk, ss, lnv, rstd, ub, uT, PB[0], ident, "xin%d" % (t % 2))
        proj_tm(k, PF[0], uT, Wf)
        k.act(kf[:], PF[0][:], AF.Sigmoid, scale=-1.0)
        k.tt(kf[:], kf[:], omlb[:], ALU.mult)
        k.act(lf[:], kf[:], AF.Ln, scale=-1.0, bias=1.0)
        k.mm(PF[0][:], M1[:], lf[:])
        k.mm(PF[1][:], M2[:], lf[:])
        for h in range(4):
            k.mm(PF[2][:, 2 * h:2 * h + 2], lf[:, h * 128:(h + 1) * 128], cind[:])
        k.act(eb[:], PF[0][:], AF.Exp)
        k.act(enb[:], PF[0][:], AF.Exp, scale=-1.0)
        k.act(esf[:], PF[1][:], AF.Exp)
        k.act(dlh[:], PF[2][:, 0:8], AF.Exp)
        proj_tm(k, PF[3], uT, Wq)
        k.tt(qe[:], PF[3][:], eb[:], ALU.mult)
        k.tt(ke[:], kf[:], enb[:], ALU.mult)
        k.tt(kdh[:], kf[:], esf[:], ALU.mult)
        proj_tm(k, PF[4], uT, Wi)
        k.cp(vh[:], PF[4][:], eng="act")
        proj_tm(k, PF[5], uT, Wg)
        k.act(gsw[:], PF[5][:], AF.Silu)
        k.tt(v4(gsw), v4(gsw), bc_mid(hgn, (A, A), [128, 4, 128]), ALU.mult)
        for h in range(4):
            k.tr(PB[1][:, h * 128:(h + 1) * 128], qe[:, h * 128:(h + 1) * 128], ident[:])
            k.tr(PB[1][:, 512 + h * 128:512 + (h + 1) * 128], ke[:, h * 128:(h + 1) * 128], ident[:])
        k.cp(qkT[:], PB[1][:])
        for h in range(4):
            k.mm(PF[0][:, h * 128:(h + 1) * 128], qkT[:, 512 + h * 128:512 + (h + 1) * 128],
                 qkT[:, h * 128:(h + 1) * 128])
        k.tt(attnT[:], v4(PF[0]), bc_mid(M1, (A, A), [128, 4, 128]), ALU.mult)
        po = PF[1]
        for h in range(4):
            hs = slice(h * 128, (h + 1) * 128)
            k.mm(po[:, hs], attnT[:, h, :], vh[:, hs], start=True, stop=False)
            for c in range(2):
                cs = slice(64 * c, 64 * c + 64)
                k.mm(po[cs, hs], qkT[:, h * 128 + 64 * c:h * 128 + 64 * c + 64], Shb.p(h, (A, h, A)),
                     start=False, stop=True)
                k.mm(PF[2 + (h % 2)][:, 0:128], kdh[cs, hs], vh[cs, hs])
                k.stt(Sh.p(h, (A, h, A)), Sh.p(h, (A, h, A)), dlh[:, 2 * h + c:2 * h + c + 1],
                      PF[2 + (h % 2)][:, 0:128], ALU.mult, ALU.add)
                k.cp(Shb.p(h, (A, h, A)), Sh.p(h, (A, h, A)), eng="act")
        head_norm_gate(k, po, ss4, ln4, r4, osb, gsw, ob.p(0, (A, slice(0, 512))))

        if CUT == "hg":
            k.dma("sp", oab_d.rows(t), ob[:], "oabw%d" % (t % 2))
            continue
        k.cp(cvin[:, :, 0:3], cvin[:, :, 128:131])
        for i in range(3):
            pf = PF[2 + i]
            for cc in range(4):
                for c in range(8):
                    k.mm(pf[:, cc * 128:(cc + 1) * 128], Wc[i][:, c, cc * 128:(cc + 1) * 128],
                         uT[:, c * 128:(c + 1) * 128], start=(c == 0), stop=(c == 7))
            k.cp(cvin[:, 4 * i:4 * i + 4, 3:131], v4(pf), eng="act")
        for c in range(12):
            k.act(cva[:, c, :], cvin[:, c, 0:128], AF.Copy, scale=cw[:, c, 0:1])
            k.act(tmpA[:, c, :], cvin[:, c, 2:130], AF.Copy, scale=cw[:, c, 2:3])
        for c in range(12):
            k.stt(cva[:, c, :], cvin[:, c, 1:129], cw[:, c, 1:2], cva[:, c, :], ALU.mult, ALU.add)
            k.stt(tmpA[:, c, :], cvin[:, c, 3:131], cw[:, c, 3:4], tmpA[:, c, :], ALU.mult, ALU.add)
        k.tt(cva[:], cva[:], tmpA[:], ALU.add)
        k.act(qkvF[:], cva[:], AF.Silu)
        for c in range(8):
            k.tr(PB[0][:, c * 128:(c + 1) * 128], qkvF[:, c, :], ident[:])
        k.cp(qkvT[:, 0:1024], PB[0][:])
        for c in range(8, 12):
            k.tr(PB[1][:, (c - 8) * 128:(c - 7) * 128], qkvF[:, c, :], ident[:])
        k.cp(qkvT[:, 1024:1536], PB[1][:, 0:512], eng="act")
        if CUT == "conv":
            k.cp(ob.p(1, (A, slice(512, 1024))), qkvT[:, 0:512])
            k.dma("sp", oab_d.rows(t), ob[:], "oabw%d" % (t % 2))
            continue
        for j in range(8):
            k.act(osd[:, (j % 4) * 128:(j % 4 + 1) * 128], qkvT[:, j * 128:(j + 1) * 128], AF.Square,
                  accum=ss8[:, j:j + 1])
        k.act(ln8[:], ss8[:], AF.Ln, bias=EPS)
        k.act(r8[:], ln8[:], AF.Exp, scale=-0.5)
        if CUT == "n1":
            k.dma("sp", oab_d.rows(t), ob[:], "oabw%d" % (t % 2))
            continue
        pab = PF[5]
        for c in range(8):
            k.mm(pab[:, 0:8], uT[:, c * 128:(c + 1) * 128], Wab[:, c, :], start=(c == 0), stop=(c == 7))
        k.cp(ab8[:], pab[:, 0:8], eng="act")
        k.act(beta[:], ab8[:, 4:8], AF.Sigmoid)
        k.tt(gt[:], ab8[:, 0:4], dtb[:], ALU.add)
        k.act(gt[:], gt[:], AF.Exp)
        k.act(gt[:], gt[:], AF.Ln, bias=1.0)
        k.tt(gt[:], gt[:], negA[:], ALU.mult)
        if CUT == "n2":
            k.dma("sp", oab_d.rows(t), ob[:], "oabw%d" % (t % 2))
            continue
        k.mm(pab[:, 16:20], M1[:], gt[:])
        k.mm(pab[:, 24:28], M2[:], gt[:])
        k.tt(gmask[:], bc(gt, (A, A), [128, 4, 2]), bc_mid(cind, (A, A), [128, 4, 2]), ALU.mult)
        k.mm(pab[:, 32:40], ones[:], V(gmask.t[:, :, :].rearrange("p h c -> p (h c)"), gmask.toks))
        k.act(egc[:], pab[:, 16:20], AF.Exp)
        k.act(egs[:], pab[:, 24:28], AF.Exp)
        k.act(dld[:], pab[:, 32:40], AF.Exp)
        if CUT == "n3":
            k.dma("sp", oab_d.rows(t), ob[:], "oabw%d" % (t % 2))
            continue
        k.ts(sq4[:], r8[:, 0:4], float(128 ** -0.5), None, ALU.mult)
        k.tt(bg[:], beta[:], egc[:], ALU.mult)

        def hv(b, lo):
            return V(b.t[:, lo:lo + 512].rearrange("p (h d) -> p h d", h=4), b.toks)

        sh3 = [128, 4, 128]
        for h in range(4):
            hs = slice(h * 128, (h + 1) * 128)
            k.act(kn[:, hs], qkvT[:, 512 + h * 128:512 + (h + 1) * 128], AF.Copy, scale=r8[:, 4 + h:5 + h])
            k.act(qn[:, hs], qkvT[:, h * 128:(h + 1) * 128], AF.Copy, scale=sq4[:, h:h + 1])
            k.act(vbd[:, hs], qkvT[:, 1024 + h * 128:1024 + (h + 1) * 128], AF.Copy, scale=beta[:, h:h + 1])
        for h in range(4):
            hs = slice(h * 128, (h + 1) * 128)
            k.ts(kbg[:, hs], kn[:, hs], bg[:, h:h + 1], None, ALU.mult)
            k.ts(kdd[:, hs], kn[:, hs], egs[:, h:h + 1], None, ALU.mult)
            k.ts(qg[:, hs], qn[:, hs], egc[:, h:h + 1], None, ALU.mult)
        if CUT == "n4":
            k.dma("sp", oab_d.rows(t), ob[:], "oabw%d" % (t % 2))
            continue
        for h in range(4):
            hs = slice(h * 128, (h + 1) * 128)
            k.tr(PB[0][:, hs], kn[:, hs], ident[:])
            k.tr(PB[0][:, 512 + h * 128:512 + (h + 1) * 128], qn[:, hs], ident[:])
            k.tr(PB[1][:, hs], qg[:, hs], ident[:])
        k.cp(V(kqT.t[:, :, :].rearrange("p h d -> p (h d)"), kqT.toks), PB[0][:])
        k.cp(V(qgT.t[:, :, :].rearrange("p h d -> p (h d)"), qgT.toks), PB[1][:, 0:512], eng="act")
        if CUT == "prep":
            k.cp(ob.p(1, (A, slice(512, 1024))), kbg[:])
            k.dma("sp", oab_d.rows(t), ob[:], "oabw%d" % (t % 2))
            continue
        for h in range(4):
            hs = slice(h * 128, (h + 1) * 128)
            k.ts(Mg.p(h, (A, h, A)), M1[:], gt[:, h:h + 1], None, ALU.mult)
            k.mm(PF[2][:, hs], Mg.p(h, (A, h, A)), M2[:], start=True, stop=False)
            k.mm(PF[2][:, hs], ident_f[:], NS[:], start=False, stop=True)
            k.mm(PF[3][:, hs], M2[:], Mg.p(h, (A, h, A)), start=True, stop=False)
            k.mm(PF[3][:, hs], ident_f[:], NCT[:], start=False, stop=True)
            k.mm(PF[0][:, hs], kqT[:, h, :], kqT[:, h, :])
            k.mm(PF[4][:, hs], kqT[:, h, :], kqT[:, 4 + h, :])
        k.act(Ls[:], v4(PF[2]), AF.Exp)
        k.act(LT[:], v4(PF[3]), AF.Exp)
        for h in range(4):
            hs = slice(h * 128, (h + 1) * 128)
            k.stt(Am[0][:, h, :], PF[0][:, hs], beta[:, h:h + 1], Ls[:, h, :], ALU.mult, ALU.mult)
        k.tt(aqk[:], v4(PF[4]), LT[:], ALU.mult)
        for h in range(4):
            k.tr(PB[0][:, h * 128:(h + 1) * 128], Am[0][:, h, :], ident[:])
        k.cp(V(AmT[0].t[:, :, :].rearrange("p h d -> p (h d)"), AmT[0].toks), PB[0][:, 0:512], eng="act")
        k.tt(TT[0][:], bc_mid(ident_f, (A, A), sh3), AmT[0][:], ALU.subtract)
        cur = 0
        for r in range(1, 6):
            nxt = 1 - cur
            pP, pPT, pT = PF[2], PF[3], PF[4]
            for h in range(4):
                hs = slice(h * 128, (h + 1) * 128)
                k.mm(pP[:, hs], AmT[cur][:, h, :], Am[cur][:, h, :])
                if r < 5:
                    k.mm(pPT[:, hs], Am[cur][:, h, :], AmT[cur][:, h, :])
            k.cp(Am[nxt][:], v4(pP))
            if r < 5:
                k.cp(AmT[nxt][:], v4(pPT), eng="act")
            for h in range(4):
                hs = slice(h * 128, (h + 1) * 128)
                k.mm(pT[:, hs], Am[nxt][:, h, :], TT[cur][:, h, :], start=True, stop=False)
                k.mm(pT[:, hs], ident[:], TT[cur][:, h, :], start=False, stop=True)
            k.cp(TT[nxt][:], v4(pT))
            cur = nxt
        TTf = TT[cur]
        if CUT == "inv":
            k.cp(ob.p(1, (A, slice(512, 1024))), V(TTf.t[:, :, :].rearrange("p h d -> p (h d)"), TTf.toks))
            k.dma("sp", oab_d.rows(t), ob[:], "oabw%d" % (t % 2))
            continue
        for h in range(4):
            hs = slice(h * 128, (h + 1) * 128)
            k.mm(PF[2][:, hs], kbg[:, hs], TTf[:, h, :])
        k.act(nwT[:], v4(PF[2]), AF.Copy, scale=-1.0)
        pod = PF[4]
        for h in range(4):
            hs = slice(h * 128, (h + 1) * 128)
            pvn = PF[3] if h % 2 == 0 else PF[2]
            k.mm(pvn[:, hs], TTf[:, h, :], vbd[:, hs], start=True, stop=False)
            for c in range(2):
                cs = slice(64 * c, 64 * c + 64)
                k.mm(pvn[cs, hs], nwT[:, h, cs], Sdb.p(h, (A, h, A)), start=False, stop=True)
                k.mm(pod[cs, hs], qgT[:, h, cs], Sdb.p(h, (A, h, A)), start=True, stop=False)
                k.cp(vn.p(h, (cs, h, A)), pvn[cs, hs], eng="act")
                k.mm(PF[(h % 2)][:, 0:128], kdd[cs, hs], vn.p(h, (cs, h, A)))
                k.stt(Sd.p(h, (A, h, A)), Sd.p(h, (A, h, A)), dld[:, 2 * h + c:2 * h + c + 1],
                      PF[(h % 2)][:, 0:128], ALU.mult, ALU.add)
                k.cp(Sdb.p(h, (A, h, A)), Sd.p(h, (A, h, A)), eng="act")
            k.mm(pod[:, hs], aqk[:, h, :], vn.p(h, (A, h, A)), start=False, stop=True)
        proj_tm(k, PF[5], uT, Wdg)
        k.act(gsd[:], PF[5][:], AF.Silu)
        k.tt(v4(gsd), v4(gsd), bc_mid(dnn, (A, A), [128, 4, 128]), ALU.mult)
        head_norm_gate(k, pod, ss4, ln4, r4, osd, gsd, ob.p(1, (A, slice(512, 1024))))
        k.dma("pool", oab_d.rows(t), ob[:], "oabw%d" % (t % 2))


def phase_b(k, es, l, NT, PF, PB, ident, ident_f, xin_d, oab_d, hout_d, pre_mix_d, w_in_d, w_o_hg_d, w_o_dn_d, w_out_d,
            post_mix_d):
    cnt = [0]
    win = w_in_d[l]
    Wm = [load_wcols(k, es, "Wm%d" % i, win, 4104 + 512 * i, 4616 + 512 * i, cnt) for i in range(4)]
    Whg = load_wcols(k, es, "Whg", w_o_hg_d[l], 0, D, cnt, kch=4)
    Wdn = load_wcols(k, es, "Wdn", w_o_dn_d[l], 0, D, cnt, kch=4)
    Wout = load_wcols(k, es, "Wout", w_out_d[l], 0, D, cnt)
    gcol = load_gcol(k, es, "gcolB", pre_mix_d[l], PF[0], ident_f, "cst0")
    gbc = k.sb(es, "gbcB", [128, D], F32)
    k.dma("sp", gbc[:], post_mix_d[l:l + 1, :].partition_broadcast(128), "cst1")
    for wt in Wm:
        fold_gain(k, wt, gcol, 8, 512)
    xt = [k.sb(es, "xtB%d" % i, [128, D], F32) for i in range(2)]
    ot = [k.sb(es, "otB%d" % i, [128, D], BF16) for i in range(2)]
    ht = [k.sb(es, "htB%d" % i, [128, D], F32) for i in range(2)]
    junk = k.sb(es, "junkB", [128, D], BF16, nparts=2)
    ss = k.sb(es, "ssB", [128, 2], F32)
    lnv = k.sb(es, "lnvB", [128, 1], F32)
    rstd = k.sb(es, "rstdB", [128, 1], F32)
    ss2 = k.sb(es, "ss2B", [128, 2], F32)
    lnv2 = k.sb(es, "lnv2B", [128, 1], F32)
    rstd2 = k.sb(es, "rstd2B", [128, 1], F32)
    ub = k.sb(es, "ubB", [128, D], BF16)
    uT = k.sb(es, "uTB", [128, D], BF16)
    oT = k.sb(es, "oTB", [128, D], BF16)
    sgm = k.sb(es, "sgm", [128, 4, 512], F32, nparts=4)
    t1 = k.sb(es, "t1B", [128, 512], F32)
    mg = k.sb(es, "mgB", [128, D], BF16, nparts=2)
    mgT = k.sb(es, "mgTB", [128, D], BF16)
    A = slice(None)
    for t in range(NT):
        x_ = xt[t % 2]
        o_ = ot[t % 2]
        h_ = ht[t % 2]
        k.dma("sp", o_[:], oab_d.rows(t), "oabr%d" % (t % 2))
        x_to_uT(k, xin_d, t, x_, junk, ss, lnv, rstd, ub, uT, PB[0], ident, "xin%d" % (t % 2))
        for i in range(4):
            proj_tm(k, PF[i], uT, Wm[i])
            k.act(sgm.p(i, (A, i, A)), PF[i][:], AF.Sigmoid)
        for c in range(8):
            k.tr(PB[1][:, c * 128:(c + 1) * 128], o_[:, c * 128:(c + 1) * 128], ident[:])
        k.cp(oT[:], PB[1][:])
        for hh in range(2):
            cs = slice(hh * 512, (hh + 1) * 512)
            pa, pb_ = PF[2 * hh], PF[2 * hh + 1]
            for c in range(4):
                k.mm(pa[:], oT[:, c * 128:(c + 1) * 128], Whg[:, c, cs], start=(c == 0), stop=(c == 3))
            for c in range(4):
                k.mm(pb_[:], oT[:, 512 + c * 128:512 + (c + 1) * 128], Wdn[:, c, cs], start=(c == 0), stop=(c == 3))
            k.tt(t1[:], pa[:], sgm.p(hh, (A, hh, A)), ALU.mult)
            k.tt(sgm.p(2 + hh, (A, 2 + hh, A)), pb_[:], sgm.p(2 + hh, (A, 2 + hh, A)), ALU.mult)
            k.tt(mg.p(hh, (A, cs)), t1[:], sgm.p(2 + hh, (A, 2 + hh, A)), ALU.add)
        for c in range(8):
            k.tr(PB[0][:, c * 128:(c + 1) * 128], mg.p(c // 4, (A, slice(c * 128, (c + 1) * 128))), ident[:])
        k.cp(mgT[:], PB[0][:], eng="act")
        for hh in range(2):
            for c in range(8):
                k.mm(PF[4 + hh][:], mgT[:, c * 128:(c + 1) * 128], Wout[:, c, hh * 512:(hh + 1) * 512],
                     start=(c == 0), stop=(c == 7))
        rms_stats(k, [PF[4][:], PF[5][:]], ss2,
                  [junk.p(0, (A, slice(0, 512))), junk.p(1, (A, slice(512, 1024)))], rstd2, lnv2)
        for hh in range(2):
            sl = slice(hh * 512, (hh + 1) * 512)
            k.stt(h_[:, sl], PF[4 + hh][:], rstd2[:, 0:1], gbc[:, sl], ALU.mult, ALU.mult)
        k.tt(h_[:], h_[:], x_[:], ALU.add, eng="pool")
        k.dma("pool", hout_d.rows(t), h_[:], "hout%d" % (t % 2))


_CACHE = {}

WNAMES = ["hg_lower_bounds", "pre_mix_w", "w_in", "hg_norm_w", "conv_w", "dn_a_log", "dn_dt_bias", "dn_norm_w",
          "w_o_hg", "w_o_dn", "w_out", "post_mix_w", "pre_ffn_w", "w_gate_up", "w_down", "post_ffn_w"]


def kernel(**inputs):
    x = np.ascontiguousarray(inputs["x"], dtype=np.float32)
    B, T, _ = x.shape
    if "nc" not in _CACHE:
        _CACHE["nc"] = build(T=T, L=2)
    nc = _CACHE["nc"]
    shared = {n: np.ascontiguousarray(inputs[n], dtype=np.float32) for n in WNAMES}
    in_maps = []
    for b in range(B):
        m = dict(shared)
        m["x"] = x[b]
        in_maps.append(m)
    res = run_bass_kernel_spmd(nc, in_maps, core_ids=list(range(B)))
    return np.stack([r["out"] for r in res.results], axis=0)
```

```python
from contextlib import ExitStack

import numpy as np
import concourse.bass as bass
import concourse.mybir as mybir
from concourse.bass_utils import run_bass_kernel_spmd

F32 = mybir.dt.float32
BF16 = mybir.dt.bfloat16
AF = mybir.ActivationFunctionType
ALU = mybir.AluOpType

D = 1024
DFF = 2816
INW = 6152
EPS = 1e-6
ENGS = ("pe", "act", "dve", "pool", "sp")
CUT = None
SCHED_WINDOW = 256


class Tok:
    __slots__ = ("last_w", "readers")

    def __init__(self):
        self.last_w = None
        self.readers = []


class Op:
    __slots__ = ("eng", "fn", "idx", "deps", "signal", "semval", "clock", "dma_chan", "waits", "barrier",
                 "cost", "start", "finish", "users", "nun", "ready", "done", "chan_snap")

    def __init__(self, eng, fn):
        self.eng = eng
        self.fn = fn
        self.idx = -1
        self.deps = []
        self.signal = False
        self.semval = 0
        self.clock = None
        self.dma_chan = None
        self.waits = []
        self.barrier = False
        self.cost = 100.0
        self.start = 0.0
        self.finish = 0.0
        self.users = None
        self.nun = 0
        self.ready = 0.0
        self.done = False
        self.chan_snap = None


class Prog:
    def __init__(self):
        self.streams = {e: [] for e in ENGS}
        self.chan_count = {}
        self.chan_last = {}
        self.all_ops = []

    def op(self, eng, fn, reads=(), writes=(), dma_chan=None, cost=100.0):
        o = Op(eng, fn)
        o.dma_chan = dma_chan
        o.cost = cost
        deps = []
        for r in reads:
            if r.last_w is not None:
                deps.append((r.last_w, "raw"))
        for w in writes:
            if w.last_w is not None:
                deps.append((w.last_w, "waw"))
            for rd in w.readers:
                deps.append((rd, "war"))
        o.deps = deps
        for r in reads:
            r.readers.append(o)
        for w in writes:
            w.last_w = o
            w.readers = []
        o.idx = len(self.streams[eng])
        self.streams[eng].append(o)
        self.all_ops.append(o)
        if dma_chan is not None:
            c = self.chan_count.get(dma_chan, 0) + 1
            self.chan_count[dma_chan] = c
            o.semval = 16 * c
            self.chan_last[dma_chan] = o
        return o

    def barrier(self):
        m = Op("sp", None)
        m.barrier = True
        m.chan_snap = list(self.chan_last.values())
        self.all_ops.append(m)

    def schedule(self, window=64, lat=120.0):
        segs = [[]]
        marks = []
        for o in self.all_ops:
            if o.barrier:
                marks.append(o)
                segs.append([])
            else:
                segs[-1].append(o)
        new_streams = {e: [] for e in ENGS}
        new_all = []
        free = {e: 0.0 for e in ENGS}
        reorder = ("pe", "act", "dve")
        for si, seg in enumerate(segs):
            st = {e: [] for e in ENGS}
            inseg = set()
            for o in seg:
                st[o.eng].append(o)
                o.users = []
                o.nun = 0
                o.ready = 0.0
                o.done = False
                inseg.add(id(o))
            for o in seg:
                seen = set()
                for d, _ in o.deps:
                    if d is o or id(d) in seen:
                        continue
                    seen.add(id(d))
                    if id(d) in inseg:
                        d.users.append(o)
                        o.nun += 1
            head = {e: 0 for e in ENGS}
            nleft = len(seg)
            order = []
            while nleft:
                best = None
                for e in ENGS:
                    lst = st[e]
                    h = head[e]
                    while h < len(lst) and lst[h].done:
                        h += 1
                    head[e] = h
                    if h >= len(lst):
                        continue
                    if e in reorder:
                        cand = None
                        cstart = None
                        fe = free[e]
                        for j in range(h, min(len(lst), h + window)):
                            o = lst[j]
                            if o.done or o.nun:
                                continue
                            s0 = o.ready if o.ready > fe else fe
                            if cand is None or s0 < cstart - 1e-9:
                                cand, cstart = o, s0
                                if s0 <= fe:
                                    break
                    else:
                        o = lst[h]
                        if o.nun:
                            continue
                        cand = o
                        cstart = o.ready if o.ready > free[e] else free[e]
                    if cand is not None and (best is None or cstart < best[0]):
                        best = (cstart, cand)
                assert best is not None, "scheduler deadlock"
                t0, o = best
                o.start = t0
                o.done = True
                nleft -= 1
                if o.dma_chan is not None:
                    free[o.eng] = t0 + 60.0
                    o.finish = t0 + o.cost
                else:
                    o.finish = t0 + o.cost
                    free[o.eng] = o.finish
                for u in o.users:
                    u.nun -= 1
                    r = o.finish + (lat if u.eng != o.eng or o.dma_chan is not None else 40.0)
                    if r > u.ready:
                        u.ready = r
                order.append(o)
            order.sort(key=lambda q: q.start)
            for o in order:
                o.idx = len(new_streams[o.eng])
                new_streams[o.eng].append(o)
                new_all.append(o)
            if si < len(marks):
                tmax = max(free.values())
                tmax = max([tmax] + [o.finish for o in order]) if order else tmax
                for e in ENGS:
                    free[e] = tmax
                lasts = []
                for e in ENGS:
                    for o in reversed(new_streams[e]):
                        if o.dma_chan is None and not o.barrier:
                            lasts.append(o)
                            break
                for e in ENGS:
                    b = Op(e, None)
                    b.barrier = True
                    b.deps = [(d, "raw") for d in lasts if d.eng != e] + [(d, "raw") for d in marks[si].chan_snap]
                    b.idx = len(new_streams[e])
                    new_streams[e].append(b)
                    new_all.append(b)
        self.streams = new_streams
        self.all_ops = new_all
        self.est_ns = max(free.values())

    def resolve(self):
        eng_clock = {e: ({}, {}) for e in ENGS}
        for o in self.all_ops:
            cc, dc = eng_clock[o.eng]
            waits = []
            for d, kind in o.deps:
                if d is o:
                    continue
                if d.dma_chan is not None:
                    if dc.get(d.dma_chan, 0) >= d.semval:
                        continue
                    waits.append(d)
                    dc[d.dma_chan] = d.semval
                else:
                    if d.eng == o.eng and kind != "raw" and o.eng == "pe":
                        continue
                    if cc.get(d.eng, -1) >= d.idx:
                        continue
                    waits.append(d)
                    d.signal = True
                    cc[d.eng] = d.idx
                dcc, ddc = d.clock
                for k, v in dcc.items():
                    if cc.get(k, -1) < v:
                        cc[k] = v
                for k, v in ddc.items():
                    if dc.get(k, 0) < v:
                        dc[k] = v
            best = {}
            for d in waits:
                if d.dma_chan is not None:
                    key = ("d", d.dma_chan)
                    val = d.semval
                else:
                    key = ("c", d.eng)
                    val = d.idx
                cur = best.get(key)
                if cur is None or val > cur[0]:
                    best[key] = (val, d)
            o.waits = [v[1] for v in best.values()]
            o.clock = (dict(cc), dict(dc))
            if o.dma_chan is None:
                o.clock[0][o.eng] = max(o.clock[0].get(o.eng, -1), o.idx - 1)
        for e in ENGS:
            n = 0
            for o in self.streams[e]:
                if o.dma_chan is None and o.signal:
                    n += 1
                    o.semval = n

    def emit(self, nc, final_waits=()):
        self.schedule(window=SCHED_WINDOW)
        self.resolve()
        with ExitStack() as es:
            sems = {}
            for e in ENGS:
                sems[("c", e)] = es.enter_context(nc.semaphore("s_" + e))
            for ch in self.chan_count:
                sems[("d", ch)] = es.enter_context(nc.semaphore("d_" + str(ch)))
            block = es.enter_context(nc.Block())

            def run_stream(ename):
                def body(eng):
                    for o in self.streams[ename]:
                        for d in o.waits:
                            if d.dma_chan is not None:
                                eng.wait_ge(sems[("d", d.dma_chan)], d.semval)
                            else:
                                eng.wait_ge(sems[("c", d.eng)], d.semval)
                        if o.fn is None:
                            continue
                        ins = o.fn(eng)
                        if o.dma_chan is not None:
                            ins.then_inc(sems[("d", o.dma_chan)], 16)
                        elif o.signal:
                            ins.then_inc(sems[("c", ename)], 1)
                    if ename == "sp":
                        done = {}
                        for d in final_waits:
                            done[d.dma_chan] = max(done.get(d.dma_chan, 0), d.semval)
                        for ch, v in done.items():
                            eng.wait_ge(sems[("d", ch)], v)

                return body

            block.tensor(run_stream("pe"))
            block.scalar(run_stream("act"))
            block.vector(run_stream("dve"))
            block.gpsimd(run_stream("pool"))
            block.sync(run_stream("sp"))


class V:
    __slots__ = ("ap", "toks")

    def __init__(self, ap, toks):
        self.ap = ap
        self.toks = tuple(toks)


class Buf:
    def __init__(self, t, nparts=1):
        self.t = t
        self.toks = [Tok() for _ in range(nparts)]

    def __getitem__(self, idx):
        return V(self.t[idx], self.toks)

    def p(self, i, idx):
        if isinstance(i, int):
            return V(self.t[idx], (self.toks[i],))
        return V(self.t[idx], [self.toks[j] for j in i])


def _fsz(v):
    ap = v.ap if isinstance(v, V) else v
    n = 1
    for d in ap.shape[1:]:
        n *= int(d)
    return n


def _is_f32(v):
    return v.ap.dtype == F32


class K:
    def __init__(self, nc):
        self.nc = nc
        self.P = Prog()

    def sb(self, es, name, shape, dtype, nparts=1):
        self._uid = getattr(self, "_uid", 0) + 1
        return Buf(es.enter_context(self.nc.sbuf_tensor("%s_%d" % (name, self._uid), list(shape), dtype)), nparts)

    def ps(self, es, name, shape, dtype):
        return Buf(es.enter_context(self.nc.psum_tensor(name, list(shape), dtype)), 1)

    @staticmethod
    def _rw(*vs):
        t = []
        for v in vs:
            if isinstance(v, V):
                t.extend(v.toks)
        return t

    @staticmethod
    def _a(v):
        return v.ap if isinstance(v, V) else v

    def mm(self, out, lhsT, rhs, start=True, stop=True):
        n = _fsz(rhs)
        c = 30.0 + n * (1.7 if _is_f32(rhs) else 0.45)
        self.P.op("pe", lambda e: e.matmul(out.ap, lhsT=lhsT.ap, rhs=rhs.ap, start=start, stop=stop),
                  reads=self._rw(lhsT, rhs), writes=out.toks, cost=c)

    def tr(self, out, in_, ident):
        self.P.op("pe", lambda e: e.transpose(out=out.ap, in_=in_.ap, identity=ident.ap),
                  reads=self._rw(in_, ident), writes=out.toks, cost=100.0)

    def act(self, out, in_, func, bias=None, scale=None, accum=None, eng="act"):
        kw = {}
        if bias is not None:
            kw["bias"] = self._a(bias)
        if scale is not None:
            kw["scale"] = self._a(scale)
        if accum is not None:
            kw["accum_out"] = accum.ap
        self.P.op(eng, lambda e: e.activation(out=out.ap, in_=in_.ap, func=func, **kw),
                  reads=self._rw(in_, bias, scale), writes=self._rw(out, accum), cost=220.0 + _fsz(in_) * 1.05)

    def tt(self, out, in0, in1, op, eng="dve"):
        self.P.op(eng, lambda e: e.tensor_tensor(out=out.ap, in0=in0.ap, in1=in1.ap, op=op),
                  reads=self._rw(in0, in1), writes=out.toks,
                  cost=(100.0 + _fsz(out) * 1.05) * (2.0 if eng == "pool" else 1.0))

    def ts(self, out, in0, s1, s2, op0, op1=None, eng="dve", accum=None):
        kw = {}
        if op1 is not None:
            kw["op1"] = op1
        if accum is not None:
            kw["accum_out"] = accum.ap
        self.P.op(eng, lambda e: e.tensor_scalar(out=out.ap, in0=in0.ap, scalar1=self._a(s1), scalar2=self._a(s2),
                                                 op0=op0, **kw),
                  reads=self._rw(in0, s1, s2), writes=self._rw(out, accum), cost=100.0 + _fsz(out) * 0.8)

    def stt(self, out, in0, scalar, in1, op0, op1, eng="dve"):
        self.P.op(eng, lambda e: e.scalar_tensor_tensor(out=out.ap, in0=in0.ap, scalar=self._a(scalar), in1=in1.ap,
                                                        op0=op0, op1=op1),
                  reads=self._rw(in0, scalar, in1), writes=out.toks, cost=100.0 + _fsz(out) * 1.05)

    def cp(self, out, in_, eng="dve"):
        if eng == "act":
            self.P.op("act", lambda e: e.copy(out=out.ap, in_=in_.ap), reads=in_.toks, writes=out.toks,
                      cost=220.0 + _fsz(out) * 1.05)
        else:
            self.P.op(eng, lambda e: e.tensor_copy(out=out.ap, in_=in_.ap), reads=in_.toks, writes=out.toks,
                      cost=100.0 + _fsz(out) * 0.8)

    def recip(self, out, in_):
        self.P.op("dve", lambda e: e.reciprocal(out=out.ap, in_=in_.ap), reads=in_.toks, writes=out.toks)

    def memset(self, out, val, eng="pool"):
        self.P.op(eng, lambda e: e.memset(out.ap, val), writes=out.toks)

    def asel(self, out, in_, pattern, cmp, fill, base, cm):
        self.P.op("pool", lambda e: e.affine_select(out=out.ap, in_=in_.ap, pattern=pattern, compare_op=cmp,
                                                    fill=fill, base=base, channel_multiplier=cm),
                  reads=in_.toks, writes=out.toks)

    def dma(self, q, out, in_, chan, noncontig=False):
        kw = {"allow_slow_non_contiguous": True} if noncontig else {}
        ap = out.ap if isinstance(out, V) else out
        nbytes = 128.0 * 4
        try:
            nbytes = float(ap.nbytes())
        except Exception:
            pass
        return self.P.op(q, lambda e: e.dma_start(out=self._a(out), in_=self._a(in_), **kw),
                         reads=self._rw(in_), writes=self._rw(out), dma_chan=chan, cost=2500.0 + nbytes / 150.0)


class DramT:
    def __init__(self, ap, ntiles):
        self.ap = ap
        self.toks = [Tok() for _ in range(max(1, ntiles))]

    def rows(self, t, n=128):
        return V(self.ap[t * 128:t * 128 + n], (self.toks[t],))


def build(T=8192, L=2, phases=("A", "B", "C"), dbg=()):
    NT = T // 128
    nc = bass.Bass("TRN2", target_bir_lowering=False)
    k = K(nc)
    P = k.P

    def din(name, shape):
        return nc.dram_tensor(name, list(shape), F32, kind="ExternalInput").ap()

    x_d = DramT(din("x", [T, D]), NT)
    hglb_d = din("hg_lower_bounds", [L, 512])
    pre_mix_d = din("pre_mix_w", [L, D])
    w_in_d = din("w_in", [L, D, INW])
    hg_norm_d = din("hg_norm_w", [L, 128])
    conv_d = din("conv_w", [L, 4, 1536])
    alog_d = din("dn_a_log", [L, 4])
    dtb_d = din("dn_dt_bias", [L, 4])
    dn_norm_d = din("dn_norm_w", [L, 128])
    w_o_hg_d = din("w_o_hg", [L, 512, D])
    w_o_dn_d = din("w_o_dn", [L, 512, D])
    w_out_d = din("w_out", [L, D, D])
    post_mix_d = din("post_mix_w", [L, D])
    pre_ffn_d = din("pre_ffn_w", [L, D])
    w_gu_d = din("w_gate_up", [L, D, 2 * DFF])
    w_dn_d = din("w_down", [L, DFF, D])
    post_ffn_d = din("post_ffn_w", [L, D])
    out_d = DramT(nc.dram_tensor("out", [T, D], F32, kind="ExternalOutput").ap(), NT)
    skind = "ExternalOutput" if dbg else "Internal"
    hbuf_d = DramT(nc.dram_tensor("hbuf", [T, D], F32, kind=skind).ap(), NT)
    x1_d = DramT(nc.dram_tensor("x1buf", [T, D], F32, kind=skind).ap(), NT)
    oab_d = DramT(nc.dram_tensor("oab", [T, D], BF16, kind=skind).ap(), NT)

    final_ops = []
    with ExitStack() as pes:
        PF = [k.ps(pes, "pf%d" % i, [128, 512], F32) for i in range(6)]
        PB = [k.ps(pes, "pb%d" % i, [128, 1024], BF16) for i in range(2)]
        ident_f = k.sb(pes, "ident_f", [128, 128], F32)
        ident = k.sb(pes, "ident", [128, 128], BF16)
        k.memset(ident_f[:], 1.0)
        k.asel(ident_f[:], ident_f[:], [[-1, 128]], ALU.is_equal, 0.0, 0, 1)
        k.cp(ident[:], ident_f[:])

        if "A" in phases:
            mk = make_masks(k, pes)
        for l in range(L):
            xin = x_d if l == 0 else x1_d
            xout = out_d if l == L - 1 else x1_d
            if "A" in phases:
                with ExitStack() as es:
                    phase_a(k, es, l, NT, PF, PB, ident, ident_f, xin, oab_d, hglb_d, pre_mix_d, w_in_d, hg_norm_d,
                            conv_d, alog_d, dtb_d, dn_norm_d, mk)
                P.barrier()
            if "B" in phases:
                with ExitStack() as es:
                    phase_b(k, es, l, NT, PF, PB, ident, ident_f, xin, oab_d, hbuf_d, pre_mix_d, w_in_d, w_o_hg_d, w_o_dn_d,
                            w_out_d, post_mix_d)
                P.barrier()
            if "C" in phases:
                with ExitStack() as es:
                    phase_c(k, es, l, NT, PF, PB, ident, ident_f, hbuf_d if ("A" in phases or "B" in phases) else xin,
                            xout, pre_ffn_d, w_gu_d, w_dn_d, post_ffn_d, final_ops, l == L - 1)
                P.barrier()
        P.emit(nc, final_waits=final_ops)
    return nc


def rms_stats(k, src_list, ss, tmp_junk, rstd, lnv):
    n = len(src_list)
    for i, s in enumerate(src_list):
        k.act(tmp_junk[i], s, AF.Square, scale=float(D ** -0.5), accum=ss[:, i:i + 1])
    if n == 2:
        k.tt(ss[:, 0:1], ss[:, 0:1], ss[:, 1:2], ALU.add)
    k.act(lnv[:, 0:1], ss[:, 0:1], AF.Ln, bias=EPS)
    k.act(rstd[:, 0:1], lnv[:, 0:1], AF.Exp, scale=-0.5)


def load_weight_cast(k, dst, src_ap, chan_prefix, counter):
    ch = "%s%d" % (chan_prefix, counter[0])
    counter[0] += 1
    return k.dma("pool", dst, src_ap, ch)


def phase_c(k, es, l, NT, PF, PB, ident, ident_f, hin_d, xout_d, pre_ffn_d, w_gu_d, w_dn_d, post_ffn_d, final_ops, is_last):
    nc = k.nc
    NB = 6
    bw = [512] * 5 + [256]
    boff = [512 * i for i in range(6)]
    wg = [k.sb(es, "wg%d" % b, [128, 8, bw[b]], BF16) for b in range(NB)]
    wu = [k.sb(es, "wu%d" % b, [128, 8, bw[b]], BF16) for b in range(NB)]
    wd = [k.sb(es, "wd%d" % h, [128, 11, D], BF16) for h in range(2)]
    gcol = load_gcol(k, es, "gcol", pre_ffn_d[l], PF[0], ident_f, "cst0")
    gbc = k.sb(es, "gbc", [128, D], F32)
    cnt = [0]
    k.dma("sp", gbc[:], post_ffn_d[l:l + 1, :].partition_broadcast(128), "cst1")
    wsrc = w_gu_d[l].rearrange("(k p) n -> p k n", p=128)
    for b in range(NB):
        for (wt, off) in ((wg[b], boff[b]), (wu[b], DFF + boff[b])):
            load_weight_cast(k, wt[:], wsrc[:, :, off:off + bw[b]], "w", cnt)
    dsrc = w_dn_d[l].rearrange("(k p) n -> p k n", p=128)
    for h in range(2):
        load_weight_cast(k, wd[h][:], dsrc[:, 11 * h:11 * h + 11, :], "w", cnt)
    for b in range(NB):
        for wt in (wg[b], wu[b]):
            k.tt(wt[:], wt[:], V(gcol.t[:, :].unsqueeze(2).broadcast_to([128, 8, bw[b]]), gcol.toks), ALU.mult)

    ht = [k.sb(es, "ht%d" % i, [128, D], F32) for i in range(2)]
    ot = [k.sb(es, "ot%d" % i, [128, D], F32) for i in range(2)]
    def two(name, shape, dt, nparts=1):
        return [k.sb(es, "%s%d" % (name, i), shape, dt, nparts=nparts) for i in range(2)]
    junkx = two("junk", [128, D], BF16, 2)
    ssx = two("ss", [128, 2], F32)
    lnvx = two("lnv", [128, 1], F32)
    rstdx = two("rstd", [128, 1], F32)
    ss2x = two("ss2", [128, 2], F32)
    lnv2x = two("lnv2", [128, 1], F32)
    rstd2x = two("rstd2", [128, 1], F32)
    vbx = two("vb", [128, D], BF16)
    vTx = two("vT", [128, D], BF16)
    sg = [k.sb(es, "sg%d" % i, [128, 512], F32) for i in range(2)]
    actbx = two("actb", [128, DFF], BF16, NB)
    actTx = two("actT", [128, DFF], BF16, 3)

    for t in range(NT):
        h = ht[t % 2]
        o = ot[t % 2]
        i2 = t % 2
        junk, ss, lnv, rstd, ss2, lnv2, rstd2 = junkx[i2], ssx[i2], lnvx[i2], rstdx[i2], ss2x[i2], lnv2x[i2], rstd2x[i2]
        vb, vT, actb, actT = vbx[i2], vTx[i2], actbx[i2], actTx[i2]
        k.dma("sp", h[:], hin_d.rows(t), "hin%d" % (t % 2))
        rms_stats(k, [h[:]], ss, [junk[:]], rstd, lnv)
        k.act(vb[:], h[:], AF.Copy, scale=rstd[:, 0:1])
        for c in range(8):
            k.tr(PB[0][:, c * 128:(c + 1) * 128], vb[:, c * 128:(c + 1) * 128], ident[:])
        k.cp(vT[:], PB[0][:])
        for b in range(NB):
            pg = PF[(2 * b) % 4]
            pu = PF[(2 * b + 1) % 4]
            n = bw[b]
            for c in range(8):
                k.mm(pg[:, 0:n], vT[:, c * 128:(c + 1) * 128], wg[b][:, c, :], start=(c == 0), stop=(c == 7))
            for c in range(8):
                k.mm(pu[:, 0:n], vT[:, c * 128:(c + 1) * 128], wu[b][:, c, :], start=(c == 0), stop=(c == 7))
            s = sg[b % 2]
            k.act(s[:, 0:n], pg[:, 0:n], AF.Silu)
            k.tt(actb.p(b, (slice(None), slice(boff[b], boff[b] + n))), s[:, 0:n], pu[:, 0:n], ALU.mult)
        for g in range(3):
            c0 = g * 8
            c1 = min(22, c0 + 8)
            pb = PB[(g + 1) % 2]
            for c in range(c0, c1):
                blk = (c * 128) // 512
                k.tr(pb[:, (c - c0) * 128:(c - c0 + 1) * 128],
                     actb.p(blk, (slice(None), slice(c * 128, (c + 1) * 128))), ident[:])
            k.cp(actT.p(g, (slice(None), slice(c0 * 128, c1 * 128))), pb[:, 0:(c1 - c0) * 128],
                 eng=("act" if g == 1 else "dve"))
        for hh in range(2):
            pf = PF[4 + hh]
            for c in range(22):
                k.mm(pf[:], actT.p(c // 8, (slice(None), slice(c * 128, (c + 1) * 128))),
                     wd[c // 11][:, c % 11, hh * 512:(hh + 1) * 512], start=(c == 0), stop=(c == 21))
        rms_stats(k, [PF[4][:], PF[5][:]], ss2,
                  [junk.p(0, (slice(None), slice(0, 512))), junk.p(1, (slice(None), slice(512, 1024)))], rstd2, lnv2)
        for hh in range(2):
            sl = slice(hh * 512, (hh + 1) * 512)
            k.stt(o[:, sl], PF[4 + hh][:], rstd2[:, 0:1], gbc[:, sl], ALU.mult, ALU.mult)
        k.tt(o[:], o[:], h[:], ALU.add, eng="pool")
        d = k.dma("pool", xout_d.rows(t), o[:], "oout%d" % (t % 2))
        if is_last:
            final_ops.append(d)


def bc(buf, idx, shape):
    ap = buf.t[idx]
    return V(ap.unsqueeze(len(ap.shape)).broadcast_to(list(shape)), buf.toks)


def bc_mid(buf, idx, shape):
    ap = buf.t[idx]
    return V(ap.unsqueeze(1).broadcast_to(list(shape)), buf.toks)


def load_wcols(k, es, name, src_l, c0, c1, cnt, kch=8):
    wt = k.sb(es, name, [128, kch, c1 - c0], BF16)
    load_weight_cast(k, wt[:], src_l.rearrange("(k p) n -> p k n", p=128)[:, :, c0:c1], "w", cnt)
    return wt


def load_gcol(k, es, name, src_row_ap, PFb, ident_f, chan):
    rows = k.sb(es, name + "_r", [8, 128], F32)
    gcol = k.sb(es, name, [128, 8], F32)
    k.dma("sp", rows[:], src_row_ap.rearrange("(k p) -> k p", p=128), chan)
    k.tr(PFb[:, 0:8], rows[:], ident_f[0:8, 0:8])
    k.cp(gcol[:], PFb[:, 0:8])
    return gcol


def fold_gain(k, wt, gcol, kch, n):
    k.tt(wt[:], wt[:], V(gcol.t[:, :].unsqueeze(2).broadcast_to([128, kch, n]), gcol.toks), ALU.mult)


def x_to_uT(k, xin_d, t, xt, junk, ss, lnv, rstd, ub, uT, PBk, ident, chan):
    k.dma("sp", xt[:], xin_d.rows(t), chan)
    rms_stats(k, [xt[:]], ss, [junk[:]], rstd, lnv)
    k.act(ub[:], xt[:], AF.Copy, scale=rstd[:, 0:1])
    for c in range(8):
        k.tr(PBk[:, c * 128:(c + 1) * 128], ub[:, c * 128:(c + 1) * 128], ident[:])
    k.cp(uT[:], PBk[:])


def proj_tm(k, pf, uT, wt, n=512, c0=0):
    for c in range(8):
        k.mm(pf[:, 0:n], uT[:, c * 128:(c + 1) * 128], wt[:, c, c0:c0 + n], start=(c == 0), stop=(c == 7))


def make_masks(k, es):
    m = {}
    M1 = k.sb(es, "M1", [128, 128], F32)
    M2 = k.sb(es, "M2", [128, 128], F32)
    NS = k.sb(es, "NEGS", [128, 128], F32)
    NCT = k.sb(es, "NEGCT", [128, 128], F32)
    cind = k.sb(es, "cind", [128, 2], F32)
    ones = k.sb(es, "ones_f", [128, 128], F32)
    k.memset(M1[:], 1.0)
    k.asel(M1[:], M1[:], [[1, 128]], ALU.is_ge, 0.0, 0, -1)
    k.memset(M1[0:64, 64:128], 0.0)
    k.memset(M2[:], 1.0)
    k.asel(M2[:], M2[:], [[-1, 128]], ALU.is_gt, 0.0, 0, 1)
    k.memset(M2[64:128, 0:64], 0.0)
    k.ts(NS[:], M2[:], -1.0, 30000.0, ALU.add, ALU.mult)
    k.ts(NCT[:], M1[:], -1.0, 30000.0, ALU.add, ALU.mult)
    k.memset(cind[:], 0.0)
    k.memset(cind[0:64, 0:1], 1.0)
    k.memset(cind[64:128, 1:2], 1.0)
    k.memset(ones[:], 1.0)
    return dict(M1=M1, M2=M2, NS=NS, NCT=NCT, cind=cind, ones=ones)


def head_norm_gate(k, po, ss4, ln4, r4, osb, gsw, dst):
    for h in range(4):
        k.act(osb[:, h * 128:(h + 1) * 128], po[:, h * 128:(h + 1) * 128], AF.Square, scale=float(128 ** -0.5),
              accum=ss4[:, h:h + 1])
    k.act(ln4[:], ss4[:], AF.Ln, bias=EPS)
    k.act(r4[:], ln4[:], AF.Exp, scale=-0.5)
    k.tt(V(osb.t[:, :].rearrange("p (h d) -> p h d", h=4), osb.toks),
         V(po.t[:, :].rearrange("p (h d) -> p h d", h=4), po.toks), bc(r4, (slice(None), slice(None)), [128, 4, 128]),
         ALU.mult)
    k.tt(dst, osb[:], gsw[:], ALU.mult)


def phase_a(k, es, l, NT, PF, PB, ident, ident_f, xin_d, oab_d, hglb_d, pre_mix_d, w_in_d, hg_norm_d, conv_d,
            alog_d, dtb_d, dn_norm_d, mk):
    nc = k.nc
    M1, M2, NS, NCT, cind, ones = mk["M1"], mk["M2"], mk["NS"], mk["NCT"], mk["cind"], mk["ones"]
    cnt = [0]
    win = w_in_d[l]
    Wq = load_wcols(k, es, "Wq", win, 0, 512, cnt)
    Wf = load_wcols(k, es, "Wf", win, 512, 1024, cnt)
    Wi = load_wcols(k, es, "Wi", win, 1024, 1536, cnt)
    Wg = load_wcols(k, es, "Wg", win, 1536, 2048, cnt)
    Wc = [load_wcols(k, es, "Wc%d" % i, win, 2048 + 512 * i, 2560 + 512 * i, cnt) for i in range(3)]
    Wdg = load_wcols(k, es, "Wdg", win, 3584, 4096, cnt)
    Wab = load_wcols(k, es, "Wab", win, 4096, 4104, cnt)
    gcol = load_gcol(k, es, "gcolA", pre_mix_d[l], PF[0], ident_f, "cst0")
    for wt in (Wq, Wf, Wi, Wg, Wc[0], Wc[1], Wc[2], Wdg):
        fold_gain(k, wt, gcol, 8, 512)
    fold_gain(k, Wab, gcol, 8, 8)
    cw = k.sb(es, "cw", [128, 12, 4], F32)
    cwr = k.sb(es, "cwr", [4, 1536], F32)
    k.dma("sp", cwr[:], conv_d[l], "cst1")
    for c in range(12):
        k.tr(PF[1][:, 4 * c:4 * c + 4], cwr[:, c * 128:(c + 1) * 128], ident_f[0:4, 0:4])
    k.cp(V(cw.t[:, :, :].rearrange("p c j -> p (c j)"), cw.toks), PF[1][:, 0:48])
    dtb = k.sb(es, "dtb", [128, 4], F32)
    k.dma("sp", dtb[:], dtb_d[l:l + 1, :].partition_broadcast(128), "cst2")
    negA = k.sb(es, "negA", [128, 4], F32)
    k.dma("sp", negA[:], alog_d[l:l + 1, :].partition_broadcast(128), "cst3")
    k.act(negA[:], negA[:], AF.Exp)
    k.ts(negA[:], negA[:], -1.0, None, ALU.mult)
    hgn = k.sb(es, "hgn", [128, 128], F32)
    k.dma("sp", hgn[:], hg_norm_d[l:l + 1, :].partition_broadcast(128), "cst4")
    dnn = k.sb(es, "dnn", [128, 128], F32)
    k.dma("sp", dnn[:], dn_norm_d[l:l + 1, :].partition_broadcast(128), "cst5")
    omlb = k.sb(es, "omlb", [128, 512], F32)
    if l == 0:
        k.memset(omlb[:], 1.0)
    else:
        lb0 = k.sb(es, "lb0", [128, 512], F32)
        k.dma("sp", omlb[:], hglb_d[1:2, :].partition_broadcast(128), "cst6")
        k.dma("sp", lb0[:], hglb_d[0:1, :].partition_broadcast(128), "cst7")
        k.tt(omlb[:], omlb[:], lb0[:], ALU.subtract)
        k.act(omlb[:], omlb[:], AF.Exp)
        k.ts(omlb[:], omlb[:], 1.0, None, ALU.add)
        k.recip(omlb[:], omlb[:])
    Sh = k.sb(es, "Sh", [128, 4, 128], F32, nparts=4)
    Shb = k.sb(es, "Shb", [128, 4, 128], BF16, nparts=4)
    Sd = k.sb(es, "Sd", [128, 4, 128], F32, nparts=4)
    Sdb = k.sb(es, "Sdb", [128, 4, 128], BF16, nparts=4)
    for S_ in (Sh, Shb, Sd, Sdb):
        k.memset(S_[:], 0.0)
    cvin = k.sb(es, "cvin", [128, 12, 131], F32)
    k.memset(cvin[:], 0.0)
    xt = [k.sb(es, "xtA%d" % i, [128, D], F32) for i in range(2)]
    junk = k.sb(es, "junkA", [128, D], BF16)
    ss = k.sb(es, "ssA", [128, 2], F32)
    lnv = k.sb(es, "lnvA", [128, 1], F32)
    rstd = k.sb(es, "rstdA", [128, 1], F32)
    ub2 = [k.sb(es, "ubA%d" % i, [128, D], BF16) for i in range(2)]
    uT2 = [k.sb(es, "uTA%d" % i, [128, D], BF16) for i in range(2)]
    kf = k.sb(es, "kf", [128, 512], F32)
    lf = k.sb(es, "lf", [128, 512], F32)
    eb = k.sb(es, "eb", [128, 512], F32)
    enb = k.sb(es, "enb", [128, 512], F32)
    esf = k.sb(es, "esf", [128, 512], F32)
    qe = k.sb(es, "qe", [128, 512], BF16)
    ke = k.sb(es, "ke", [128, 512], BF16)
    kdh = k.sb(es, "kdh", [128, 512], BF16)
    vh = k.sb(es, "vh", [128, 512], BF16)
    gsw = k.sb(es, "gsw", [128, 512], F32)
    qkT = k.sb(es, "qkT", [128, 1024], BF16)
    attnT = k.sb(es, "attnT", [128, 4, 128], BF16)
    dlh = k.sb(es, "dlh", [128, 8], F32)
    osb = k.sb(es, "osb", [128, 512], F32)
    ss4 = k.sb(es, "ss4", [128, 4], F32)
    ln4 = k.sb(es, "ln4", [128, 4], F32)
    r4 = k.sb(es, "r4", [128, 4], F32)
    oab = [k.sb(es, "oabt%d" % i, [128, D], BF16, nparts=2) for i in range(2)]
    cva = k.sb(es, "cva", [128, 12, 128], F32)
    tmpA = k.sb(es, "tmpA", [128, 12, 128], F32)
    qkvF = k.sb(es, "qkvF", [128, 12, 128], BF16)
    qkvT = k.sb(es, "qkvT", [128, 1536], BF16)
    ss8 = k.sb(es, "ss8", [128, 8], F32)
    ln8 = k.sb(es, "ln8", [128, 8], F32)
    r8 = k.sb(es, "r8", [128, 8], F32)
    sq4 = k.sb(es, "sq4", [128, 4], F32)
    beta = k.sb(es, "beta", [128, 4], F32)
    ab8 = k.sb(es, "ab8", [128, 8], F32)
    gt = k.sb(es, "gt", [128, 4], F32)
    egc = k.sb(es, "egc", [128, 4], F32)
    egs = k.sb(es, "egs", [128, 4], F32)
    bg = k.sb(es, "bg", [128, 4], F32)
    gmask = k.sb(es, "gmask", [128, 4, 2], F32)
    dld = k.sb(es, "dld", [128, 8], F32)
    kn = k.sb(es, "kn", [128, 512], BF16)
    kbg = k.sb(es, "kbg", [128, 512], BF16)
    kdd = k.sb(es, "kdd", [128, 512], BF16)
    vbd = k.sb(es, "vbd", [128, 512], BF16)
    qn = k.sb(es, "qn", [128, 512], BF16)
    qg = k.sb(es, "qg", [128, 512], BF16)
    kqT = k.sb(es, "kqT", [128, 8, 128], BF16)
    qgT = k.sb(es, "qgT", [128, 4, 128], BF16)
    Mg = k.sb(es, "Mg", [128, 4, 128], F32, nparts=4)
    Ls = k.sb(es, "Ls", [128, 4, 128], F32)
    LT = k.sb(es, "LT", [128, 4, 128], F32)
    Am = [k.sb(es, "Am%d" % i, [128, 4, 128], BF16) for i in range(2)]
    AmT = [k.sb(es, "AmT%d" % i, [128, 4, 128], BF16) for i in range(2)]
    TT = [k.sb(es, "TT%d" % i, [128, 4, 128], BF16) for i in range(2)]
    nwT = k.sb(es, "nwT", [128, 4, 128], BF16)
    aqk = k.sb(es, "aqk", [128, 4, 128], BF16)
    vn = k.sb(es, "vn", [128, 4, 128], BF16, nparts=4)
    gsd2 = [k.sb(es, "gsd%d" % i, [128, 512], F32) for i in range(2)]
    osd = k.sb(es, "osd", [128, 512], F32)
    A = slice(None)

    def v4(b):
        return V(b.t[:, :].rearrange("p (h d) -> p h d", h=4), b.toks)

    for t in range(NT):
        x_ = xt[t % 2]
        ob = oab[t % 2]
        ub = ub2[t % 2]
        uT = uT2[t % 2]
        gsd = gsd2[t % 2]
        x_to_uT(k, xin_d, t, x_, junk, ss, lnv, rstd, ub, uT, PB[0], ident, "xin%d" % (t % 2))
        proj_tm(k, PF[0], uT, Wf)
        k.act(kf[:], PF[0][:], AF.Sigmoid, scale=-1.0)
        k.tt(kf[:], kf[:], omlb[:], ALU.mult)
        k.act(lf[:], kf[:], AF.Ln, scale=-1.0, bias=1.0)
        k.mm(PF[0][:], M1[:], lf[:])
        k.mm(PF[1][:], M2[:], lf[:])
        for h in range(4):
            k.mm(PF[2][:, 2 * h:2 * h + 2], lf[:, h * 128:(h + 1) * 128], cind[:])
        k.act(eb[:], PF[0][:], AF.Exp)
        k.act(enb[:], PF[0][:], AF.Exp, scale=-1.0)
        k.act(esf[:], PF[1][:], AF.Exp)
        k.act(dlh[:], PF[2][:, 0:8], AF.Exp)
        proj_tm(k, PF[3], uT, Wq)
        k.tt(qe[:], PF[3][:], eb[:], ALU.mult)
        k.tt(ke[:], kf[:], enb[:], ALU.mult)
        k.tt(kdh[:], kf[:], esf[:], ALU.mult)
        proj_tm(k, PF[4], uT, Wi)
        k.cp(vh[:], PF[4][:], eng="act")
        proj_tm(k, PF[5], uT, Wg)
        k.act(gsw[:], PF[5][:], AF.Silu)
        k.tt(v4(gsw), v4(gsw), bc_mid(hgn, (A, A), [128, 4, 128]), ALU.mult)
        proj_tm(k, PF[5], uT, Wdg)
        k.act(gsd[:], PF[5][:], AF.Silu)
        k.tt(v4(gsd), v4(gsd), bc_mid(dnn, (A, A), [128, 4, 128]), ALU.mult)
        for h in range(4):
            k.tr(PB[1][:, h * 128:(h + 1) * 128], qe[:, h * 128:(h + 1) * 128], ident[:])
            k.tr(PB[1][:, 512 + h * 128:512 + (h + 1) * 128], ke[:, h * 128:(h + 1) * 128], ident[:])
        k.cp(qkT[:], PB[1][:])
        for h in range(4):
            k.mm(PF[0][:, h * 128:(h + 1) * 128], qkT[:, 512 + h * 128:512 + (h + 1) * 128],
                 qkT[:, h * 128:(h + 1) * 128])
        k.tt(attnT[:], v4(PF[0]), bc_mid(M1, (A, A), [128, 4, 128]), ALU.mult)
        po = PF[1]
        for h in range(4):
            hs = slice(h * 128, (h + 1) * 128)
            k.mm(po[:, hs], attnT[:, h, :], vh[:, hs], start=True, stop=False)
            for c in range(2):
                cs = slice(64 * c, 64 * c + 64)
                k.mm(po[cs, hs], qkT[:, h * 128 + 64 * c:h * 128 + 64 * c + 64], Shb.p(h, (A, h, A)),
                     start=False, stop=True)
                k.mm(PF[2 + (h % 2)][:, 0:128], kdh[cs, hs], vh[cs, hs])
                k.stt(Sh.p(h, (A, h, A)), Sh.p(h, (A, h, A)), dlh[:, 2 * h + c:2 * h + c + 1],
                      PF[2 + (h % 2)][:, 0:128], ALU.mult, ALU.add)
                k.cp(Shb.p(h, (A, h, A)), Sh.p(h, (A, h, A)), eng="act")
        head_norm_gate(k, po, ss4, ln4, r4, osb, gsw, ob.p(0, (A, slice(0, 512))))

        if CUT == "hg":
            k.dma("sp", oab_d.rows(t), ob[:], "oabw%d" % (t % 2))
            continue
        k.cp(cvin[:, :, 0:3], cvin[:, :, 128:131])
        for i in range(3):
            pf = PF[2 + i]
            for cc in range(4):
                for c in range(8):
                    k.mm(pf[:, cc * 128:(cc + 1) * 128], Wc[i][:, c, cc * 128:(cc + 1) * 128],
                         uT[:, c * 128:(c + 1) * 128], start=(c == 0), stop=(c == 7))
            k.cp(cvin[:, 4 * i:4 * i + 4, 3:131], v4(pf), eng="act")
        for c in range(12):
            k.act(cva[:, c, :], cvin[:, c, 0:128], AF.Copy, scale=cw[:, c, 0:1])
            k.act(tmpA[:, c, :], cvin[:, c, 2:130], AF.Copy, scale=cw[:, c, 2:3])
        for c in range(12):
            k.stt(cva[:, c, :], cvin[:, c, 1:129], cw[:, c, 1:2], cva[:, c, :], ALU.mult, ALU.add)
            k.stt(tmpA[:, c, :], cvin[:, c, 3:131], cw[:, c, 3:4], tmpA[:, c, :], ALU.mult, ALU.add)
        k.tt(cva[:], cva[:], tmpA[:], ALU.add)
        k.act(qkvF[:], cva[:], AF.Silu)
        for c in range(8):
            k.tr(PB[0][:, c * 128:(c + 1) * 128], qkvF[:, c, :], ident[:])
        k.cp(qkvT[:, 0:1024], PB[0][:])
        for c in range(8, 12):
            k.tr(PB[1][:, (c - 8) * 128:(c - 7) * 128], qkvF[:, c, :], ident[:])
        k.cp(qkvT[:, 1024:1536], PB[1][:, 0:512], eng="act")
        if CUT == "conv":
            k.cp(ob.p(1, (A, slice(512, 1024))), qkvT[:, 0:512])
            k.dma("sp", oab_d.rows(t), ob[:], "oabw%d" % (t % 2))
            continue
        for j in range(8):
            k.act(osd[:, (j % 4) * 128:(j % 4 + 1) * 128], qkvT[:, j * 128:(j + 1) * 128], AF.Square,
                  accum=ss8[:, j:j + 1])
        k.act(ln8[:], ss8[:], AF.Ln, bias=EPS)
        k.act(r8[:], ln8[:], AF.Exp, scale=-0.5)
        if CUT == "n1":
            k.dma("sp", oab_d.rows(t), ob[:], "oabw%d" % (t % 2))
            continue
        pab = PF[5]
        for c in range(8):
            k.mm(pab[:, 0:8], uT[:, c * 128:(c + 1) * 128], Wab[:, c, :], start=(c == 0), stop=(c == 7))
        k.cp(ab8[:], pab[:, 0:8], eng="act")
        k.act(beta[:], ab8[:, 4:8], AF.Sigmoid)
        k.tt(gt[:], ab8[:, 0:4], dtb[:], ALU.add)
        k.act(gt[:], gt[:], AF.Exp)
        k.act(gt[:], gt[:], AF.Ln, bias=1.0)
        k.tt(gt[:], gt[:], negA[:], ALU.mult)
        if CUT == "n2":
            k.dma("sp", oab_d.rows(t), ob[:], "oabw%d" % (t % 2))
            continue
        k.mm(pab[:, 16:20], M1[:], gt[:])
        k.mm(pab[:, 24:28], M2[:], gt[:])
        k.tt(gmask[:], bc(gt, (A, A), [128, 4, 2]), bc_mid(cind, (A, A), [128, 4, 2]), ALU.mult)
        k.mm(pab[:, 32:40], ones[:], V(gmask.t[:, :, :].rearrange("p h c -> p (h c)"), gmask.toks))
        k.act(egc[:], pab[:, 16:20], AF.Exp)
        k.act(egs[:], pab[:, 24:28], AF.Exp)
        k.act(dld[:], pab[:, 32:40], AF.Exp)
        if CUT == "n3":
            k.dma("sp", oab_d.rows(t), ob[:], "oabw%d" % (t % 2))
            continue
        k.ts(sq4[:], r8[:, 0:4], float(128 ** -0.5), None, ALU.mult)
        k.tt(bg[:], beta[:], egc[:], ALU.mult)

        def hv(b, lo):
            return V(b.t[:, lo:lo + 512].rearrange("p (h d) -> p h d", h=4), b.toks)

        sh3 = [128, 4, 128]
        for h in range(4):
            hs = slice(h * 128, (h + 1) * 128)
            k.act(kn[:, hs], qkvT[:, 512 + h * 128:512 + (h + 1) * 128], AF.Copy, scale=r8[:, 4 + h:5 + h])
            k.act(qn[:, hs], qkvT[:, h * 128:(h + 1) * 128], AF.Copy, scale=sq4[:, h:h + 1])
            k.act(vbd[:, hs], qkvT[:, 1024 + h * 128:1024 + (h + 1) * 128], AF.Copy, scale=beta[:, h:h + 1])
        for h in range(4):
            hs = slice(h * 128, (h + 1) * 128)
            k.ts(kbg[:, hs], kn[:, hs], bg[:, h:h + 1], None, ALU.mult)
            k.ts(kdd[:, hs], kn[:, hs], egs[:, h:h + 1], None, ALU.mult)
            k.ts(qg[:, hs], qn[:, hs], egc[:, h:h + 1], None, ALU.mult)
        if CUT == "n4":
            k.dma("sp", oab_d.rows(t), ob[:], "oabw%d" % (t % 2))
            continue
        for h in range(4):
            hs = slice(h * 128, (h + 1) * 128)
            k.tr(PB[0][:, hs], kn[:, hs], ident[:])
            k.tr(PB[0][:, 512 + h * 128:512 + (h + 1) * 128], qn[:, hs], ident[:])
            k.tr(PB[1][:, hs], qg[:, hs], ident[:])
        k.cp(V(kqT.t[:, :, :].rearrange("p h d -> p (h d)"), kqT.toks), PB[0][:])
        k.cp(V(qgT.t[:, :, :].rearrange("p h d -> p (h d)"), qgT.toks), PB[1][:, 0:512], eng="act")
        if CUT == "prep":
            k.cp(ob.p(1, (A, slice(512, 1024))), kbg[:])
            k.dma("sp", oab_d.rows(t), ob[:], "oabw%d" % (t % 2))
            continue
        for h in range(4):
            hs = slice(h * 128, (h + 1) * 128)
            k.ts(Mg.p(h, (A, h, A)), M1[:], gt[:, h:h + 1], None, ALU.mult)
            k.mm(PF[2][:, hs], Mg.p(h, (A, h, A)), M2[:], start=True, stop=False)
            k.mm(PF[2][:, hs], ident_f[:], NS[:], start=False, stop=True)
            k.mm(PF[3][:, hs], M2[:], Mg.p(h, (A, h, A)), start=True, stop=False)
            k.mm(PF[3][:, hs], ident_f[:], NCT[:], start=False, stop=True)
            k.mm(PF[0][:, hs], kqT[:, h, :], kqT[:, h, :])
            k.mm(PF[4][:, hs], kqT[:, h, :], kqT[:, 4 + h, :])
        k.act(Ls[:], v4(PF[2]), AF.Exp)
        k.act(LT[:], v4(PF[3]), AF.Exp)
        for h in range(4):
            hs = slice(h * 128, (h + 1) * 128)
            k.stt(Am[0][:, h, :], PF[0][:, hs], beta[:, h:h + 1], Ls[:, h, :], ALU.mult, ALU.mult)
        k.tt(aqk[:], v4(PF[4]), LT[:], ALU.mult)
        for h in range(4):
            k.tr(PB[0][:, h * 128:(h + 1) * 128], Am[0][:, h, :], ident[:])
        k.cp(V(AmT[0].t[:, :, :].rearrange("p h d -> p (h d)"), AmT[0].toks), PB[0][:, 0:512], eng="act")
        k.tt(TT[0][:], bc_mid(ident_f, (A, A), sh3), AmT[0][:], ALU.subtract)
        cur = 0
        for r in range(1, 6):
            nxt = 1 - cur
            pP, pPT, pT = PF[2], PF[3], PF[4]
            for h in range(4):
                hs = slice(h * 128, (h + 1) * 128)
                k.mm(pP[:, hs], AmT[cur][:, h, :], Am[cur][:, h, :])
                if r < 5:
                    k.mm(pPT[:, hs], Am[cur][:, h, :], AmT[cur][:, h, :])
            k.cp(Am[nxt][:], v4(pP))
            if r < 5:
                k.cp(AmT[nxt][:], v4(pPT), eng="act")
            for h in range(4):
                hs = slice(h * 128, (h + 1) * 128)
                k.mm(pT[:, hs], Am[nxt][:, h, :], TT[cur][:, h, :], start=True, stop=False)
                k.mm(pT[:, hs], ident[:], TT[cur][:, h, :], start=False, stop=True)
            k.cp(TT[nxt][:], v4(pT))
            cur = nxt
        TTf = TT[cur]
        if CUT == "inv":
            k.cp(ob.p(1, (A, slice(512, 1024))), V(TTf.t[:, :, :].rearrange("p h d -> p (h d)"), TTf.toks))
            k.dma("sp", oab_d.rows(t), ob[:], "oabw%d" % (t % 2))
            continue
        for h in range(4):
            hs = slice(h * 128, (h + 1) * 128)
            k.mm(PF[2][:, hs], kbg[:, hs], TTf[:, h, :])
        k.act(nwT[:], v4(PF[2]), AF.Copy, scale=-1.0)
        pod = PF[4]
        for h in range(4):
            hs = slice(h * 128, (h + 1) * 128)
            pvn = PF[3] if h % 2 == 0 else PF[2]
            k.mm(pvn[:, hs], TTf[:, h, :], vbd[:, hs], start=True, stop=False)
            for c in range(2):
                cs = slice(64 * c, 64 * c + 64)
                k.mm(pvn[cs, hs], nwT[:, h, cs], Sdb.p(h, (A, h, A)), start=False, stop=True)
                k.mm(pod[cs, hs], qgT[:, h, cs], Sdb.p(h, (A, h, A)), start=True, stop=False)
                k.cp(vn.p(h, (cs, h, A)), pvn[cs, hs], eng="act")
                k.mm(PF[(h % 2)][:, 0:128], kdd[cs, hs], vn.p(h, (cs, h, A)))
                k.stt(Sd.p(h, (A, h, A)), Sd.p(h, (A, h, A)), dld[:, 2 * h + c:2 * h + c + 1],
                      PF[(h % 2)][:, 0:128], ALU.mult, ALU.add)
                k.cp(Sdb.p(h, (A, h, A)), Sd.p(h, (A, h, A)), eng="act")
            k.mm(pod[:, hs], aqk[:, h, :], vn.p(h, (A, h, A)), start=False, stop=True)
        head_norm_gate(k, pod, ss4, ln4, r4, osd, gsd, ob.p(1, (A, slice(512, 1024))))
        k.dma("pool", oab_d.rows(t), ob[:], "oabw%d" % (t % 2))


def phase_b(k, es, l, NT, PF, PB, ident, ident_f, xin_d, oab_d, hout_d, pre_mix_d, w_in_d, w_o_hg_d, w_o_dn_d, w_out_d,
            post_mix_d):
    cnt = [0]
    win = w_in_d[l]
    Wm = [load_wcols(k, es, "Wm%d" % i, win, 4104 + 512 * i, 4616 + 512 * i, cnt) for i in range(4)]
    Whg = load_wcols(k, es, "Whg", w_o_hg_d[l], 0, D, cnt, kch=4)
    Wdn = load_wcols(k, es, "Wdn", w_o_dn_d[l], 0, D, cnt, kch=4)
    Wout = load_wcols(k, es, "Wout", w_out_d[l], 0, D, cnt)
    gcol = load_gcol(k, es, "gcolB", pre_mix_d[l], PF[0], ident_f, "cst0")
    gbc = k.sb(es, "gbcB", [128, D], F32)
    k.dma("sp", gbc[:], post_mix_d[l:l + 1, :].partition_broadcast(128), "cst1")
    for wt in Wm:
        fold_gain(k, wt, gcol, 8, 512)
    xt = [k.sb(es, "xtB%d" % i, [128, D], F32) for i in range(2)]
    ot = [k.sb(es, "otB%d" % i, [128, D], BF16) for i in range(2)]
    ht = [k.sb(es, "htB%d" % i, [128, D], F32) for i in range(2)]
    def two(name, shape, dt, nparts=1):
        return [k.sb(es, "%s%d" % (name, i), shape, dt, nparts=nparts) for i in range(2)]
    junk2 = two("junkB", [128, D], BF16, 2)
    ssx = two("ssB", [128, 2], F32)
    lnvx = two("lnvB", [128, 1], F32)
    rstdx = two("rstdB", [128, 1], F32)
    ss2x = two("ss2B", [128, 2], F32)
    lnv2x = two("lnv2B", [128, 1], F32)
    rstd2x = two("rstd2B", [128, 1], F32)
    ubx = two("ubB", [128, D], BF16)
    uTx = two("uTB", [128, D], BF16)
    oTx = two("oTB", [128, D], BF16)
    sgmx = two("sgm", [128, 4, 512], F32, 4)
    t1x = two("t1B", [128, 512], F32)
    mgx = two("mgB", [128, D], BF16, 2)
    mgTx = two("mgTB", [128, D], BF16)
    A = slice(None)
    for t in range(NT):
        x_ = xt[t % 2]
        o_ = ot[t % 2]
        h_ = ht[t % 2]
        i2 = t % 2
        junk, ss, lnv, rstd, ss2, lnv2, rstd2 = junk2[i2], ssx[i2], lnvx[i2], rstdx[i2], ss2x[i2], lnv2x[i2], rstd2x[i2]
        ub, uT, oT, sgm, t1, mg, mgT = ubx[i2], uTx[i2], oTx[i2], sgmx[i2], t1x[i2], mgx[i2], mgTx[i2]
        k.dma("sp", o_[:], oab_d.rows(t), "oabr%d" % (t % 2))
        x_to_uT(k, xin_d, t, x_, junk, ss, lnv, rstd, ub, uT, PB[0], ident, "xin%d" % (t % 2))
        for i in range(4):
            proj_tm(k, PF[i], uT, Wm[i])
            k.act(sgm.p(i, (A, i, A)), PF[i][:], AF.Sigmoid)
        for c in range(8):
            k.tr(PB[1][:, c * 128:(c + 1) * 128], o_[:, c * 128:(c + 1) * 128], ident[:])
        k.cp(oT[:], PB[1][:])
        for hh in range(2):
            cs = slice(hh * 512, (hh + 1) * 512)
            pa, pb_ = PF[2 * hh], PF[2 * hh + 1]
            for c in range(4):
                k.mm(pa[:], oT[:, c * 128:(c + 1) * 128], Whg[:, c, cs], start=(c == 0), stop=(c == 3))
            for c in range(4):
                k.mm(pb_[:], oT[:, 512 + c * 128:512 + (c + 1) * 128], Wdn[:, c, cs], start=(c == 0), stop=(c == 3))
            k.tt(t1[:], pa[:], sgm.p(hh, (A, hh, A)), ALU.mult)
            k.tt(sgm.p(2 + hh, (A, 2 + hh, A)), pb_[:], sgm.p(2 + hh, (A, 2 + hh, A)), ALU.mult)
            k.tt(mg.p(hh, (A, cs)), t1[:], sgm.p(2 + hh, (A, 2 + hh, A)), ALU.add)
        for c in range(8):
            k.tr(PB[0][:, c * 128:(c + 1) * 128], mg.p(c // 4, (A, slice(c * 128, (c + 1) * 128))), ident[:])
        k.cp(mgT[:], PB[0][:], eng="act")
        for hh in range(2):
            for c in range(8):
                k.mm(PF[4 + hh][:], mgT[:, c * 128:(c + 1) * 128], Wout[:, c, hh * 512:(hh + 1) * 512],
                     start=(c == 0), stop=(c == 7))
        rms_stats(k, [PF[4][:], PF[5][:]], ss2,
                  [junk.p(0, (A, slice(0, 512))), junk.p(1, (A, slice(512, 1024)))], rstd2, lnv2)
        for hh in range(2):
            sl = slice(hh * 512, (hh + 1) * 512)
            k.stt(h_[:, sl], PF[4 + hh][:], rstd2[:, 0:1], gbc[:, sl], ALU.mult, ALU.mult)
        k.tt(h_[:], h_[:], x_[:], ALU.add, eng="pool")
        k.dma("pool", hout_d.rows(t), h_[:], "hout%d" % (t % 2))


_CACHE = {}

WNAMES = ["hg_lower_bounds", "pre_mix_w", "w_in", "hg_norm_w", "conv_w", "dn_a_log", "dn_dt_bias", "dn_norm_w",
          "w_o_hg", "w_o_dn", "w_out", "post_mix_w", "pre_ffn_w", "w_gate_up", "w_down", "post_ffn_w"]


def kernel(**inputs):
    x = np.ascontiguousarray(inputs["x"], dtype=np.float32)
    B, T, _ = x.shape
    if "nc" not in _CACHE:
        _CACHE["nc"] = build(T=T, L=2)
    nc = _CACHE["nc"]
    shared = {n: np.ascontiguousarray(inputs[n], dtype=np.float32) for n in WNAMES}
    in_maps = []
    for b in range(B):
        m = dict(shared)
        m["x"] = x[b]
        in_maps.append(m)
    res = run_bass_kernel_spmd(nc, in_maps, core_ids=list(range(B)))
    return np.stack([r["out"] for r in res.results], axis=0)
```

```python
from contextlib import ExitStack

import numpy as np
import concourse.bass as bass
import concourse.mybir as mybir
from concourse.bass_utils import run_bass_kernel_spmd

F32 = mybir.dt.float32
BF16 = mybir.dt.bfloat16
AF = mybir.ActivationFunctionType
ALU = mybir.AluOpType

D = 1024
DFF = 2816
INW = 6152
EPS = 1e-6
ENGS = ("pe", "act", "dve", "pool", "sp")
CUT = None
SCHED_WINDOW = 256


class Tok:
    __slots__ = ("last_w", "readers")

    def __init__(self):
        self.last_w = None
        self.readers = []


class Op:
    __slots__ = ("eng", "fn", "idx", "deps", "signal", "semval", "clock", "dma_chan", "waits", "barrier",
                 "cost", "start", "finish", "users", "nun", "ready", "done", "chan_snap")

    def __init__(self, eng, fn):
        self.eng = eng
        self.fn = fn
        self.idx = -1
        self.deps = []
        self.signal = False
        self.semval = 0
        self.clock = None
        self.dma_chan = None
        self.waits = []
        self.barrier = False
        self.cost = 100.0
        self.start = 0.0
        self.finish = 0.0
        self.users = None
        self.nun = 0
        self.ready = 0.0
        self.done = False
        self.chan_snap = None


class Prog:
    def __init__(self):
        self.streams = {e: [] for e in ENGS}
        self.chan_count = {}
        self.chan_last = {}
        self.all_ops = []

    def op(self, eng, fn, reads=(), writes=(), dma_chan=None, cost=100.0):
        o = Op(eng, fn)
        o.dma_chan = dma_chan
        o.cost = cost
        deps = []
        for r in reads:
            if r.last_w is not None:
                deps.append((r.last_w, "raw"))
        for w in writes:
            if w.last_w is not None:
                deps.append((w.last_w, "waw"))
            for rd in w.readers:
                deps.append((rd, "war"))
        o.deps = deps
        for r in reads:
            r.readers.append(o)
        for w in writes:
            w.last_w = o
            w.readers = []
        o.idx = len(self.streams[eng])
        self.streams[eng].append(o)
        self.all_ops.append(o)
        if dma_chan is not None:
            c = self.chan_count.get(dma_chan, 0) + 1
            self.chan_count[dma_chan] = c
            o.semval = 16 * c
            self.chan_last[dma_chan] = o
        return o

    def barrier(self):
        m = Op("sp", None)
        m.barrier = True
        m.chan_snap = list(self.chan_last.values())
        self.all_ops.append(m)

    def schedule(self, window=64, lat=120.0):
        segs = [[]]
        marks = []
        for o in self.all_ops:
            if o.barrier:
                marks.append(o)
                segs.append([])
            else:
                segs[-1].append(o)
        new_streams = {e: [] for e in ENGS}
        new_all = []
        free = {e: 0.0 for e in ENGS}
        reorder = ("pe", "act", "dve")
        for si, seg in enumerate(segs):
            st = {e: [] for e in ENGS}
            inseg = set()
            for o in seg:
                st[o.eng].append(o)
                o.users = []
                o.nun = 0
                o.ready = 0.0
                o.done = False
                inseg.add(id(o))
            for o in seg:
                seen = set()
                for d, _ in o.deps:
                    if d is o or id(d) in seen:
                        continue
                    seen.add(id(d))
                    if id(d) in inseg:
                        d.users.append(o)
                        o.nun += 1
            head = {e: 0 for e in ENGS}
            nleft = len(seg)
            order = []
            while nleft:
                best = None
                for e in ENGS:
                    lst = st[e]
                    h = head[e]
                    while h < len(lst) and lst[h].done:
                        h += 1
                    head[e] = h
                    if h >= len(lst):
                        continue
                    if e in reorder:
                        cand = None
                        cstart = None
                        fe = free[e]
                        for j in range(h, min(len(lst), h + window)):
                            o = lst[j]
                            if o.done or o.nun:
                                continue
                            s0 = o.ready if o.ready > fe else fe
                            if cand is None or s0 < cstart - 1e-9:
                                cand, cstart = o, s0
                                if s0 <= fe:
                                    break
                    else:
                        o = lst[h]
                        if o.nun:
                            continue
                        cand = o
                        cstart = o.ready if o.ready > free[e] else free[e]
                    if cand is not None and (best is None or cstart < best[0]):
                        best = (cstart, cand)
                assert best is not None, "scheduler deadlock"
                t0, o = best
                o.start = t0
                o.done = True
                nleft -= 1
                if o.dma_chan is not None:
                    free[o.eng] = t0 + 60.0
                    o.finish = t0 + o.cost
                else:
                    o.finish = t0 + o.cost
                    free[o.eng] = o.finish
                for u in o.users:
                    u.nun -= 1
                    r = o.finish + (lat if u.eng != o.eng or o.dma_chan is not None else 40.0)
                    if r > u.ready:
                        u.ready = r
                order.append(o)
            order.sort(key=lambda q: q.start)
            for o in order:
                o.idx = len(new_streams[o.eng])
                new_streams[o.eng].append(o)
                new_all.append(o)
            if si < len(marks):
                tmax = max(free.values())
                tmax = max([tmax] + [o.finish for o in order]) if order else tmax
                for e in ENGS:
                    free[e] = tmax
                lasts = []
                for e in ENGS:
                    for o in reversed(new_streams[e]):
                        if o.dma_chan is None and not o.barrier:
                            lasts.append(o)
                            break
                for e in ENGS:
                    b = Op(e, None)
                    b.barrier = True
                    b.deps = [(d, "raw") for d in lasts if d.eng != e] + [(d, "raw") for d in marks[si].chan_snap]
                    b.idx = len(new_streams[e])
                    new_streams[e].append(b)
                    new_all.append(b)
        self.streams = new_streams
        self.all_ops = new_all
        self.est_ns = max(free.values())

    def resolve(self):
        eng_clock = {e: ({}, {}) for e in ENGS}
        for o in self.all_ops:
            cc, dc = eng_clock[o.eng]
            waits = []
            for d, kind in o.deps:
                if d is o:
                    continue
                if d.dma_chan is not None:
                    if dc.get(d.dma_chan, 0) >= d.semval:
                        continue
                    waits.append(d)
                    dc[d.dma_chan] = d.semval
                else:
                    if d.eng == o.eng and kind != "raw" and o.eng == "pe":
                        continue
                    if cc.get(d.eng, -1) >= d.idx:
                        continue
                    waits.append(d)
                    d.signal = True
                    cc[d.eng] = d.idx
                dcc, ddc = d.clock
                for k, v in dcc.items():
                    if cc.get(k, -1) < v:
                        cc[k] = v
                for k, v in ddc.items():
                    if dc.get(k, 0) < v:
                        dc[k] = v
            best = {}
            for d in waits:
                if d.dma_chan is not None:
                    key = ("d", d.dma_chan)
                    val = d.semval
                else:
                    key = ("c", d.eng)
                    val = d.idx
                cur = best.get(key)
                if cur is None or val > cur[0]:
                    best[key] = (val, d)
            o.waits = [v[1] for v in best.values()]
            o.clock = (dict(cc), dict(dc))
            if o.dma_chan is None:
                o.clock[0][o.eng] = max(o.clock[0].get(o.eng, -1), o.idx - 1)
        for e in ENGS:
            n = 0
            for o in self.streams[e]:
                if o.dma_chan is None and o.signal:
                    n += 1
                    o.semval = n

    def emit(self, nc, final_waits=()):
        self.schedule(window=SCHED_WINDOW)
        self.resolve()
        with ExitStack() as es:
            sems = {}
            for e in ENGS:
                sems[("c", e)] = es.enter_context(nc.semaphore("s_" + e))
            for ch in self.chan_count:
                sems[("d", ch)] = es.enter_context(nc.semaphore("d_" + str(ch)))
            block = es.enter_context(nc.Block())

            def run_stream(ename):
                def body(eng):
                    for o in self.streams[ename]:
                        for d in o.waits:
                            if d.dma_chan is not None:
                                eng.wait_ge(sems[("d", d.dma_chan)], d.semval)
                            else:
                                eng.wait_ge(sems[("c", d.eng)], d.semval)
                        if o.fn is None:
                            continue
                        ins = o.fn(eng)
                        if o.dma_chan is not None:
                            ins.then_inc(sems[("d", o.dma_chan)], 16)
                        elif o.signal:
                            ins.then_inc(sems[("c", ename)], 1)
                    if ename == "sp":
                        done = {}
                        for d in final_waits:
                            done[d.dma_chan] = max(done.get(d.dma_chan, 0), d.semval)
                        for ch, v in done.items():
                            eng.wait_ge(sems[("d", ch)], v)

                return body

            block.tensor(run_stream("pe"))
            block.scalar(run_stream("act"))
            block.vector(run_stream("dve"))
            block.gpsimd(run_stream("pool"))
            block.sync(run_stream("sp"))


class V:
    __slots__ = ("ap", "toks")

    def __init__(self, ap, toks):
        self.ap = ap
        self.toks = tuple(toks)


class Buf:
    def __init__(self, t, nparts=1):
        self.t = t
        self.toks = [Tok() for _ in range(nparts)]

    def __getitem__(self, idx):
        return V(self.t[idx], self.toks)

    def p(self, i, idx):
        if isinstance(i, int):
            return V(self.t[idx], (self.toks[i],))
        return V(self.t[idx], [self.toks[j] for j in i])


def _fsz(v):
    ap = v.ap if isinstance(v, V) else v
    n = 1
    for d in ap.shape[1:]:
        n *= int(d)
    return n


def _is_f32(v):
    return v.ap.dtype == F32


class K:
    def __init__(self, nc):
        self.nc = nc
        self.P = Prog()

    def sb(self, es, name, shape, dtype, nparts=1):
        self._uid = getattr(self, "_uid", 0) + 1
        return Buf(es.enter_context(self.nc.sbuf_tensor("%s_%d" % (name, self._uid), list(shape), dtype)), nparts)

    def ps(self, es, name, shape, dtype):
        return Buf(es.enter_context(self.nc.psum_tensor(name, list(shape), dtype)), 1)

    @staticmethod
    def _rw(*vs):
        t = []
        for v in vs:
            if isinstance(v, V):
                t.extend(v.toks)
        return t

    @staticmethod
    def _a(v):
        return v.ap if isinstance(v, V) else v

    def mm(self, out, lhsT, rhs, start=True, stop=True):
        n = _fsz(rhs)
        c = 30.0 + n * (1.7 if _is_f32(rhs) else 0.45)
        self.P.op("pe", lambda e: e.matmul(out.ap, lhsT=lhsT.ap, rhs=rhs.ap, start=start, stop=stop),
                  reads=self._rw(lhsT, rhs), writes=out.toks, cost=c)

    def tr(self, out, in_, ident):
        self.P.op("pe", lambda e: e.transpose(out=out.ap, in_=in_.ap, identity=ident.ap),
                  reads=self._rw(in_, ident), writes=out.toks, cost=100.0)

    def act(self, out, in_, func, bias=None, scale=None, accum=None, eng="act"):
        kw = {}
        if bias is not None:
            kw["bias"] = self._a(bias)
        if scale is not None:
            kw["scale"] = self._a(scale)
        if accum is not None:
            kw["accum_out"] = accum.ap
        self.P.op(eng, lambda e: e.activation(out=out.ap, in_=in_.ap, func=func, **kw),
                  reads=self._rw(in_, bias, scale), writes=self._rw(out, accum), cost=220.0 + _fsz(in_) * 1.05)

    def tt(self, out, in0, in1, op, eng="dve"):
        self.P.op(eng, lambda e: e.tensor_tensor(out=out.ap, in0=in0.ap, in1=in1.ap, op=op),
                  reads=self._rw(in0, in1), writes=out.toks,
                  cost=(100.0 + _fsz(out) * 1.05) * (2.0 if eng == "pool" else 1.0))

    def ts(self, out, in0, s1, s2, op0, op1=None, eng="dve", accum=None):
        kw = {}
        if op1 is not None:
            kw["op1"] = op1
        if accum is not None:
            kw["accum_out"] = accum.ap
        self.P.op(eng, lambda e: e.tensor_scalar(out=out.ap, in0=in0.ap, scalar1=self._a(s1), scalar2=self._a(s2),
                                                 op0=op0, **kw),
                  reads=self._rw(in0, s1, s2), writes=self._rw(out, accum), cost=100.0 + _fsz(out) * 0.8)

    def stt(self, out, in0, scalar, in1, op0, op1, eng="dve"):
        self.P.op(eng, lambda e: e.scalar_tensor_tensor(out=out.ap, in0=in0.ap, scalar=self._a(scalar), in1=in1.ap,
                                                        op0=op0, op1=op1),
                  reads=self._rw(in0, scalar, in1), writes=out.toks, cost=100.0 + _fsz(out) * 1.05)

    def cp(self, out, in_, eng="dve"):
        if eng == "act":
            self.P.op("act", lambda e: e.copy(out=out.ap, in_=in_.ap), reads=in_.toks, writes=out.toks,
                      cost=220.0 + _fsz(out) * 1.05)
        else:
            self.P.op(eng, lambda e: e.tensor_copy(out=out.ap, in_=in_.ap), reads=in_.toks, writes=out.toks,
                      cost=100.0 + _fsz(out) * 0.8)

    def recip(self, out, in_):
        self.P.op("dve", lambda e: e.reciprocal(out=out.ap, in_=in_.ap), reads=in_.toks, writes=out.toks)

    def memset(self, out, val, eng="pool"):
        self.P.op(eng, lambda e: e.memset(out.ap, val), writes=out.toks)

    def asel(self, out, in_, pattern, cmp, fill, base, cm):
        self.P.op("pool", lambda e: e.affine_select(out=out.ap, in_=in_.ap, pattern=pattern, compare_op=cmp,
                                                    fill=fill, base=base, channel_multiplier=cm),
                  reads=in_.toks, writes=out.toks)

    def dma(self, q, out, in_, chan, noncontig=False):
        kw = {"allow_slow_non_contiguous": True} if noncontig else {}
        ap = out.ap if isinstance(out, V) else out
        nbytes = 128.0 * 4
        try:
            nbytes = float(ap.nbytes())
        except Exception:
            pass
        return self.P.op(q, lambda e: e.dma_start(out=self._a(out), in_=self._a(in_), **kw),
                         reads=self._rw(in_), writes=self._rw(out), dma_chan=chan, cost=2500.0 + nbytes / 150.0)


class DramT:
    def __init__(self, ap, ntiles):
        self.ap = ap
        self.toks = [Tok() for _ in range(max(1, ntiles))]

    def rows(self, t, n=128):
        return V(self.ap[t * 128:t * 128 + n], (self.toks[t],))


def build(T=8192, L=2, phases=("A", "B", "C"), dbg=()):
    NT = T // 128
    nc = bass.Bass("TRN2", target_bir_lowering=False)
    k = K(nc)
    P = k.P

    def din(name, shape):
        return nc.dram_tensor(name, list(shape), F32, kind="ExternalInput").ap()

    x_d = DramT(din("x", [T, D]), NT)
    hglb_d = din("hg_lower_bounds", [L, 512])
    pre_mix_d = din("pre_mix_w", [L, D])
    w_in_d = din("w_in", [L, D, INW])
    hg_norm_d = din("hg_norm_w", [L, 128])
    conv_d = din("conv_w", [L, 4, 1536])
    alog_d = din("dn_a_log", [L, 4])
    dtb_d = din("dn_dt_bias", [L, 4])
    dn_norm_d = din("dn_norm_w", [L, 128])
    w_o_hg_d = din("w_o_hg", [L, 512, D])
    w_o_dn_d = din("w_o_dn", [L, 512, D])
    w_out_d = din("w_out", [L, D, D])
    post_mix_d = din("post_mix_w", [L, D])
    pre_ffn_d = din("pre_ffn_w", [L, D])
    w_gu_d = din("w_gate_up", [L, D, 2 * DFF])
    w_dn_d = din("w_down", [L, DFF, D])
    post_ffn_d = din("post_ffn_w", [L, D])
    out_d = DramT(nc.dram_tensor("out", [T, D], F32, kind="ExternalOutput").ap(), NT)
    skind = "ExternalOutput" if dbg else "Internal"
    hbuf_d = DramT(nc.dram_tensor("hbuf", [T, D], F32, kind=skind).ap(), NT)
    x1_d = DramT(nc.dram_tensor("x1buf", [T, D], F32, kind=skind).ap(), NT)
    oab_d = DramT(nc.dram_tensor("oab", [T, D], BF16, kind=skind).ap(), NT)

    final_ops = []
    with ExitStack() as pes:
        PF = [k.ps(pes, "pf%d" % i, [128, 512], F32) for i in range(6)]
        PB = [k.ps(pes, "pb%d" % i, [128, 1024], BF16) for i in range(2)]
        ident_f = k.sb(pes, "ident_f", [128, 128], F32)
        ident = k.sb(pes, "ident", [128, 128], BF16)
        k.memset(ident_f[:], 1.0)
        k.asel(ident_f[:], ident_f[:], [[-1, 128]], ALU.is_equal, 0.0, 0, 1)
        k.cp(ident[:], ident_f[:])

        if "A" in phases:
            mk = make_masks(k, pes)
        for l in range(L):
            xin = x_d if l == 0 else x1_d
            xout = out_d if l == L - 1 else x1_d
            if "A" in phases:
                with ExitStack() as es:
                    phase_a(k, es, l, NT, PF, PB, ident, ident_f, xin, oab_d, hglb_d, pre_mix_d, w_in_d, hg_norm_d,
                            conv_d, alog_d, dtb_d, dn_norm_d, mk)
                P.barrier()
            if "B" in phases:
                with ExitStack() as es:
                    phase_b(k, es, l, NT, PF, PB, ident, ident_f, xin, oab_d, hbuf_d, pre_mix_d, w_in_d, w_o_hg_d, w_o_dn_d,
                            w_out_d, post_mix_d)
                P.barrier()
            if "C" in phases:
                with ExitStack() as es:
                    phase_c(k, es, l, NT, PF, PB, ident, ident_f, hbuf_d if ("A" in phases or "B" in phases) else xin,
                            xout, pre_ffn_d, w_gu_d, w_dn_d, post_ffn_d, final_ops, l == L - 1)
                P.barrier()
        P.emit(nc, final_waits=final_ops)
    return nc


def rms_stats(k, src_list, ss, tmp_junk, rstd, lnv):
    n = len(src_list)
    for i, s in enumerate(src_list):
        k.act(tmp_junk[i], s, AF.Square, scale=float(D ** -0.5), accum=ss[:, i:i + 1])
    if n == 2:
        k.tt(ss[:, 0:1], ss[:, 0:1], ss[:, 1:2], ALU.add)
    k.act(lnv[:, 0:1], ss[:, 0:1], AF.Ln, bias=EPS)
    k.act(rstd[:, 0:1], lnv[:, 0:1], AF.Exp, scale=-0.5)


def load_weight_cast(k, dst, src_ap, chan_prefix, counter):
    ch = "%s%d" % (chan_prefix, counter[0])
    counter[0] += 1
    return k.dma("pool", dst, src_ap, ch)


def phase_c(k, es, l, NT, PF, PB, ident, ident_f, hin_d, xout_d, pre_ffn_d, w_gu_d, w_dn_d, post_ffn_d, final_ops, is_last):
    nc = k.nc
    NB = 6
    bw = [512] * 5 + [256]
    boff = [512 * i for i in range(6)]
    wg = [k.sb(es, "wg%d" % b, [128, 8, bw[b]], BF16) for b in range(NB)]
    wu = [k.sb(es, "wu%d" % b, [128, 8, bw[b]], BF16) for b in range(NB)]
    wd = [k.sb(es, "wd%d" % h, [128, 11, D], BF16) for h in range(2)]
    gcol = load_gcol(k, es, "gcol", pre_ffn_d[l], PF[0], ident_f, "cst0")
    gbc = k.sb(es, "gbc", [128, D], F32)
    cnt = [0]
    k.dma("sp", gbc[:], post_ffn_d[l:l + 1, :].partition_broadcast(128), "cst1")
    wsrc = w_gu_d[l].rearrange("(k p) n -> p k n", p=128)
    for b in range(NB):
        for (wt, off) in ((wg[b], boff[b]), (wu[b], DFF + boff[b])):
            load_weight_cast(k, wt[:], wsrc[:, :, off:off + bw[b]], "w", cnt)
    dsrc = w_dn_d[l].rearrange("(k p) n -> p k n", p=128)
    for h in range(2):
        load_weight_cast(k, wd[h][:], dsrc[:, 11 * h:11 * h + 11, :], "w", cnt)
    for b in range(NB):
        for wt in (wg[b], wu[b]):
            k.tt(wt[:], wt[:], V(gcol.t[:, :].unsqueeze(2).broadcast_to([128, 8, bw[b]]), gcol.toks), ALU.mult)

    ht = [k.sb(es, "ht%d" % i, [128, D], F32) for i in range(2)]
    ot = [k.sb(es, "ot%d" % i, [128, D], F32) for i in range(2)]
    def two(name, shape, dt, nparts=1):
        return [k.sb(es, "%s%d" % (name, i), shape, dt, nparts=nparts) for i in range(2)]
    junkx = two("junk", [128, D], BF16, 2)
    ssx = two("ss", [128, 2], F32)
    lnvx = two("lnv", [128, 1], F32)
    rstdx = two("rstd", [128, 1], F32)
    ss2x = two("ss2", [128, 2], F32)
    lnv2x = two("lnv2", [128, 1], F32)
    rstd2x = two("rstd2", [128, 1], F32)
    vbx = two("vb", [128, D], BF16)
    vTx = two("vT", [128, D], BF16)
    sg = [k.sb(es, "sg%d" % i, [128, 512], F32) for i in range(2)]
    actbx = two("actb", [128, DFF], BF16, NB)
    actTx = two("actT", [128, DFF], BF16, 3)

    for t in range(NT):
        h = ht[t % 2]
        o = ot[t % 2]
        i2 = t % 2
        junk, ss, lnv, rstd, ss2, lnv2, rstd2 = junkx[i2], ssx[i2], lnvx[i2], rstdx[i2], ss2x[i2], lnv2x[i2], rstd2x[i2]
        vb, vT, actb, actT = vbx[i2], vTx[i2], actbx[i2], actTx[i2]
        k.dma("sp", h[:], hin_d.rows(t), "hin%d" % (t % 2))
        rms_stats(k, [h[:]], ss, [junk[:]], rstd, lnv)
        k.act(vb[:], h[:], AF.Copy, scale=rstd[:, 0:1])
        for c in range(8):
            k.tr(PB[0][:, c * 128:(c + 1) * 128], vb[:, c * 128:(c + 1) * 128], ident[:])
        k.cp(vT[:], PB[0][:])
        for b in range(NB):
            pg = PF[(2 * b) % 4]
            pu = PF[(2 * b + 1) % 4]
            n = bw[b]
            for c in range(8):
                k.mm(pg[:, 0:n], vT[:, c * 128:(c + 1) * 128], wg[b][:, c, :], start=(c == 0), stop=(c == 7))
            for c in range(8):
                k.mm(pu[:, 0:n], vT[:, c * 128:(c + 1) * 128], wu[b][:, c, :], start=(c == 0), stop=(c == 7))
            s = sg[b % 2]
            k.act(s[:, 0:n], pg[:, 0:n], AF.Silu)
            k.tt(actb.p(b, (slice(None), slice(boff[b], boff[b] + n))), s[:, 0:n], pu[:, 0:n], ALU.mult)
        for g in range(3):
            c0 = g * 8
            c1 = min(22, c0 + 8)
            pb = PB[(g + 1) % 2]
            for c in range(c0, c1):
                blk = (c * 128) // 512
                k.tr(pb[:, (c - c0) * 128:(c - c0 + 1) * 128],
                     actb.p(blk, (slice(None), slice(c * 128, (c + 1) * 128))), ident[:])
            k.cp(actT.p(g, (slice(None), slice(c0 * 128, c1 * 128))), pb[:, 0:(c1 - c0) * 128],
                 eng=("act" if g == 1 else "dve"))
        for hh in range(2):
            pf = PF[4 + hh]
            for c in range(22):
                k.mm(pf[:], actT.p(c // 8, (slice(None), slice(c * 128, (c + 1) * 128))),
                     wd[c // 11][:, c % 11, hh * 512:(hh + 1) * 512], start=(c == 0), stop=(c == 21))
        rms_stats(k, [PF[4][:], PF[5][:]], ss2,
                  [junk.p(0, (slice(None), slice(0, 512))), junk.p(1, (slice(None), slice(512, 1024)))], rstd2, lnv2)
        for hh in range(2):
            sl = slice(hh * 512, (hh + 1) * 512)
            k.stt(o[:, sl], PF[4 + hh][:], rstd2[:, 0:1], gbc[:, sl], ALU.mult, ALU.mult)
        k.tt(o[:], o[:], h[:], ALU.add, eng="pool")
        d = k.dma("pool", xout_d.rows(t), o[:], "oout%d" % (t % 2))
        if is_last:
            final_ops.append(d)


def bc(buf, idx, shape):
    ap = buf.t[idx]
    return V(ap.unsqueeze(len(ap.shape)).broadcast_to(list(shape)), buf.toks)


def bc_mid(buf, idx, shape):
    ap = buf.t[idx]
    return V(ap.unsqueeze(1).broadcast_to(list(shape)), buf.toks)


def load_wcols(k, es, name, src_l, c0, c1, cnt, kch=8):
    wt = k.sb(es, name, [128, kch, c1 - c0], BF16)
    load_weight_cast(k, wt[:], src_l.rearrange("(k p) n -> p k n", p=128)[:, :, c0:c1], "w", cnt)
    return wt


def load_gcol(k, es, name, src_row_ap, PFb, ident_f, chan):
    rows = k.sb(es, name + "_r", [8, 128], F32)
    gcol = k.sb(es, name, [128, 8], F32)
    k.dma("sp", rows[:], src_row_ap.rearrange("(k p) -> k p", p=128), chan)
    k.tr(PFb[:, 0:8], rows[:], ident_f[0:8, 0:8])
    k.cp(gcol[:], PFb[:, 0:8])
    return gcol


def fold_gain(k, wt, gcol, kch, n):
    k.tt(wt[:], wt[:], V(gcol.t[:, :].unsqueeze(2).broadcast_to([128, kch, n]), gcol.toks), ALU.mult)


def x_to_uT(k, xin_d, t, xt, junk, ss, lnv, rstd, ub, uT, PBk, ident, chan):
    k.dma("sp", xt[:], xin_d.rows(t), chan)
    rms_stats(k, [xt[:]], ss, [junk[:]], rstd, lnv)
    k.act(ub[:], xt[:], AF.Copy, scale=rstd[:, 0:1])
    for c in range(8):
        k.tr(PBk[:, c * 128:(c + 1) * 128], ub[:, c * 128:(c + 1) * 128], ident[:])
    k.cp(uT[:], PBk[:])


def proj_tm(k, pf, uT, wt, n=512, c0=0):
    for c in range(8):
        k.mm(pf[:, 0:n], uT[:, c * 128:(c + 1) * 128], wt[:, c, c0:c0 + n], start=(c == 0), stop=(c == 7))


def make_masks(k, es):
    m = {}
    M1 = k.sb(es, "M1", [128, 128], F32)
    M2 = k.sb(es, "M2", [128, 128], F32)
    NS = k.sb(es, "NEGS", [128, 128], F32)
    NCT = k.sb(es, "NEGCT", [128, 128], F32)
    cind = k.sb(es, "cind", [128, 2], F32)
    ones = k.sb(es, "ones_f", [128, 128], F32)
    k.memset(M1[:], 1.0)
    k.asel(M1[:], M1[:], [[1, 128]], ALU.is_ge, 0.0, 0, -1)
    k.memset(M1[0:64, 64:128], 0.0)
    k.memset(M2[:], 1.0)
    k.asel(M2[:], M2[:], [[-1, 128]], ALU.is_gt, 0.0, 0, 1)
    k.memset(M2[64:128, 0:64], 0.0)
    k.ts(NS[:], M2[:], -1.0, 30000.0, ALU.add, ALU.mult)
    k.ts(NCT[:], M1[:], -1.0, 30000.0, ALU.add, ALU.mult)
    k.memset(cind[:], 0.0)
    k.memset(cind[0:64, 0:1], 1.0)
    k.memset(cind[64:128, 1:2], 1.0)
    k.memset(ones[:], 1.0)
    return dict(M1=M1, M2=M2, NS=NS, NCT=NCT, cind=cind, ones=ones)


def head_norm_gate(k, po, ss4, ln4, r4, osb, gsw, dst):
    for h in range(4):
        k.act(osb[:, h * 128:(h + 1) * 128], po[:, h * 128:(h + 1) * 128], AF.Square, scale=float(128 ** -0.5),
              accum=ss4[:, h:h + 1])
    k.act(ln4[:], ss4[:], AF.Ln, bias=EPS)
    k.act(r4[:], ln4[:], AF.Exp, scale=-0.5)
    k.tt(V(osb.t[:, :].rearrange("p (h d) -> p h d", h=4), osb.toks),
         V(po.t[:, :].rearrange("p (h d) -> p h d", h=4), po.toks), bc(r4, (slice(None), slice(None)), [128, 4, 128]),
         ALU.mult)
    k.tt(dst, osb[:], gsw[:], ALU.mult)


def phase_a(k, es, l, NT, PF, PB, ident, ident_f, xin_d, oab_d, hglb_d, pre_mix_d, w_in_d, hg_norm_d, conv_d,
            alog_d, dtb_d, dn_norm_d, mk):
    nc = k.nc
    M1, M2, NS, NCT, cind, ones = mk["M1"], mk["M2"], mk["NS"], mk["NCT"], mk["cind"], mk["ones"]
    cnt = [0]
    win = w_in_d[l]
    Wq = load_wcols(k, es, "Wq", win, 0, 512, cnt)
    Wf = load_wcols(k, es, "Wf", win, 512, 1024, cnt)
    Wi = load_wcols(k, es, "Wi", win, 1024, 1536, cnt)
    Wg = load_wcols(k, es, "Wg", win, 1536, 2048, cnt)
    Wc = [load_wcols(k, es, "Wc%d" % i, win, 2048 + 512 * i, 2560 + 512 * i, cnt) for i in range(3)]
    Wdg = load_wcols(k, es, "Wdg", win, 3584, 4096, cnt)
    Wab = load_wcols(k, es, "Wab", win, 4096, 4104, cnt)
    gcol = load_gcol(k, es, "gcolA", pre_mix_d[l], PF[0], ident_f, "cst0")
    for wt in (Wq, Wf, Wi, Wg, Wc[0], Wc[1], Wc[2], Wdg):
        fold_gain(k, wt, gcol, 8, 512)
    fold_gain(k, Wab, gcol, 8, 8)
    cw = k.sb(es, "cw", [128, 12, 4], F32)
    cwr = k.sb(es, "cwr", [4, 1536], F32)
    k.dma("sp", cwr[:], conv_d[l], "cst1")
    for c in range(12):
        k.tr(PF[1][:, 4 * c:4 * c + 4], cwr[:, c * 128:(c + 1) * 128], ident_f[0:4, 0:4])
    k.cp(V(cw.t[:, :, :].rearrange("p c j -> p (c j)"), cw.toks), PF[1][:, 0:48])
    dtb = k.sb(es, "dtb", [128, 4], F32)
    k.dma("sp", dtb[:], dtb_d[l:l + 1, :].partition_broadcast(128), "cst2")
    negA = k.sb(es, "negA", [128, 4], F32)
    k.dma("sp", negA[:], alog_d[l:l + 1, :].partition_broadcast(128), "cst3")
    k.act(negA[:], negA[:], AF.Exp)
    k.ts(negA[:], negA[:], -1.0, None, ALU.mult)
    hgn = k.sb(es, "hgn", [128, 128], F32)
    k.dma("sp", hgn[:], hg_norm_d[l:l + 1, :].partition_broadcast(128), "cst4")
    dnn = k.sb(es, "dnn", [128, 128], F32)
    k.dma("sp", dnn[:], dn_norm_d[l:l + 1, :].partition_broadcast(128), "cst5")
    omlb = k.sb(es, "omlb", [128, 512], F32)
    if l == 0:
        k.memset(omlb[:], 1.0)
    else:
        lb0 = k.sb(es, "lb0", [128, 512], F32)
        k.dma("sp", omlb[:], hglb_d[1:2, :].partition_broadcast(128), "cst6")
        k.dma("sp", lb0[:], hglb_d[0:1, :].partition_broadcast(128), "cst7")
        k.tt(omlb[:], omlb[:], lb0[:], ALU.subtract)
        k.act(omlb[:], omlb[:], AF.Exp)
        k.ts(omlb[:], omlb[:], 1.0, None, ALU.add)
        k.recip(omlb[:], omlb[:])
    Sh = k.sb(es, "Sh", [128, 4, 128], F32, nparts=4)
    Shb = k.sb(es, "Shb", [128, 4, 128], BF16, nparts=4)
    Sd = k.sb(es, "Sd", [128, 4, 128], F32, nparts=4)
    Sdb = k.sb(es, "Sdb", [128, 4, 128], BF16, nparts=4)
    for S_ in (Sh, Shb, Sd, Sdb):
        k.memset(S_[:], 0.0)
    cvin = k.sb(es, "cvin", [128, 12, 131], BF16)
    k.memset(cvin[:], 0.0)
    diagw = k.sb(es, "diagw", [128, 12, 4, 128], BF16)
    for c in range(12):
        for j in range(4):
            k.ts(diagw[:, c, j, :], ident_f[:], cw[:, c, j:j + 1], None, ALU.mult)
    xt = [k.sb(es, "xtA%d" % i, [128, D], F32) for i in range(2)]
    junk = k.sb(es, "junkA", [128, D], BF16)
    ss = k.sb(es, "ssA", [128, 2], F32)
    lnv = k.sb(es, "lnvA", [128, 1], F32)
    rstd = k.sb(es, "rstdA", [128, 1], F32)
    ub2 = [k.sb(es, "ubA%d" % i, [128, D], BF16) for i in range(2)]
    uT2 = [k.sb(es, "uTA%d" % i, [128, D], BF16) for i in range(2)]
    kf = k.sb(es, "kf", [128, 512], F32)
    lf = k.sb(es, "lf", [128, 512], F32)
    eb = k.sb(es, "eb", [128, 512], F32)
    enb = k.sb(es, "enb", [128, 512], F32)
    esf = k.sb(es, "esf", [128, 512], F32)
    qe = k.sb(es, "qe", [128, 512], BF16)
    ke = k.sb(es, "ke", [128, 512], BF16)
    kdh = k.sb(es, "kdh", [128, 512], BF16)
    vh = k.sb(es, "vh", [128, 512], BF16)
    gsw = k.sb(es, "gsw", [128, 512], F32)
    qkT = k.sb(es, "qkT", [128, 1024], BF16)
    attnT = k.sb(es, "attnT", [128, 4, 128], BF16)
    dlh = k.sb(es, "dlh", [128, 8], F32)
    osb = k.sb(es, "osb", [128, 512], F32)
    ss4 = k.sb(es, "ss4", [128, 4], F32)
    ln4 = k.sb(es, "ln4", [128, 4], F32)
    r4 = k.sb(es, "r4", [128, 4], F32)
    oab = [k.sb(es, "oabt%d" % i, [128, D], BF16, nparts=2) for i in range(2)]
    qkvF = k.sb(es, "qkvF", [128, 12, 128], BF16)
    qkvT = k.sb(es, "qkvT", [128, 1536], BF16)
    ss8 = k.sb(es, "ss8", [128, 8], F32)
    ln8 = k.sb(es, "ln8", [128, 8], F32)
    r8 = k.sb(es, "r8", [128, 8], F32)
    sq4 = k.sb(es, "sq4", [128, 4], F32)
    beta = k.sb(es, "beta", [128, 4], F32)
    ab8 = k.sb(es, "ab8", [128, 8], F32)
    gt = k.sb(es, "gt", [128, 4], F32)
    egc = k.sb(es, "egc", [128, 4], F32)
    egs = k.sb(es, "egs", [128, 4], F32)
    bg = k.sb(es, "bg", [128, 4], F32)
    gmask = k.sb(es, "gmask", [128, 4, 2], F32)
    dld = k.sb(es, "dld", [128, 8], F32)
    kn = k.sb(es, "kn", [128, 512], BF16)
    kbg = k.sb(es, "kbg", [128, 512], BF16)
    kdd = k.sb(es, "kdd", [128, 512], BF16)
    vbd = k.sb(es, "vbd", [128, 512], BF16)
    qn = k.sb(es, "qn", [128, 512], BF16)
    qg = k.sb(es, "qg", [128, 512], BF16)
    kqT = k.sb(es, "kqT", [128, 8, 128], BF16)
    qgT = k.sb(es, "qgT", [128, 4, 128], BF16)
    Mg = k.sb(es, "Mg", [128, 4, 128], F32, nparts=4)
    Ls = k.sb(es, "Ls", [128, 4, 128], F32)
    LT = k.sb(es, "LT", [128, 4, 128], F32)
    Am = [k.sb(es, "Am%d" % i, [128, 4, 128], BF16) for i in range(2)]
    AmT = [k.sb(es, "AmT%d" % i, [128, 4, 128], BF16) for i in range(2)]
    TT = [k.sb(es, "TT%d" % i, [128, 4, 128], BF16) for i in range(2)]
    nwT = k.sb(es, "nwT", [128, 4, 128], BF16)
    aqk = k.sb(es, "aqk", [128, 4, 128], BF16)
    vn = k.sb(es, "vn", [128, 4, 128], BF16, nparts=4)
    gsd2 = [k.sb(es, "gsd%d" % i, [128, 512], F32) for i in range(2)]
    osd = k.sb(es, "osd", [128, 512], F32)
    A = slice(None)

    def v4(b):
        return V(b.t[:, :].rearrange("p (h d) -> p h d", h=4), b.toks)

    for t in range(NT):
        x_ = xt[t % 2]
        ob = oab[t % 2]
        ub = ub2[t % 2]
        uT = uT2[t % 2]
        gsd = gsd2[t % 2]
        x_to_uT(k, xin_d, t, x_, junk, ss, lnv, rstd, ub, uT, PB[0], ident, "xin%d" % (t % 2))
        proj_tm(k, PF[0], uT, Wf)
        k.act(kf[:], PF[0][:], AF.Sigmoid, scale=-1.0)
        k.tt(kf[:], kf[:], omlb[:], ALU.mult)
        k.act(lf[:], kf[:], AF.Ln, scale=-1.0, bias=1.0)
        k.mm(PF[0][:], M1[:], lf[:])
        k.mm(PF[1][:], M2[:], lf[:])
        for h in range(4):
            k.mm(PF[2][:, 2 * h:2 * h + 2], lf[:, h * 128:(h + 1) * 128], cind[:])
        k.act(eb[:], PF[0][:], AF.Exp)
        k.act(enb[:], PF[0][:], AF.Exp, scale=-1.0)
        k.act(esf[:], PF[1][:], AF.Exp)
        k.act(dlh[:], PF[2][:, 0:8], AF.Exp)
        proj_tm(k, PF[3], uT, Wq)
        k.tt(qe[:], PF[3][:], eb[:], ALU.mult)
        k.tt(ke[:], kf[:], enb[:], ALU.mult)
        k.tt(kdh[:], kf[:], esf[:], ALU.mult)
        proj_tm(k, PF[4], uT, Wi)
        k.cp(vh[:], PF[4][:], eng="act")
        proj_tm(k, PF[5], uT, Wg)
        k.act(gsw[:], PF[5][:], AF.Silu)
        k.tt(v4(gsw), v4(gsw), bc_mid(hgn, (A, A), [128, 4, 128]), ALU.mult)
        proj_tm(k, PF[5], uT, Wdg)
        k.act(gsd[:], PF[5][:], AF.Silu)
        k.tt(v4(gsd), v4(gsd), bc_mid(dnn, (A, A), [128, 4, 128]), ALU.mult)
        for h in range(4):
            k.tr(PB[1][:, h * 128:(h + 1) * 128], qe[:, h * 128:(h + 1) * 128], ident[:])
            k.tr(PB[1][:, 512 + h * 128:512 + (h + 1) * 128], ke[:, h * 128:(h + 1) * 128], ident[:])
        k.cp(qkT[:], PB[1][:])
        for h in range(4):
            k.mm(PF[0][:, h * 128:(h + 1) * 128], qkT[:, 512 + h * 128:512 + (h + 1) * 128],
                 qkT[:, h * 128:(h + 1) * 128])
        k.tt(attnT[:], v4(PF[0]), bc_mid(M1, (A, A), [128, 4, 128]), ALU.mult)
        po = PF[1]
        for h in range(4):
            hs = slice(h * 128, (h + 1) * 128)
            k.mm(po[:, hs], attnT[:, h, :], vh[:, hs], start=True, stop=False)
            for c in range(2):
                cs = slice(64 * c, 64 * c + 64)
                k.mm(po[cs, hs], qkT[:, h * 128 + 64 * c:h * 128 + 64 * c + 64], Shb.p(h, (A, h, A)),
                     start=False, stop=True)
                k.mm(PF[2 + (h % 2)][:, 0:128], kdh[cs, hs], vh[cs, hs])
                k.stt(Sh.p(h, (A, h, A)), Sh.p(h, (A, h, A)), dlh[:, 2 * h + c:2 * h + c + 1],
                      PF[2 + (h % 2)][:, 0:128], ALU.mult, ALU.add)
                k.cp(Shb.p(h, (A, h, A)), Sh.p(h, (A, h, A)))
        head_norm_gate(k, po, ss4, ln4, r4, osb, gsw, ob.p(0, (A, slice(0, 512))))

        if CUT == "hg":
            k.dma("sp", oab_d.rows(t), ob[:], "oabw%d" % (t % 2))
            continue
        k.cp(cvin[:, :, 0:3], cvin[:, :, 128:131])
        for i in range(3):
            pf = PF[2 + i]
            for cc in range(4):
                for c in range(8):
                    k.mm(pf[:, cc * 128:(cc + 1) * 128], Wc[i][:, c, cc * 128:(cc + 1) * 128],
                         uT[:, c * 128:(c + 1) * 128], start=(c == 0), stop=(c == 7))
            k.cp(cvin[:, 4 * i:4 * i + 4, 3:131], v4(pf), eng="act")
        for i in range(3):
            pf = PF[2 + i]
            for cc in range(4):
                c = 4 * i + cc
                for j in range(4):
                    k.mm(pf[:, cc * 128:(cc + 1) * 128], diagw[:, c, j, :], cvin[:, c, j:j + 128],
                         start=(j == 0), stop=(j == 3))
            k.act(qkvF[:, 4 * i:4 * i + 4, :], v4(pf), AF.Silu)
        for c in range(8):
            k.tr(PB[0][:, c * 128:(c + 1) * 128], qkvF[:, c, :], ident[:])
        k.cp(qkvT[:, 0:1024], PB[0][:])
        for c in range(8, 12):
            k.tr(PB[1][:, (c - 8) * 128:(c - 7) * 128], qkvF[:, c, :], ident[:])
        k.cp(qkvT[:, 1024:1536], PB[1][:, 0:512], eng="act")
        if CUT == "conv":
            k.cp(ob.p(1, (A, slice(512, 1024))), qkvT[:, 0:512])
            k.dma("sp", oab_d.rows(t), ob[:], "oabw%d" % (t % 2))
            continue
        for j in range(8):
            k.act(osd[:, (j % 4) * 128:(j % 4 + 1) * 128], qkvT[:, j * 128:(j + 1) * 128], AF.Square,
                  accum=ss8[:, j:j + 1])
        k.act(ln8[:], ss8[:], AF.Ln, bias=EPS)
        k.act(r8[:], ln8[:], AF.Exp, scale=-0.5)
        if CUT == "n1":
            k.dma("sp", oab_d.rows(t), ob[:], "oabw%d" % (t % 2))
            continue
        pab = PF[5]
        for c in range(8):
            k.mm(pab[:, 0:8], uT[:, c * 128:(c + 1) * 128], Wab[:, c, :], start=(c == 0), stop=(c == 7))
        k.cp(ab8[:], pab[:, 0:8], eng="act")
        k.act(beta[:], ab8[:, 4:8], AF.Sigmoid)
        k.tt(gt[:], ab8[:, 0:4], dtb[:], ALU.add)
        k.act(gt[:], gt[:], AF.Exp)
        k.act(gt[:], gt[:], AF.Ln, bias=1.0)
        k.tt(gt[:], gt[:], negA[:], ALU.mult)
        if CUT == "n2":
            k.dma("sp", oab_d.rows(t), ob[:], "oabw%d" % (t % 2))
            continue
        k.mm(pab[:, 16:20], M1[:], gt[:])
        k.mm(pab[:, 24:28], M2[:], gt[:])
        k.tt(gmask[:], bc(gt, (A, A), [128, 4, 2]), bc_mid(cind, (A, A), [128, 4, 2]), ALU.mult)
        k.mm(pab[:, 32:40], ones[:], V(gmask.t[:, :, :].rearrange("p h c -> p (h c)"), gmask.toks))
        k.act(egc[:], pab[:, 16:20], AF.Exp)
        k.act(egs[:], pab[:, 24:28], AF.Exp)
        k.act(dld[:], pab[:, 32:40], AF.Exp)
        if CUT == "n3":
            k.dma("sp", oab_d.rows(t), ob[:], "oabw%d" % (t % 2))
            continue
        k.ts(sq4[:], r8[:, 0:4], float(128 ** -0.5), None, ALU.mult)
        k.tt(bg[:], beta[:], egc[:], ALU.mult)

        def hv(b, lo):
            return V(b.t[:, lo:lo + 512].rearrange("p (h d) -> p h d", h=4), b.toks)

        sh3 = [128, 4, 128]
        for h in range(4):
            hs = slice(h * 128, (h + 1) * 128)
            k.act(kn[:, hs], qkvT[:, 512 + h * 128:512 + (h + 1) * 128], AF.Copy, scale=r8[:, 4 + h:5 + h])
            k.act(qn[:, hs], qkvT[:, h * 128:(h + 1) * 128], AF.Copy, scale=sq4[:, h:h + 1])
            k.act(vbd[:, hs], qkvT[:, 1024 + h * 128:1024 + (h + 1) * 128], AF.Copy, scale=beta[:, h:h + 1])
        for h in range(4):
            hs = slice(h * 128, (h + 1) * 128)
            k.ts(kbg[:, hs], kn[:, hs], bg[:, h:h + 1], None, ALU.mult)
            k.ts(kdd[:, hs], kn[:, hs], egs[:, h:h + 1], None, ALU.mult)
            k.ts(qg[:, hs], qn[:, hs], egc[:, h:h + 1], None, ALU.mult)
        if CUT == "n4":
            k.dma("sp", oab_d.rows(t), ob[:], "oabw%d" % (t % 2))
            continue
        for h in range(4):
            hs = slice(h * 128, (h + 1) * 128)
            k.tr(PB[0][:, hs], kn[:, hs], ident[:])
            k.tr(PB[0][:, 512 + h * 128:512 + (h + 1) * 128], qn[:, hs], ident[:])
            k.tr(PB[1][:, hs], qg[:, hs], ident[:])
        k.cp(V(kqT.t[:, :, :].rearrange("p h d -> p (h d)"), kqT.toks), PB[0][:])
        k.cp(V(qgT.t[:, :, :].rearrange("p h d -> p (h d)"), qgT.toks), PB[1][:, 0:512], eng="act")
        if CUT == "prep":
            k.cp(ob.p(1, (A, slice(512, 1024))), kbg[:])
            k.dma("sp", oab_d.rows(t), ob[:], "oabw%d" % (t % 2))
            continue
        for h in range(4):
            hs = slice(h * 128, (h + 1) * 128)
            k.ts(Mg.p(h, (A, h, A)), M1[:], gt[:, h:h + 1], None, ALU.mult)
            k.mm(PF[2][:, hs], Mg.p(h, (A, h, A)), M2[:], start=True, stop=False)
            k.mm(PF[2][:, hs], ident_f[:], NS[:], start=False, stop=True)
            k.mm(PF[3][:, hs], M2[:], Mg.p(h, (A, h, A)), start=True, stop=False)
            k.mm(PF[3][:, hs], ident_f[:], NCT[:], start=False, stop=True)
            k.mm(PF[0][:, hs], kqT[:, h, :], kqT[:, h, :])
            k.mm(PF[4][:, hs], kqT[:, h, :], kqT[:, 4 + h, :])
        k.act(Ls[:], v4(PF[2]), AF.Exp)
        k.act(LT[:], v4(PF[3]), AF.Exp)
        for h in range(4):
            hs = slice(h * 128, (h + 1) * 128)
            k.stt(Am[0][:, h, :], PF[0][:, hs], beta[:, h:h + 1], Ls[:, h, :], ALU.mult, ALU.mult)
        k.tt(aqk[:], v4(PF[4]), LT[:], ALU.mult)
        for h in range(4):
            k.tr(PB[0][:, h * 128:(h + 1) * 128], Am[0][:, h, :], ident[:])
        k.cp(V(AmT[0].t[:, :, :].rearrange("p h d -> p (h d)"), AmT[0].toks), PB[0][:, 0:512], eng="act")
        k.tt(TT[0][:], bc_mid(ident_f, (A, A), sh3), AmT[0][:], ALU.subtract)
        cur = 0
        for r in range(1, 6):
            nxt = 1 - cur
            pP, pPT, pT = PF[2], PF[3], PF[4]
            for h in range(4):
                hs = slice(h * 128, (h + 1) * 128)
                k.mm(pP[:, hs], AmT[cur][:, h, :], Am[cur][:, h, :])
                if r < 5:
                    k.mm(pPT[:, hs], Am[cur][:, h, :], AmT[cur][:, h, :])
            k.cp(Am[nxt][:], v4(pP))
            if r < 5:
                k.cp(AmT[nxt][:], v4(pPT), eng="act")
            for h in range(4):
                hs = slice(h * 128, (h + 1) * 128)
                k.mm(pT[:, hs], Am[nxt][:, h, :], TT[cur][:, h, :], start=True, stop=False)
                k.mm(pT[:, hs], ident[:], TT[cur][:, h, :], start=False, stop=True)
            k.cp(TT[nxt][:], v4(pT))
            cur = nxt
        TTf = TT[cur]
        if CUT == "inv":
            k.cp(ob.p(1, (A, slice(512, 1024))), V(TTf.t[:, :, :].rearrange("p h d -> p (h d)"), TTf.toks))
            k.dma("sp", oab_d.rows(t), ob[:], "oabw%d" % (t % 2))
            continue
        for h in range(4):
            hs = slice(h * 128, (h + 1) * 128)
            k.mm(PF[2][:, hs], kbg[:, hs], TTf[:, h, :])
        k.act(nwT[:], v4(PF[2]), AF.Copy, scale=-1.0)
        pod = PF[4]
        for h in range(4):
            hs = slice(h * 128, (h + 1) * 128)
            pvn = PF[3] if h % 2 == 0 else PF[2]
            k.mm(pvn[:, hs], TTf[:, h, :], vbd[:, hs], start=True, stop=False)
            for c in range(2):
                cs = slice(64 * c, 64 * c + 64)
                k.mm(pvn[cs, hs], nwT[:, h, cs], Sdb.p(h, (A, h, A)), start=False, stop=True)
                k.mm(pod[cs, hs], qgT[:, h, cs], Sdb.p(h, (A, h, A)), start=True, stop=False)
                k.cp(vn.p(h, (cs, h, A)), pvn[cs, hs], eng="act")
                k.mm(PF[(h % 2)][:, 0:128], kdd[cs, hs], vn.p(h, (cs, h, A)))
                k.stt(Sd.p(h, (A, h, A)), Sd.p(h, (A, h, A)), dld[:, 2 * h + c:2 * h + c + 1],
                      PF[(h % 2)][:, 0:128], ALU.mult, ALU.add)
                k.cp(Sdb.p(h, (A, h, A)), Sd.p(h, (A, h, A)))
            k.mm(pod[:, hs], aqk[:, h, :], vn.p(h, (A, h, A)), start=False, stop=True)
        head_norm_gate(k, pod, ss4, ln4, r4, osd, gsd, ob.p(1, (A, slice(512, 1024))))
        k.dma("pool", oab_d.rows(t), ob[:], "oabw%d" % (t % 2))


def phase_b(k, es, l, NT, PF, PB, ident, ident_f, xin_d, oab_d, hout_d, pre_mix_d, w_in_d, w_o_hg_d, w_o_dn_d, w_out_d,
            post_mix_d):
    cnt = [0]
    win = w_in_d[l]
    Wm = [load_wcols(k, es, "Wm%d" % i, win, 4104 + 512 * i, 4616 + 512 * i, cnt) for i in range(4)]
    Whg = load_wcols(k, es, "Whg", w_o_hg_d[l], 0, D, cnt, kch=4)
    Wdn = load_wcols(k, es, "Wdn", w_o_dn_d[l], 0, D, cnt, kch=4)
    Wout = load_wcols(k, es, "Wout", w_out_d[l], 0, D, cnt)
    gcol = load_gcol(k, es, "gcolB", pre_mix_d[l], PF[0], ident_f, "cst0")
    gbc = k.sb(es, "gbcB", [128, D], F32)
    k.dma("sp", gbc[:], post_mix_d[l:l + 1, :].partition_broadcast(128), "cst1")
    for wt in Wm:
        fold_gain(k, wt, gcol, 8, 512)
    xt = [k.sb(es, "xtB%d" % i, [128, D], F32) for i in range(2)]
    ot = [k.sb(es, "otB%d" % i, [128, D], BF16) for i in range(2)]
    ht = [k.sb(es, "htB%d" % i, [128, D], F32) for i in range(2)]
    def two(name, shape, dt, nparts=1):
        return [k.sb(es, "%s%d" % (name, i), shape, dt, nparts=nparts) for i in range(2)]
    junk2 = two("junkB", [128, D], BF16, 2)
    ssx = two("ssB", [128, 2], F32)
    lnvx = two("lnvB", [128, 1], F32)
    rstdx = two("rstdB", [128, 1], F32)
    ss2x = two("ss2B", [128, 2], F32)
    lnv2x = two("lnv2B", [128, 1], F32)
    rstd2x = two("rstd2B", [128, 1], F32)
    ubx = two("ubB", [128, D], BF16)
    uTx = two("uTB", [128, D], BF16)
    oTx = two("oTB", [128, D], BF16)
    sgmx = two("sgm", [128, 4, 512], F32, 4)
    t1x = two("t1B", [128, 512], F32)
    mgx = two("mgB", [128, D], BF16, 2)
    mgTx = two("mgTB", [128, D], BF16)
    A = slice(None)
    for t in range(NT):
        x_ = xt[t % 2]
        o_ = ot[t % 2]
        h_ = ht[t % 2]
        i2 = t % 2
        junk, ss, lnv, rstd, ss2, lnv2, rstd2 = junk2[i2], ssx[i2], lnvx[i2], rstdx[i2], ss2x[i2], lnv2x[i2], rstd2x[i2]
        ub, uT, oT, sgm, t1, mg, mgT = ubx[i2], uTx[i2], oTx[i2], sgmx[i2], t1x[i2], mgx[i2], mgTx[i2]
        k.dma("sp", o_[:], oab_d.rows(t), "oabr%d" % (t % 2))
        x_to_uT(k, xin_d, t, x_, junk, ss, lnv, rstd, ub, uT, PB[0], ident, "xin%d" % (t % 2))
        for i in range(4):
            proj_tm(k, PF[i], uT, Wm[i])
            k.act(sgm.p(i, (A, i, A)), PF[i][:], AF.Sigmoid)
        for c in range(8):
            k.tr(PB[1][:, c * 128:(c + 1) * 128], o_[:, c * 128:(c + 1) * 128], ident[:])
        k.cp(oT[:], PB[1][:])
        for hh in range(2):
            cs = slice(hh * 512, (hh + 1) * 512)
            pa, pb_ = PF[2 * hh], PF[2 * hh + 1]
            for c in range(4):
                k.mm(pa[:], oT[:, c * 128:(c + 1) * 128], Whg[:, c, cs], start=(c == 0), stop=(c == 3))
            for c in range(4):
                k.mm(pb_[:], oT[:, 512 + c * 128:512 + (c + 1) * 128], Wdn[:, c, cs], start=(c == 0), stop=(c == 3))
            k.tt(t1[:], pa[:], sgm.p(hh, (A, hh, A)), ALU.mult)
            k.tt(sgm.p(2 + hh, (A, 2 + hh, A)), pb_[:], sgm.p(2 + hh, (A, 2 + hh, A)), ALU.mult)
            k.tt(mg.p(hh, (A, cs)), t1[:], sgm.p(2 + hh, (A, 2 + hh, A)), ALU.add)
        for c in range(8):
            k.tr(PB[0][:, c * 128:(c + 1) * 128], mg.p(c // 4, (A, slice(c * 128, (c + 1) * 128))), ident[:])
        k.cp(mgT[:], PB[0][:], eng="act")
        for hh in range(2):
            for c in range(8):
                k.mm(PF[4 + hh][:], mgT[:, c * 128:(c + 1) * 128], Wout[:, c, hh * 512:(hh + 1) * 512],
                     start=(c == 0), stop=(c == 7))
        rms_stats(k, [PF[4][:], PF[5][:]], ss2,
                  [junk.p(0, (A, slice(0, 512))), junk.p(1, (A, slice(512, 1024)))], rstd2, lnv2)
        for hh in range(2):
            sl = slice(hh * 512, (hh + 1) * 512)
            k.stt(h_[:, sl], PF[4 + hh][:], rstd2[:, 0:1], gbc[:, sl], ALU.mult, ALU.mult)
        k.tt(h_[:], h_[:], x_[:], ALU.add, eng="pool")
        k.dma("pool", hout_d.rows(t), h_[:], "hout%d" % (t % 2))


_CACHE = {}

WNAMES = ["hg_lower_bounds", "pre_mix_w", "w_in", "hg_norm_w", "conv_w", "dn_a_log", "dn_dt_bias", "dn_norm_w",
          "w_o_hg", "w_o_dn", "w_out", "post_mix_w", "pre_ffn_w", "w_gate_up", "w_down", "post_ffn_w"]


def kernel(**inputs):
    x = np.ascontiguousarray(inputs["x"], dtype=np.float32)
    B, T, _ = x.shape
    if "nc" not in _CACHE:
        _CACHE["nc"] = build(T=T, L=2)
    nc = _CACHE["nc"]
    shared = {n: np.ascontiguousarray(inputs[n], dtype=np.float32) for n in WNAMES}
    in_maps = []
    for b in range(B):
        m = dict(shared)
        m["x"] = x[b]
        in_maps.append(m)
    res = run_bass_kernel_spmd(nc, in_maps, core_ids=list(range(B)))
    return np.stack([r["out"] for r in res.results], axis=0)
```

```python
from contextlib import ExitStack

import numpy as np
import concourse.bass as bass
import concourse.mybir as mybir
from concourse.bass_utils import run_bass_kernel_spmd

F32 = mybir.dt.float32
BF16 = mybir.dt.bfloat16
AF = mybir.ActivationFunctionType
ALU = mybir.AluOpType

D = 1024
DFF = 2816
INW = 6152
EPS = 1e-6
ENGS = ("pe", "act", "dve", "pool", "sp")
CUT = None
SCHED_WINDOW = 256


class Tok:
    __slots__ = ("last_w", "readers")

    def __init__(self):
        self.last_w = None
        self.readers = []


class Op:
    __slots__ = ("eng", "fn", "idx", "deps", "signal", "semval", "clock", "dma_chan", "waits", "barrier",
                 "cost", "start", "finish", "users", "nun", "ready", "done", "chan_snap")

    def __init__(self, eng, fn):
        self.eng = eng
        self.fn = fn
        self.idx = -1
        self.deps = []
        self.signal = False
        self.semval = 0
        self.clock = None
        self.dma_chan = None
        self.waits = []
        self.barrier = False
        self.cost = 100.0
        self.start = 0.0
        self.finish = 0.0
        self.users = None
        self.nun = 0
        self.ready = 0.0
        self.done = False
        self.chan_snap = None


class Prog:
    def __init__(self):
        self.streams = {e: [] for e in ENGS}
        self.chan_count = {}
        self.chan_last = {}
        self.all_ops = []

    def op(self, eng, fn, reads=(), writes=(), dma_chan=None, cost=100.0):
        o = Op(eng, fn)
        o.dma_chan = dma_chan
        o.cost = cost
        deps = []
        for r in reads:
            if r.last_w is not None:
                deps.append((r.last_w, "raw"))
        for w in writes:
            if w.last_w is not None:
                deps.append((w.last_w, "waw"))
            for rd in w.readers:
                deps.append((rd, "war"))
        o.deps = deps
        for r in reads:
            r.readers.append(o)
        for w in writes:
            w.last_w = o
            w.readers = []
        o.idx = len(self.streams[eng])
        self.streams[eng].append(o)
        self.all_ops.append(o)
        if dma_chan is not None:
            c = self.chan_count.get(dma_chan, 0) + 1
            self.chan_count[dma_chan] = c
            o.semval = 16 * c
            self.chan_last[dma_chan] = o
        return o

    def barrier(self):
        m = Op("sp", None)
        m.barrier = True
        m.chan_snap = list(self.chan_last.values())
        self.all_ops.append(m)

    def schedule(self, window=64, lat=120.0):
        segs = [[]]
        marks = []
        for o in self.all_ops:
            if o.barrier:
                marks.append(o)
                segs.append([])
            else:
                segs[-1].append(o)
        new_streams = {e: [] for e in ENGS}
        new_all = []
        free = {e: 0.0 for e in ENGS}
        reorder = ("pe", "act", "dve")
        for si, seg in enumerate(segs):
            st = {e: [] for e in ENGS}
            inseg = set()
            for o in seg:
                st[o.eng].append(o)
                o.users = []
                o.nun = 0
                o.ready = 0.0
                o.done = False
                inseg.add(id(o))
            for o in seg:
                seen = set()
                for d, _ in o.deps:
                    if d is o or id(d) in seen:
                        continue
                    seen.add(id(d))
                    if id(d) in inseg:
                        d.users.append(o)
                        o.nun += 1
            head = {e: 0 for e in ENGS}
            nleft = len(seg)
            order = []
            while nleft:
                best = None
                for e in ENGS:
                    lst = st[e]
                    h = head[e]
                    while h < len(lst) and lst[h].done:
                        h += 1
                    head[e] = h
                    if h >= len(lst):
                        continue
                    if e in reorder:
                        cand = None
                        cstart = None
                        fe = free[e]
                        for j in range(h, min(len(lst), h + window)):
                            o = lst[j]
                            if o.done or o.nun:
                                continue
                            s0 = o.ready if o.ready > fe else fe
                            if cand is None or s0 < cstart - 1e-9:
                                cand, cstart = o, s0
                                if s0 <= fe:
                                    break
                    else:
                        o = lst[h]
                        if o.nun:
                            continue
                        cand = o
                        cstart = o.ready if o.ready > free[e] else free[e]
                    if cand is not None and (best is None or cstart < best[0]):
                        best = (cstart, cand)
                assert best is not None, "scheduler deadlock"
                t0, o = best
                o.start = t0
                o.done = True
                nleft -= 1
                if o.dma_chan is not None:
                    free[o.eng] = t0 + 60.0
                    o.finish = t0 + o.cost
                else:
                    o.finish = t0 + o.cost
                    free[o.eng] = o.finish
                for u in o.users:
                    u.nun -= 1
                    r = o.finish + (lat if u.eng != o.eng or o.dma_chan is not None else 40.0)
                    if r > u.ready:
                        u.ready = r
                order.append(o)
            order.sort(key=lambda q: q.start)
            for o in order:
                o.idx = len(new_streams[o.eng])
                new_streams[o.eng].append(o)
                new_all.append(o)
            if si < len(marks):
                tmax = max(free.values())
                tmax = max([tmax] + [o.finish for o in order]) if order else tmax
                for e in ENGS:
                    free[e] = tmax
                lasts = []
                for e in ENGS:
                    for o in reversed(new_streams[e]):
                        if o.dma_chan is None and not o.barrier:
                            lasts.append(o)
                            break
                for e in ENGS:
                    b = Op(e, None)
                    b.barrier = True
                    b.deps = [(d, "raw") for d in lasts if d.eng != e] + [(d, "raw") for d in marks[si].chan_snap]
                    b.idx = len(new_streams[e])
                    new_streams[e].append(b)
                    new_all.append(b)
        self.streams = new_streams
        self.all_ops = new_all
        self.est_ns = max(free.values())

    def resolve(self):
        eng_clock = {e: ({}, {}) for e in ENGS}
        for o in self.all_ops:
            cc, dc = eng_clock[o.eng]
            waits = []
            for d, kind in o.deps:
                if d is o:
                    continue
                if d.dma_chan is not None:
                    if dc.get(d.dma_chan, 0) >= d.semval:
                        continue
                    waits.append(d)
                    dc[d.dma_chan] = d.semval
                else:
                    if d.eng == o.eng and kind != "raw" and o.eng == "pe":
                        continue
                    if cc.get(d.eng, -1) >= d.idx:
                        continue
                    waits.append(d)
                    d.signal = True
                    cc[d.eng] = d.idx
                dcc, ddc = d.clock
                for k, v in dcc.items():
                    if cc.get(k, -1) < v:
                        cc[k] = v
                for k, v in ddc.items():
                    if dc.get(k, 0) < v:
                        dc[k] = v
            best = {}
            for d in waits:
                if d.dma_chan is not None:
                    key = ("d", d.dma_chan)
                    val = d.semval
                else:
                    key = ("c", d.eng)
                    val = d.idx
                cur = best.get(key)
                if cur is None or val > cur[0]:
                    best[key] = (val, d)
            o.waits = [v[1] for v in best.values()]
            o.clock = (dict(cc), dict(dc))
            if o.dma_chan is None:
                o.clock[0][o.eng] = max(o.clock[0].get(o.eng, -1), o.idx - 1)
        for e in ENGS:
            n = 0
            for o in self.streams[e]:
                if o.dma_chan is None and o.signal:
                    n += 1
                    o.semval = n

    def emit(self, nc, final_waits=()):
        self.schedule(window=SCHED_WINDOW)
        self.resolve()
        with ExitStack() as es:
            sems = {}
            for e in ENGS:
                sems[("c", e)] = es.enter_context(nc.semaphore("s_" + e))
            for ch in self.chan_count:
                sems[("d", ch)] = es.enter_context(nc.semaphore("d_" + str(ch)))
            block = es.enter_context(nc.Block())

            def run_stream(ename):
                def body(eng):
                    for o in self.streams[ename]:
                        for d in o.waits:
                            if d.dma_chan is not None:
                                eng.wait_ge(sems[("d", d.dma_chan)], d.semval)
                            else:
                                eng.wait_ge(sems[("c", d.eng)], d.semval)
                        if o.fn is None:
                            continue
                        ins = o.fn(eng)
                        if o.dma_chan is not None:
                            ins.then_inc(sems[("d", o.dma_chan)], 16)
                        elif o.signal:
                            ins.then_inc(sems[("c", ename)], 1)
                    if ename == "sp":
                        done = {}
                        for d in final_waits:
                            done[d.dma_chan] = max(done.get(d.dma_chan, 0), d.semval)
                        for ch, v in done.items():
                            eng.wait_ge(sems[("d", ch)], v)

                return body

            block.tensor(run_stream("pe"))
            block.scalar(run_stream("act"))
            block.vector(run_stream("dve"))
            block.gpsimd(run_stream("pool"))
            block.sync(run_stream("sp"))


class V:
    __slots__ = ("ap", "toks")

    def __init__(self, ap, toks):
        self.ap = ap
        self.toks = tuple(toks)


class Buf:
    def __init__(self, t, nparts=1):
        self.t = t
        self.toks = [Tok() for _ in range(nparts)]

    def __getitem__(self, idx):
        return V(self.t[idx], self.toks)

    def p(self, i, idx):
        if isinstance(i, int):
            return V(self.t[idx], (self.toks[i],))
        return V(self.t[idx], [self.toks[j] for j in i])


def _fsz(v):
    ap = v.ap if isinstance(v, V) else v
    n = 1
    for d in ap.shape[1:]:
        n *= int(d)
    return n


def _is_f32(v):
    return v.ap.dtype == F32


class K:
    def __init__(self, nc):
        self.nc = nc
        self.P = Prog()

    def sb(self, es, name, shape, dtype, nparts=1):
        self._uid = getattr(self, "_uid", 0) + 1
        return Buf(es.enter_context(self.nc.sbuf_tensor("%s_%d" % (name, self._uid), list(shape), dtype)), nparts)

    def ps(self, es, name, shape, dtype):
        return Buf(es.enter_context(self.nc.psum_tensor(name, list(shape), dtype)), 1)

    @staticmethod
    def _rw(*vs):
        t = []
        for v in vs:
            if isinstance(v, V):
                t.extend(v.toks)
        return t

    @staticmethod
    def _a(v):
        return v.ap if isinstance(v, V) else v

    def mm(self, out, lhsT, rhs, start=True, stop=True):
        n = _fsz(rhs)
        c = 30.0 + n * (1.7 if _is_f32(rhs) else 0.45)
        self.P.op("pe", lambda e: e.matmul(out.ap, lhsT=lhsT.ap, rhs=rhs.ap, start=start, stop=stop),
                  reads=self._rw(lhsT, rhs), writes=out.toks, cost=c)

    def tr(self, out, in_, ident):
        self.P.op("pe", lambda e: e.transpose(out=out.ap, in_=in_.ap, identity=ident.ap),
                  reads=self._rw(in_, ident), writes=out.toks, cost=100.0)

    def act(self, out, in_, func, bias=None, scale=None, accum=None, eng="act"):
        kw = {}
        if bias is not None:
            kw["bias"] = self._a(bias)
        if scale is not None:
            kw["scale"] = self._a(scale)
        if accum is not None:
            kw["accum_out"] = accum.ap
        self.P.op(eng, lambda e: e.activation(out=out.ap, in_=in_.ap, func=func, **kw),
                  reads=self._rw(in_, bias, scale), writes=self._rw(out, accum), cost=220.0 + _fsz(in_) * 1.05)

    def tt(self, out, in0, in1, op, eng="dve"):
        self.P.op(eng, lambda e: e.tensor_tensor(out=out.ap, in0=in0.ap, in1=in1.ap, op=op),
                  reads=self._rw(in0, in1), writes=out.toks,
                  cost=(100.0 + _fsz(out) * 1.05) * (2.0 if eng == "pool" else 1.0))

    def ts(self, out, in0, s1, s2, op0, op1=None, eng="dve", accum=None):
        kw = {}
        if op1 is not None:
            kw["op1"] = op1
        if accum is not None:
            kw["accum_out"] = accum.ap
        self.P.op(eng, lambda e: e.tensor_scalar(out=out.ap, in0=in0.ap, scalar1=self._a(s1), scalar2=self._a(s2),
                                                 op0=op0, **kw),
                  reads=self._rw(in0, s1, s2), writes=self._rw(out, accum), cost=100.0 + _fsz(out) * 0.8)

    def stt(self, out, in0, scalar, in1, op0, op1, eng="dve"):
        self.P.op(eng, lambda e: e.scalar_tensor_tensor(out=out.ap, in0=in0.ap, scalar=self._a(scalar), in1=in1.ap,
                                                        op0=op0, op1=op1),
                  reads=self._rw(in0, scalar, in1), writes=out.toks, cost=100.0 + _fsz(out) * 1.05)

    def cp(self, out, in_, eng="dve"):
        if eng == "act":
            self.P.op("act", lambda e: e.copy(out=out.ap, in_=in_.ap), reads=in_.toks, writes=out.toks,
                      cost=220.0 + _fsz(out) * 1.05)
        else:
            self.P.op(eng, lambda e: e.tensor_copy(out=out.ap, in_=in_.ap), reads=in_.toks, writes=out.toks,
                      cost=100.0 + _fsz(out) * 0.8)

    def recip(self, out, in_):
        self.P.op("dve", lambda e: e.reciprocal(out=out.ap, in_=in_.ap), reads=in_.toks, writes=out.toks)

    def memset(self, out, val, eng="pool"):
        self.P.op(eng, lambda e: e.memset(out.ap, val), writes=out.toks)

    def asel(self, out, in_, pattern, cmp, fill, base, cm):
        self.P.op("pool", lambda e: e.affine_select(out=out.ap, in_=in_.ap, pattern=pattern, compare_op=cmp,
                                                    fill=fill, base=base, channel_multiplier=cm),
                  reads=in_.toks, writes=out.toks)

    def dma(self, q, out, in_, chan, noncontig=False):
        kw = {"allow_slow_non_contiguous": True} if noncontig else {}
        ap = out.ap if isinstance(out, V) else out
        nbytes = 128.0 * 4
        try:
            nbytes = float(ap.nbytes())
        except Exception:
            pass
        return self.P.op(q, lambda e: e.dma_start(out=self._a(out), in_=self._a(in_), **kw),
                         reads=self._rw(in_), writes=self._rw(out), dma_chan=chan, cost=2500.0 + nbytes / 150.0)


class DramT:
    def __init__(self, ap, ntiles):
        self.ap = ap
        self.toks = [Tok() for _ in range(max(1, ntiles))]

    def rows(self, t, n=128):
        return V(self.ap[t * 128:t * 128 + n], (self.toks[t],))


def build(T=8192, L=2, phases=("A", "B", "C"), dbg=()):
    NT = T // 128
    nc = bass.Bass("TRN2", target_bir_lowering=False)
    k = K(nc)
    P = k.P

    def din(name, shape):
        return nc.dram_tensor(name, list(shape), F32, kind="ExternalInput").ap()

    x_d = DramT(din("x", [T, D]), NT)
    hglb_d = din("hg_lower_bounds", [L, 512])
    pre_mix_d = din("pre_mix_w", [L, D])
    w_in_d = din("w_in", [L, D, INW])
    hg_norm_d = din("hg_norm_w", [L, 128])
    conv_d = din("conv_w", [L, 4, 1536])
    alog_d = din("dn_a_log", [L, 4])
    dtb_d = din("dn_dt_bias", [L, 4])
    dn_norm_d = din("dn_norm_w", [L, 128])
    w_o_hg_d = din("w_o_hg", [L, 512, D])
    w_o_dn_d = din("w_o_dn", [L, 512, D])
    w_out_d = din("w_out", [L, D, D])
    post_mix_d = din("post_mix_w", [L, D])
    pre_ffn_d = din("pre_ffn_w", [L, D])
    w_gu_d = din("w_gate_up", [L, D, 2 * DFF])
    w_dn_d = din("w_down", [L, DFF, D])
    post_ffn_d = din("post_ffn_w", [L, D])
    out_d = DramT(nc.dram_tensor("out", [T, D], F32, kind="ExternalOutput").ap(), NT)
    skind = "ExternalOutput" if dbg else "Internal"
    hbuf_d = DramT(nc.dram_tensor("hbuf", [T, D], F32, kind=skind).ap(), NT)
    x1_d = DramT(nc.dram_tensor("x1buf", [T, D], F32, kind=skind).ap(), NT)
    oab_d = DramT(nc.dram_tensor("oab", [T, D], BF16, kind=skind).ap(), NT)

    final_ops = []
    with ExitStack() as pes:
        PF = [k.ps(pes, "pf%d" % i, [128, 512], F32) for i in range(6)]
        PB = [k.ps(pes, "pb%d" % i, [128, 1024], BF16) for i in range(2)]
        ident_f = k.sb(pes, "ident_f", [128, 128], F32)
        ident = k.sb(pes, "ident", [128, 128], BF16)
        k.memset(ident_f[:], 1.0)
        k.asel(ident_f[:], ident_f[:], [[-1, 128]], ALU.is_equal, 0.0, 0, 1)
        k.cp(ident[:], ident_f[:])

        if "A" in phases:
            mk = make_masks(k, pes)
        for l in range(L):
            xin = x_d if l == 0 else x1_d
            xout = out_d if l == L - 1 else x1_d
            if "A" in phases:
                with ExitStack() as es:
                    phase_a(k, es, l, NT, PF, PB, ident, ident_f, xin, oab_d, hglb_d, pre_mix_d, w_in_d, hg_norm_d,
                            conv_d, alog_d, dtb_d, dn_norm_d, mk)
                P.barrier()
            if "B" in phases:
                with ExitStack() as es:
                    phase_b(k, es, l, NT, PF, PB, ident, ident_f, xin, oab_d, hbuf_d, pre_mix_d, w_in_d, w_o_hg_d, w_o_dn_d,
                            w_out_d, post_mix_d)
                P.barrier()
            if "C" in phases:
                with ExitStack() as es:
                    phase_c(k, es, l, NT, PF, PB, ident, ident_f, hbuf_d if ("A" in phases or "B" in phases) else xin,
                            xout, pre_ffn_d, w_gu_d, w_dn_d, post_ffn_d, final_ops, l == L - 1)
                P.barrier()
        P.emit(nc, final_waits=final_ops)
    return nc


def rms_stats(k, src_list, ss, tmp_junk, rstd, lnv):
    n = len(src_list)
    for i, s in enumerate(src_list):
        k.act(tmp_junk[i], s, AF.Square, scale=float(D ** -0.5), accum=ss[:, i:i + 1])
    if n == 2:
        k.tt(ss[:, 0:1], ss[:, 0:1], ss[:, 1:2], ALU.add)
    k.act(lnv[:, 0:1], ss[:, 0:1], AF.Ln, bias=EPS)
    k.act(rstd[:, 0:1], lnv[:, 0:1], AF.Exp, scale=-0.5)


def load_weight_cast(k, dst, src_ap, chan_prefix, counter):
    ch = "%s%d" % (chan_prefix, counter[0])
    counter[0] += 1
    return k.dma("pool", dst, src_ap, ch)


def phase_c(k, es, l, NT, PF, PB, ident, ident_f, hin_d, xout_d, pre_ffn_d, w_gu_d, w_dn_d, post_ffn_d, final_ops, is_last):
    nc = k.nc
    NB = 6
    bw = [512] * 5 + [256]
    boff = [512 * i for i in range(6)]
    wg = [k.sb(es, "wg%d" % b, [128, 8, bw[b]], BF16) for b in range(NB)]
    wu = [k.sb(es, "wu%d" % b, [128, 8, bw[b]], BF16) for b in range(NB)]
    wd = [k.sb(es, "wd%d" % h, [128, 11, D], BF16) for h in range(2)]
    gcol = load_gcol(k, es, "gcol", pre_ffn_d[l], PF[0], ident_f, "cst0")
    gbc = k.sb(es, "gbc", [128, D], F32)
    cnt = [0]
    k.dma("sp", gbc[:], post_ffn_d[l:l + 1, :].partition_broadcast(128), "cst1")
    wsrc = w_gu_d[l].rearrange("(k p) n -> p k n", p=128)
    for b in range(NB):
        for (wt, off) in ((wg[b], boff[b]), (wu[b], DFF + boff[b])):
            load_weight_cast(k, wt[:], wsrc[:, :, off:off + bw[b]], "w", cnt)
    dsrc = w_dn_d[l].rearrange("(k p) n -> p k n", p=128)
    for h in range(2):
        load_weight_cast(k, wd[h][:], dsrc[:, 11 * h:11 * h + 11, :], "w", cnt)
    for b in range(NB):
        for wt in (wg[b], wu[b]):
            k.tt(wt[:], wt[:], V(gcol.t[:, :].unsqueeze(2).broadcast_to([128, 8, bw[b]]), gcol.toks), ALU.mult)

    ht = [k.sb(es, "ht%d" % i, [128, D], F32) for i in range(2)]
    ot = [k.sb(es, "ot%d" % i, [128, D], F32) for i in range(2)]
    def two(name, shape, dt, nparts=1):
        return [k.sb(es, "%s%d" % (name, i), shape, dt, nparts=nparts) for i in range(2)]
    junkx = two("junk", [128, D], BF16, 2)
    ssx = two("ss", [128, 2], F32)
    lnvx = two("lnv", [128, 1], F32)
    rstdx = two("rstd", [128, 1], F32)
    ss2x = two("ss2", [128, 2], F32)
    lnv2x = two("lnv2", [128, 1], F32)
    rstd2x = two("rstd2", [128, 1], F32)
    vbx = two("vb", [128, D], BF16)
    vTx = two("vT", [128, D], BF16)
    sg = [k.sb(es, "sg%d" % i, [128, 512], F32) for i in range(2)]
    actbx = two("actb", [128, DFF], BF16, NB)
    actTx = two("actT", [128, DFF], BF16, 3)

    for t in range(NT):
        h = ht[t % 2]
        o = ot[t % 2]
        i2 = t % 2
        junk, ss, lnv, rstd, ss2, lnv2, rstd2 = junkx[i2], ssx[i2], lnvx[i2], rstdx[i2], ss2x[i2], lnv2x[i2], rstd2x[i2]
        vb, vT, actb, actT = vbx[i2], vTx[i2], actbx[i2], actTx[i2]
        k.dma("sp", h[:], hin_d.rows(t), "hin%d" % (t % 2))
        rms_stats(k, [h[:]], ss, [junk[:]], rstd, lnv)
        k.act(vb[:], h[:], AF.Copy, scale=rstd[:, 0:1])
        for c in range(8):
            k.tr(PB[0][:, c * 128:(c + 1) * 128], vb[:, c * 128:(c + 1) * 128], ident[:])
        k.cp(vT[:], PB[0][:])
        for b in range(NB):
            pg = PF[(2 * b) % 4]
            pu = PF[(2 * b + 1) % 4]
            n = bw[b]
            for c in range(8):
                k.mm(pg[:, 0:n], vT[:, c * 128:(c + 1) * 128], wg[b][:, c, :], start=(c == 0), stop=(c == 7))
            for c in range(8):
                k.mm(pu[:, 0:n], vT[:, c * 128:(c + 1) * 128], wu[b][:, c, :], start=(c == 0), stop=(c == 7))
            s = sg[b % 2]
            k.act(s[:, 0:n], pg[:, 0:n], AF.Silu)
            k.tt(actb.p(b, (slice(None), slice(boff[b], boff[b] + n))), s[:, 0:n], pu[:, 0:n], ALU.mult)
        for g in range(3):
            c0 = g * 8
            c1 = min(22, c0 + 8)
            pb = PB[(g + 1) % 2]
            for c in range(c0, c1):
                blk = (c * 128) // 512
                k.tr(pb[:, (c - c0) * 128:(c - c0 + 1) * 128],
                     actb.p(blk, (slice(None), slice(c * 128, (c + 1) * 128))), ident[:])
            k.cp(actT.p(g, (slice(None), slice(c0 * 128, c1 * 128))), pb[:, 0:(c1 - c0) * 128],
                 eng=("act" if g == 1 else "dve"))
        for hh in range(2):
            pf = PF[4 + hh]
            for c in range(22):
                k.mm(pf[:], actT.p(c // 8, (slice(None), slice(c * 128, (c + 1) * 128))),
                     wd[c // 11][:, c % 11, hh * 512:(hh + 1) * 512], start=(c == 0), stop=(c == 21))
        rms_stats(k, [PF[4][:], PF[5][:]], ss2,
                  [junk.p(0, (slice(None), slice(0, 512))), junk.p(1, (slice(None), slice(512, 1024)))], rstd2, lnv2)
        for hh in range(2):
            sl = slice(hh * 512, (hh + 1) * 512)
            k.stt(o[:, sl], PF[4 + hh][:], rstd2[:, 0:1], gbc[:, sl], ALU.mult, ALU.mult)
        k.tt(o[:], o[:], h[:], ALU.add, eng="pool")
        d = k.dma("pool", xout_d.rows(t), o[:], "oout%d" % (t % 2))
        if is_last:
            final_ops.append(d)


def bc(buf, idx, shape):
    ap = buf.t[idx]
    return V(ap.unsqueeze(len(ap.shape)).broadcast_to(list(shape)), buf.toks)


def bc_mid(buf, idx, shape):
    ap = buf.t[idx]
    return V(ap.unsqueeze(1).broadcast_to(list(shape)), buf.toks)


def load_wcols(k, es, name, src_l, c0, c1, cnt, kch=8):
    wt = k.sb(es, name, [128, kch, c1 - c0], BF16)
    load_weight_cast(k, wt[:], src_l.rearrange("(k p) n -> p k n", p=128)[:, :, c0:c1], "w", cnt)
    return wt


def load_gcol(k, es, name, src_row_ap, PFb, ident_f, chan):
    rows = k.sb(es, name + "_r", [8, 128], F32)
    gcol = k.sb(es, name, [128, 8], F32)
    k.dma("sp", rows[:], src_row_ap.rearrange("(k p) -> k p", p=128), chan)
    k.tr(PFb[:, 0:8], rows[:], ident_f[0:8, 0:8])
    k.cp(gcol[:], PFb[:, 0:8])
    return gcol


def fold_gain(k, wt, gcol, kch, n):
    k.tt(wt[:], wt[:], V(gcol.t[:, :].unsqueeze(2).broadcast_to([128, kch, n]), gcol.toks), ALU.mult)


def x_to_uT(k, xin_d, t, xt, junk, ss, lnv, rstd, ub, uT, PBk, ident, chan):
    k.dma("sp", xt[:], xin_d.rows(t), chan)
    rms_stats(k, [xt[:]], ss, [junk[:]], rstd, lnv)
    k.act(ub[:], xt[:], AF.Copy, scale=rstd[:, 0:1])
    for c in range(8):
        k.tr(PBk[:, c * 128:(c + 1) * 128], ub[:, c * 128:(c + 1) * 128], ident[:])
    k.cp(uT[:], PBk[:])


def proj_tm(k, pf, uT, wt, n=512, c0=0):
    for c in range(8):
        k.mm(pf[:, 0:n], uT[:, c * 128:(c + 1) * 128], wt[:, c, c0:c0 + n], start=(c == 0), stop=(c == 7))


def make_masks(k, es):
    m = {}
    M1 = k.sb(es, "M1", [128, 128], F32)
    M2 = k.sb(es, "M2", [128, 128], F32)
    NS = k.sb(es, "NEGS", [128, 128], F32)
    NCT = k.sb(es, "NEGCT", [128, 128], F32)
    cind = k.sb(es, "cind", [128, 2], F32)
    ones = k.sb(es, "ones_f", [128, 128], F32)
    k.memset(M1[:], 1.0)
    k.asel(M1[:], M1[:], [[1, 128]], ALU.is_ge, 0.0, 0, -1)
    k.memset(M1[0:64, 64:128], 0.0)
    k.memset(M2[:], 1.0)
    k.asel(M2[:], M2[:], [[-1, 128]], ALU.is_gt, 0.0, 0, 1)
    k.memset(M2[64:128, 0:64], 0.0)
    k.ts(NS[:], M2[:], -1.0, 30000.0, ALU.add, ALU.mult)
    k.ts(NCT[:], M1[:], -1.0, 30000.0, ALU.add, ALU.mult)
    k.memset(cind[:], 0.0)
    k.memset(cind[0:64, 0:1], 1.0)
    k.memset(cind[64:128, 1:2], 1.0)
    k.memset(ones[:], 1.0)
    NSb = k.sb(es, "NEGSb", [128, 128], BF16)
    NCTb = k.sb(es, "NEGCTb", [128, 128], BF16)
    k.cp(NSb[:], NS[:])
    k.cp(NCTb[:], NCT[:])
    return dict(M1=M1, M2=M2, NS=NS, NCT=NCT, cind=cind, ones=ones, NSb=NSb, NCTb=NCTb)


def head_norm_gate(k, po, ss4, ln4, r4, osb, gsw, dst):
    for h in range(4):
        k.act(osb[:, h * 128:(h + 1) * 128], po[:, h * 128:(h + 1) * 128], AF.Square, scale=float(128 ** -0.5),
              accum=ss4[:, h:h + 1])
    k.act(ln4[:], ss4[:], AF.Ln, bias=EPS)
    k.act(r4[:], ln4[:], AF.Exp, scale=-0.5)
    k.tt(V(osb.t[:, :].rearrange("p (h d) -> p h d", h=4), osb.toks),
         V(po.t[:, :].rearrange("p (h d) -> p h d", h=4), po.toks), bc(r4, (slice(None), slice(None)), [128, 4, 128]),
         ALU.mult)
    k.tt(dst, osb[:], gsw[:], ALU.mult)


def phase_a(k, es, l, NT, PF, PB, ident, ident_f, xin_d, oab_d, hglb_d, pre_mix_d, w_in_d, hg_norm_d, conv_d,
            alog_d, dtb_d, dn_norm_d, mk):
    nc = k.nc
    M1, M2, NS, NCT, cind, ones = mk["M1"], mk["M2"], mk["NS"], mk["NCT"], mk["cind"], mk["ones"]
    NSb, NCTb = mk["NSb"], mk["NCTb"]
    cnt = [0]
    win = w_in_d[l]
    Wq = load_wcols(k, es, "Wq", win, 0, 512, cnt)
    Wf = load_wcols(k, es, "Wf", win, 512, 1024, cnt)
    Wi = load_wcols(k, es, "Wi", win, 1024, 1536, cnt)
    Wg = load_wcols(k, es, "Wg", win, 1536, 2048, cnt)
    Wc = [load_wcols(k, es, "Wc%d" % i, win, 2048 + 512 * i, 2560 + 512 * i, cnt) for i in range(3)]
    Wdg = load_wcols(k, es, "Wdg", win, 3584, 4096, cnt)
    Wab = load_wcols(k, es, "Wab", win, 4096, 4104, cnt)
    gcol = load_gcol(k, es, "gcolA", pre_mix_d[l], PF[0], ident_f, "cst0")
    for wt in (Wq, Wf, Wi, Wg, Wc[0], Wc[1], Wc[2], Wdg):
        fold_gain(k, wt, gcol, 8, 512)
    fold_gain(k, Wab, gcol, 8, 8)
    cw = k.sb(es, "cw", [128, 12, 4], F32)
    cwr = k.sb(es, "cwr", [4, 1536], F32)
    k.dma("sp", cwr[:], conv_d[l], "cst1")
    for c in range(12):
        k.tr(PF[1][:, 4 * c:4 * c + 4], cwr[:, c * 128:(c + 1) * 128], ident_f[0:4, 0:4])
    k.cp(V(cw.t[:, :, :].rearrange("p c j -> p (c j)"), cw.toks), PF[1][:, 0:48])
    dtb = k.sb(es, "dtb", [128, 4], F32)
    k.dma("sp", dtb[:], dtb_d[l:l + 1, :].partition_broadcast(128), "cst2")
    negA = k.sb(es, "negA", [128, 4], F32)
    k.dma("sp", negA[:], alog_d[l:l + 1, :].partition_broadcast(128), "cst3")
    k.act(negA[:], negA[:], AF.Exp)
    k.ts(negA[:], negA[:], -1.0, None, ALU.mult)
    hgn = k.sb(es, "hgn", [128, 128], F32)
    k.dma("sp", hgn[:], hg_norm_d[l:l + 1, :].partition_broadcast(128), "cst4")
    dnn = k.sb(es, "dnn", [128, 128], F32)
    k.dma("sp", dnn[:], dn_norm_d[l:l + 1, :].partition_broadcast(128), "cst5")
    omlb = k.sb(es, "omlb", [128, 512], F32)
    if l == 0:
        k.memset(omlb[:], 1.0)
    else:
        lb0 = k.sb(es, "lb0", [128, 512], F32)
        k.dma("sp", omlb[:], hglb_d[1:2, :].partition_broadcast(128), "cst6")
        k.dma("sp", lb0[:], hglb_d[0:1, :].partition_broadcast(128), "cst7")
        k.tt(omlb[:], omlb[:], lb0[:], ALU.subtract)
        k.act(omlb[:], omlb[:], AF.Exp)
        k.ts(omlb[:], omlb[:], 1.0, None, ALU.add)
        k.recip(omlb[:], omlb[:])
    Sh = k.sb(es, "Sh", [128, 4, 128], F32, nparts=4)
    Shb = k.sb(es, "Shb", [128, 4, 128], BF16, nparts=4)
    Sd = k.sb(es, "Sd", [128, 4, 128], F32, nparts=4)
    Sdb = k.sb(es, "Sdb", [128, 4, 128], BF16, nparts=4)
    for S_ in (Sh, Shb, Sd, Sdb):
        k.memset(S_[:], 0.0)
    cvin = k.sb(es, "cvin", [128, 12, 131], BF16)
    k.memset(cvin[:], 0.0)
    diagw = k.sb(es, "diagw", [128, 12, 4, 128], BF16)
    for c in range(12):
        for j in range(4):
            k.ts(diagw[:, c, j, :], ident_f[:], cw[:, c, j:j + 1], None, ALU.mult)
    xt = [k.sb(es, "xtA%d" % i, [128, D], F32) for i in range(2)]
    junk = k.sb(es, "junkA", [128, D], BF16)
    ss = k.sb(es, "ssA", [128, 2], F32)
    lnv = k.sb(es, "lnvA", [128, 1], F32)
    rstd = k.sb(es, "rstdA", [128, 1], F32)
    ub2 = [k.sb(es, "ubA%d" % i, [128, D], BF16) for i in range(2)]
    uT2 = [k.sb(es, "uTA%d" % i, [128, D], BF16) for i in range(2)]
    kf = k.sb(es, "kf", [128, 512], F32)
    lf = k.sb(es, "lf", [128, 512], F32)
    eb = k.sb(es, "eb", [128, 512], F32)
    enb = k.sb(es, "enb", [128, 512], F32)
    esf = k.sb(es, "esf", [128, 512], F32)
    qe = k.sb(es, "qe", [128, 512], BF16)
    ke = k.sb(es, "ke", [128, 512], BF16)
    kdh = k.sb(es, "kdh", [128, 512], BF16)
    vh = k.sb(es, "vh", [128, 512], BF16)
    gsw = k.sb(es, "gsw", [128, 512], F32)
    qkT = k.sb(es, "qkT", [128, 1024], BF16)
    attnT = k.sb(es, "attnT", [128, 4, 128], BF16)
    dlh = k.sb(es, "dlh", [128, 8], F32)
    osb = k.sb(es, "osb", [128, 512], F32)
    ss4 = k.sb(es, "ss4", [128, 4], F32)
    ln4 = k.sb(es, "ln4", [128, 4], F32)
    r4 = k.sb(es, "r4", [128, 4], F32)
    oab = [k.sb(es, "oabt%d" % i, [128, D], BF16, nparts=2) for i in range(2)]
    qkvF = k.sb(es, "qkvF", [128, 12, 128], BF16)
    qkvT = k.sb(es, "qkvT", [128, 1536], BF16)
    ss8 = k.sb(es, "ss8", [128, 8], F32)
    ln8 = k.sb(es, "ln8", [128, 8], F32)
    r8 = k.sb(es, "r8", [128, 8], F32)
    sq4 = k.sb(es, "sq4", [128, 4], F32)
    beta = k.sb(es, "beta", [128, 4], F32)
    ab8 = k.sb(es, "ab8", [128, 8], F32)
    gt = k.sb(es, "gt", [128, 4], F32)
    egc = k.sb(es, "egc", [128, 4], F32)
    egs = k.sb(es, "egs", [128, 4], F32)
    bg = k.sb(es, "bg", [128, 4], F32)
    gmask = k.sb(es, "gmask", [128, 4, 2], F32)
    dld = k.sb(es, "dld", [128, 8], F32)
    kn = k.sb(es, "kn", [128, 512], BF16)
    kbg = k.sb(es, "kbg", [128, 512], BF16)
    kdd = k.sb(es, "kdd", [128, 512], BF16)
    vbd = k.sb(es, "vbd", [128, 512], BF16)
    qn = k.sb(es, "qn", [128, 512], BF16)
    qg = k.sb(es, "qg", [128, 512], BF16)
    kqT = k.sb(es, "kqT", [128, 8, 128], BF16)
    qgT = k.sb(es, "qgT", [128, 4, 128], BF16)
    Mg = k.sb(es, "Mg", [128, 4, 128], F32, nparts=4)
    Ls = k.sb(es, "Ls", [128, 4, 128], F32)
    LT = k.sb(es, "LT", [128, 4, 128], F32)
    Am = [k.sb(es, "Am%d" % i, [128, 4, 128], BF16) for i in range(2)]
    AmT = [k.sb(es, "AmT%d" % i, [128, 4, 128], BF16) for i in range(2)]
    TT = [k.sb(es, "TT%d" % i, [128, 4, 128], BF16) for i in range(2)]
    nwT = k.sb(es, "nwT", [128, 4, 128], BF16)
    aqk = k.sb(es, "aqk", [128, 4, 128], BF16)
    vn = k.sb(es, "vn", [128, 4, 128], BF16, nparts=4)
    gsd2 = [k.sb(es, "gsd%d" % i, [128, 512], F32) for i in range(2)]
    osd = k.sb(es, "osd", [128, 512], F32)
    A = slice(None)

    def v4(b):
        return V(b.t[:, :].rearrange("p (h d) -> p h d", h=4), b.toks)

    for t in range(NT):
        x_ = xt[t % 2]
        ob = oab[t % 2]
        ub = ub2[t % 2]
        uT = uT2[t % 2]
        gsd = gsd2[t % 2]
        x_to_uT(k, xin_d, t, x_, junk, ss, lnv, rstd, ub, uT, PB[0], ident, "xin%d" % (t % 2))
        proj_tm(k, PF[0], uT, Wf)
        k.act(kf[:], PF[0][:], AF.Sigmoid, scale=-1.0)
        k.tt(kf[:], kf[:], omlb[:], ALU.mult)
        k.act(lf[:], kf[:], AF.Ln, scale=-1.0, bias=1.0)
        k.mm(PF[0][:], M1[:], lf[:])
        k.mm(PF[1][:], M2[:], lf[:])
        for h in range(4):
            k.mm(PF[2][:, 2 * h:2 * h + 2], lf[:, h * 128:(h + 1) * 128], cind[:])
        k.act(eb[:], PF[0][:], AF.Exp)
        k.act(enb[:], PF[0][:], AF.Exp, scale=-1.0)
        k.act(esf[:], PF[1][:], AF.Exp)
        k.act(dlh[:], PF[2][:, 0:8], AF.Exp)
        proj_tm(k, PF[3], uT, Wq)
        k.tt(qe[:], PF[3][:], eb[:], ALU.mult)
        k.tt(ke[:], kf[:], enb[:], ALU.mult)
        k.tt(kdh[:], kf[:], esf[:], ALU.mult)
        proj_tm(k, PF[4], uT, Wi)
        k.cp(vh[:], PF[4][:], eng="act")
        proj_tm(k, PF[5], uT, Wg)
        k.act(gsw[:], PF[5][:], AF.Silu)
        k.tt(v4(gsw), v4(gsw), bc_mid(hgn, (A, A), [128, 4, 128]), ALU.mult)
        proj_tm(k, PF[5], uT, Wdg)
        k.act(gsd[:], PF[5][:], AF.Silu)
        k.tt(v4(gsd), v4(gsd), bc_mid(dnn, (A, A), [128, 4, 128]), ALU.mult)
        for h in range(4):
            k.tr(PB[1][:, h * 128:(h + 1) * 128], qe[:, h * 128:(h + 1) * 128], ident[:])
            k.tr(PB[1][:, 512 + h * 128:512 + (h + 1) * 128], ke[:, h * 128:(h + 1) * 128], ident[:])
        k.cp(qkT[:], PB[1][:])
        for h in range(4):
            k.mm(PF[0][:, h * 128:(h + 1) * 128], qkT[:, 512 + h * 128:512 + (h + 1) * 128],
                 qkT[:, h * 128:(h + 1) * 128])
        k.tt(attnT[:], v4(PF[0]), bc_mid(M1, (A, A), [128, 4, 128]), ALU.mult)
        po = PF[1]
        for h in range(4):
            hs = slice(h * 128, (h + 1) * 128)
            k.mm(po[:, hs], attnT[:, h, :], vh[:, hs], start=True, stop=False)
            for c in range(2):
                cs = slice(64 * c, 64 * c + 64)
                k.mm(po[cs, hs], qkT[:, h * 128 + 64 * c:h * 128 + 64 * c + 64], Shb.p(h, (A, h, A)),
                     start=False, stop=True)
                k.mm(PF[2 + (h % 2)][:, 0:128], kdh[cs, hs], vh[cs, hs])
                k.stt(Sh.p(h, (A, h, A)), Sh.p(h, (A, h, A)), dlh[:, 2 * h + c:2 * h + c + 1],
                      PF[2 + (h % 2)][:, 0:128], ALU.mult, ALU.add)
                k.cp(Shb.p(h, (A, h, A)), Sh.p(h, (A, h, A)))
        head_norm_gate(k, po, ss4, ln4, r4, osb, gsw, ob.p(0, (A, slice(0, 512))))

        if CUT == "hg":
            k.dma("sp", oab_d.rows(t), ob[:], "oabw%d" % (t % 2))
            continue
        k.cp(cvin[:, :, 0:3], cvin[:, :, 128:131])
        for i in range(3):
            pf = PF[2 + i]
            for cc in range(4):
                for c in range(8):
                    k.mm(pf[:, cc * 128:(cc + 1) * 128], Wc[i][:, c, cc * 128:(cc + 1) * 128],
                         uT[:, c * 128:(c + 1) * 128], start=(c == 0), stop=(c == 7))
            k.cp(cvin[:, 4 * i:4 * i + 4, 3:131], v4(pf), eng="act")
        for i in range(3):
            pf = PF[2 + i]
            for cc in range(4):
                c = 4 * i + cc
                for j in range(4):
                    k.mm(pf[:, cc * 128:(cc + 1) * 128], diagw[:, c, j, :], cvin[:, c, j:j + 128],
                         start=(j == 0), stop=(j == 3))
            k.act(qkvF[:, 4 * i:4 * i + 4, :], v4(pf), AF.Silu)
        for c in range(8):
            k.tr(PB[0][:, c * 128:(c + 1) * 128], qkvF[:, c, :], ident[:])
        k.cp(qkvT[:, 0:1024], PB[0][:])
        for c in range(8, 12):
            k.tr(PB[1][:, (c - 8) * 128:(c - 7) * 128], qkvF[:, c, :], ident[:])
        k.cp(qkvT[:, 1024:1536], PB[1][:, 0:512], eng="act")
        if CUT == "conv":
            k.cp(ob.p(1, (A, slice(512, 1024))), qkvT[:, 0:512])
            k.dma("sp", oab_d.rows(t), ob[:], "oabw%d" % (t % 2))
            continue
        for j in range(8):
            k.act(osd[:, (j % 4) * 128:(j % 4 + 1) * 128], qkvT[:, j * 128:(j + 1) * 128], AF.Square,
                  accum=ss8[:, j:j + 1])
        k.act(ln8[:], ss8[:], AF.Ln, bias=EPS)
        k.act(r8[:], ln8[:], AF.Exp, scale=-0.5)
        if CUT == "n1":
            k.dma("sp", oab_d.rows(t), ob[:], "oabw%d" % (t % 2))
            continue
        pab = PF[5]
        for c in range(8):
            k.mm(pab[:, 0:8], uT[:, c * 128:(c + 1) * 128], Wab[:, c, :], start=(c == 0), stop=(c == 7))
        k.cp(ab8[:], pab[:, 0:8], eng="act")
        k.act(beta[:], ab8[:, 4:8], AF.Sigmoid)
        k.tt(gt[:], ab8[:, 0:4], dtb[:], ALU.add)
        k.act(gt[:], gt[:], AF.Exp)
        k.act(gt[:], gt[:], AF.Ln, bias=1.0)
        k.tt(gt[:], gt[:], negA[:], ALU.mult)
        if CUT == "n2":
            k.dma("sp", oab_d.rows(t), ob[:], "oabw%d" % (t % 2))
            continue
        k.mm(pab[:, 16:20], M1[:], gt[:])
        k.mm(pab[:, 24:28], M2[:], gt[:])
        k.tt(gmask[:], bc(gt, (A, A), [128, 4, 2]), bc_mid(cind, (A, A), [128, 4, 2]), ALU.mult)
        k.mm(pab[:, 32:40], ones[:], V(gmask.t[:, :, :].rearrange("p h c -> p (h c)"), gmask.toks))
        k.act(egc[:], pab[:, 16:20], AF.Exp)
        k.act(egs[:], pab[:, 24:28], AF.Exp)
        k.act(dld[:], pab[:, 32:40], AF.Exp)
        if CUT == "n3":
            k.dma("sp", oab_d.rows(t), ob[:], "oabw%d" % (t % 2))
            continue
        k.ts(sq4[:], r8[:, 0:4], float(128 ** -0.5), None, ALU.mult)
        k.tt(bg[:], beta[:], egc[:], ALU.mult)

        def hv(b, lo):
            return V(b.t[:, lo:lo + 512].rearrange("p (h d) -> p h d", h=4), b.toks)

        sh3 = [128, 4, 128]
        for h in range(4):
            hs = slice(h * 128, (h + 1) * 128)
            k.act(kn[:, hs], qkvT[:, 512 + h * 128:512 + (h + 1) * 128], AF.Copy, scale=r8[:, 4 + h:5 + h])
            k.act(qn[:, hs], qkvT[:, h * 128:(h + 1) * 128], AF.Copy, scale=sq4[:, h:h + 1])
            k.act(vbd[:, hs], qkvT[:, 1024 + h * 128:1024 + (h + 1) * 128], AF.Copy, scale=beta[:, h:h + 1])
        for h in range(4):
            hs = slice(h * 128, (h + 1) * 128)
            k.ts(kbg[:, hs], kn[:, hs], bg[:, h:h + 1], None, ALU.mult)
            k.ts(kdd[:, hs], kn[:, hs], egs[:, h:h + 1], None, ALU.mult)
            k.ts(qg[:, hs], qn[:, hs], egc[:, h:h + 1], None, ALU.mult)
        if CUT == "n4":
            k.dma("sp", oab_d.rows(t), ob[:], "oabw%d" % (t % 2))
            continue
        for h in range(4):
            hs = slice(h * 128, (h + 1) * 128)
            k.tr(PB[0][:, hs], kn[:, hs], ident[:])
            k.tr(PB[0][:, 512 + h * 128:512 + (h + 1) * 128], qn[:, hs], ident[:])
            k.tr(PB[1][:, hs], qg[:, hs], ident[:])
        k.cp(V(kqT.t[:, :, :].rearrange("p h d -> p (h d)"), kqT.toks), PB[0][:])
        k.cp(V(qgT.t[:, :, :].rearrange("p h d -> p (h d)"), qgT.toks), PB[1][:, 0:512], eng="act")
        if CUT == "prep":
            k.cp(ob.p(1, (A, slice(512, 1024))), kbg[:])
            k.dma("sp", oab_d.rows(t), ob[:], "oabw%d" % (t % 2))
            continue
        for h in range(4):
            hs = slice(h * 128, (h + 1) * 128)
            k.ts(Mg.p(h, (A, h, A)), M1[:], gt[:, h:h + 1], None, ALU.mult)
            k.mm(PF[2][:, hs], Mg.p(h, (A, h, A)), M2[:], start=True, stop=False)
            k.mm(PF[2][:, hs], ident[:], NSb[:], start=False, stop=True)
            k.mm(PF[3][:, hs], M2[:], Mg.p(h, (A, h, A)), start=True, stop=False)
            k.mm(PF[3][:, hs], ident[:], NCTb[:], start=False, stop=True)
            k.mm(PF[0][:, hs], kqT[:, h, :], kqT[:, h, :])
            k.mm(PF[4][:, hs], kqT[:, h, :], kqT[:, 4 + h, :])
        k.act(Ls[:], v4(PF[2]), AF.Exp)
        k.act(LT[:], v4(PF[3]), AF.Exp)
        for h in range(4):
            hs = slice(h * 128, (h + 1) * 128)
            k.stt(Am[0][:, h, :], PF[0][:, hs], beta[:, h:h + 1], Ls[:, h, :], ALU.mult, ALU.mult)
        k.tt(aqk[:], v4(PF[4]), LT[:], ALU.mult)
        for h in range(4):
            k.tr(PB[0][:, h * 128:(h + 1) * 128], Am[0][:, h, :], ident[:])
        k.cp(V(AmT[0].t[:, :, :].rearrange("p h d -> p (h d)"), AmT[0].toks), PB[0][:, 0:512], eng="act")
        k.tt(TT[0][:], bc_mid(ident_f, (A, A), sh3), AmT[0][:], ALU.subtract)
        cur = 0
        for r in range(1, 6):
            nxt = 1 - cur
            pP, pPT, pT = PF[2], PF[3], PF[4]
            for h in range(4):
                hs = slice(h * 128, (h + 1) * 128)
                k.mm(pP[:, hs], AmT[cur][:, h, :], Am[cur][:, h, :])
                if r < 5:
                    k.mm(pPT[:, hs], Am[cur][:, h, :], AmT[cur][:, h, :])
            k.cp(Am[nxt][:], v4(pP))
            if r < 5:
                k.cp(AmT[nxt][:], v4(pPT), eng="act")
            for h in range(4):
                hs = slice(h * 128, (h + 1) * 128)
                k.mm(pT[:, hs], Am[nxt][:, h, :], TT[cur][:, h, :])
            k.tt(TT[nxt][:], v4(pT), TT[cur][:], ALU.add)
            cur = nxt
        TTf = TT[cur]
        if CUT == "inv":
            k.cp(ob.p(1, (A, slice(512, 1024))), V(TTf.t[:, :, :].rearrange("p h d -> p (h d)"), TTf.toks))
            k.dma("sp", oab_d.rows(t), ob[:], "oabw%d" % (t % 2))
            continue
        for h in range(4):
            hs = slice(h * 128, (h + 1) * 128)
            k.mm(PF[2][:, hs], kbg[:, hs], TTf[:, h, :])
        k.act(nwT[:], v4(PF[2]), AF.Copy, scale=-1.0)
        pod = PF[4]
        for h in range(4):
            hs = slice(h * 128, (h + 1) * 128)
            pvn = PF[3] if h % 2 == 0 else PF[2]
            k.mm(pvn[:, hs], TTf[:, h, :], vbd[:, hs], start=True, stop=False)
            for c in range(2):
                cs = slice(64 * c, 64 * c + 64)
                k.mm(pvn[cs, hs], nwT[:, h, cs], Sdb.p(h, (A, h, A)), start=False, stop=True)
                k.mm(pod[cs, hs], qgT[:, h, cs], Sdb.p(h, (A, h, A)), start=True, stop=False)
                k.cp(vn.p(h, (cs, h, A)), pvn[cs, hs], eng="act")
                k.mm(PF[(h % 2)][:, 0:128], kdd[cs, hs], vn.p(h, (cs, h, A)))
                k.stt(Sd.p(h, (A, h, A)), Sd.p(h, (A, h, A)), dld[:, 2 * h + c:2 * h + c + 1],
                      PF[(h % 2)][:, 0:128], ALU.mult, ALU.add)
                k.cp(Sdb.p(h, (A, h, A)), Sd.p(h, (A, h, A)))
            k.mm(pod[:, hs], aqk[:, h, :], vn.p(h, (A, h, A)), start=False, stop=True)
        head_norm_gate(k, pod, ss4, ln4, r4, osd, gsd, ob.p(1, (A, slice(512, 1024))))
        k.dma("pool", oab_d.rows(t), ob[:], "oabw%d" % (t % 2))


def phase_b(k, es, l, NT, PF, PB, ident, ident_f, xin_d, oab_d, hout_d, pre_mix_d, w_in_d, w_o_hg_d, w_o_dn_d, w_out_d,
            post_mix_d):
    cnt = [0]
    win = w_in_d[l]
    Wm = [load_wcols(k, es, "Wm%d" % i, win, 4104 + 512 * i, 4616 + 512 * i, cnt) for i in range(4)]
    Whg = load_wcols(k, es, "Whg", w_o_hg_d[l], 0, D, cnt, kch=4)
    Wdn = load_wcols(k, es, "Wdn", w_o_dn_d[l], 0, D, cnt, kch=4)
    Wout = load_wcols(k, es, "Wout", w_out_d[l], 0, D, cnt)
    gcol = load_gcol(k, es, "gcolB", pre_mix_d[l], PF[0], ident_f, "cst0")
    gbc = k.sb(es, "gbcB", [128, D], F32)
    k.dma("sp", gbc[:], post_mix_d[l:l + 1, :].partition_broadcast(128), "cst1")
    for wt in Wm:
        fold_gain(k, wt, gcol, 8, 512)
    xt = [k.sb(es, "xtB%d" % i, [128, D], F32) for i in range(2)]
    ot = [k.sb(es, "otB%d" % i, [128, D], BF16) for i in range(2)]
    ht = [k.sb(es, "htB%d" % i, [128, D], F32) for i in range(2)]
    def two(name, shape, dt, nparts=1):
        return [k.sb(es, "%s%d" % (name, i), shape, dt, nparts=nparts) for i in range(2)]
    junk2 = two("junkB", [128, D], BF16, 2)
    ssx = two("ssB", [128, 2], F32)
    lnvx = two("lnvB", [128, 1], F32)
    rstdx = two("rstdB", [128, 1], F32)
    ss2x = two("ss2B", [128, 2], F32)
    lnv2x = two("lnv2B", [128, 1], F32)
    rstd2x = two("rstd2B", [128, 1], F32)
    ubx = two("ubB", [128, D], BF16)
    uTx = two("uTB", [128, D], BF16)
    oTx = two("oTB", [128, D], BF16)
    sgmx = two("sgm", [128, 4, 512], F32, 4)
    t1x = two("t1B", [128, 512], F32)
    mgx = two("mgB", [128, D], BF16, 2)
    mgTx = two("mgTB", [128, D], BF16)
    A = slice(None)
    for t in range(NT):
        x_ = xt[t % 2]
        o_ = ot[t % 2]
        h_ = ht[t % 2]
        i2 = t % 2
        junk, ss, lnv, rstd, ss2, lnv2, rstd2 = junk2[i2], ssx[i2], lnvx[i2], rstdx[i2], ss2x[i2], lnv2x[i2], rstd2x[i2]
        ub, uT, oT, sgm, t1, mg, mgT = ubx[i2], uTx[i2], oTx[i2], sgmx[i2], t1x[i2], mgx[i2], mgTx[i2]
        k.dma("sp", o_[:], oab_d.rows(t), "oabr%d" % (t % 2))
        x_to_uT(k, xin_d, t, x_, junk, ss, lnv, rstd, ub, uT, PB[0], ident, "xin%d" % (t % 2))
        for i in range(4):
            proj_tm(k, PF[i], uT, Wm[i])
            k.act(sgm.p(i, (A, i, A)), PF[i][:], AF.Sigmoid)
        for c in range(8):
            k.tr(PB[1][:, c * 128:(c + 1) * 128], o_[:, c * 128:(c + 1) * 128], ident[:])
        k.cp(oT[:], PB[1][:])
        for hh in range(2):
            cs = slice(hh * 512, (hh + 1) * 512)
            pa, pb_ = PF[2 * hh], PF[2 * hh + 1]
            for c in range(4):
                k.mm(pa[:], oT[:, c * 128:(c + 1) * 128], Whg[:, c, cs], start=(c == 0), stop=(c == 3))
            for c in range(4):
                k.mm(pb_[:], oT[:, 512 + c * 128:512 + (c + 1) * 128], Wdn[:, c, cs], start=(c == 0), stop=(c == 3))
            k.tt(t1[:], pa[:], sgm.p(hh, (A, hh, A)), ALU.mult)
            k.tt(sgm.p(2 + hh, (A, 2 + hh, A)), pb_[:], sgm.p(2 + hh, (A, 2 + hh, A)), ALU.mult)
            k.tt(mg.p(hh, (A, cs)), t1[:], sgm.p(2 + hh, (A, 2 + hh, A)), ALU.add)
        for c in range(8):
            k.tr(PB[0][:, c * 128:(c + 1) * 128], mg.p(c // 4, (A, slice(c * 128, (c + 1) * 128))), ident[:])
        k.cp(mgT[:], PB[0][:], eng="act")
        for hh in range(2):
            for c in range(8):
                k.mm(PF[4 + hh][:], mgT[:, c * 128:(c + 1) * 128], Wout[:, c, hh * 512:(hh + 1) * 512],
                     start=(c == 0), stop=(c == 7))
        rms_stats(k, [PF[4][:], PF[5][:]], ss2,
                  [junk.p(0, (A, slice(0, 512))), junk.p(1, (A, slice(512, 1024)))], rstd2, lnv2)
        for hh in range(2):
            sl = slice(hh * 512, (hh + 1) * 512)
            k.stt(h_[:, sl], PF[4 + hh][:], rstd2[:, 0:1], gbc[:, sl], ALU.mult, ALU.mult)
        k.tt(h_[:], h_[:], x_[:], ALU.add, eng="pool")
        k.dma("pool", hout_d.rows(t), h_[:], "hout%d" % (t % 2))


_CACHE = {}

WNAMES = ["hg_lower_bounds", "pre_mix_w", "w_in", "hg_norm_w", "conv_w", "dn_a_log", "dn_dt_bias", "dn_norm_w",
          "w_o_hg", "w_o_dn", "w_out", "post_mix_w", "pre_ffn_w", "w_gate_up", "w_down", "post_ffn_w"]


def kernel(**inputs):
    x = np.ascontiguousarray(inputs["x"], dtype=np.float32)
    B, T, _ = x.shape
    if "nc" not in _CACHE:
        _CACHE["nc"] = build(T=T, L=2)
    nc = _CACHE["nc"]
    shared = {n: np.ascontiguousarray(inputs[n], dtype=np.float32) for n in WNAMES}
    in_maps = []
    for b in range(B):
        m = dict(shared)
        m["x"] = x[b]
        in_maps.append(m)
    res = run_bass_kernel_spmd(nc, in_maps, core_ids=list(range(B)))
    return np.stack([r["out"] for r in res.results], axis=0)
```
